# Optimizing a Trainium2 kernel written in Bass

```python
import jax
import jax.numpy as jnp
from jax import lax
import numpy as np


D_MODEL = 1024
BATCH = 8
SEQ = 4096
DEPTH = 2

N_MIXERS = 4
GROUP_W = D_MODEL // N_MIXERS
N_GROUP_HEADS = 4
HEAD_DIM = GROUP_W // N_GROUP_HEADS
ROPE_BASE = 10000.0
RET_CHUNK = 64
RWKV_DECAY_LORA = D_MODEL // 16
RWKV_AAA_LORA = D_MODEL // 16
RWKV_GATE_LORA = D_MODEL // 8
RWKV_GN_EPS = 64e-5
CONV_W = 4
CONV_PAD_LEFT = CONV_W // 2
RGLRU_C = 8.0
GLA_KDIM = GROUP_W // 2
GLA_HEAD_K = GLA_KDIM // N_GROUP_HEADS
GLA_GATE_LORA = 16
GLA_TAU = 16.0
GLA_CHUNK = 64
N_EXPERTS = 16
EC_CAPACITY_FACTOR = 2
D_EXPERT = D_MODEL
NORM_EPS = 1e-6

SPLIT_SIZES = (
    GROUP_W, GROUP_W, GROUP_W, GROUP_W,
    GROUP_W, GROUP_W, GROUP_W,
    RWKV_DECAY_LORA, RWKV_AAA_LORA, RWKV_GATE_LORA,
    GROUP_W, GROUP_W,
    GLA_KDIM, GLA_KDIM, GROUP_W, GROUP_W, GLA_GATE_LORA,
)
IN_COLS = sum(SPLIT_SIZES)
SPLIT_POINTS = tuple(sum(SPLIT_SIZES[:i + 1]) for i in range(len(SPLIT_SIZES) - 1))

kernel_name = "hybrid_parallel_heads_ec_moe_encoder"


def _rmsnorm(x, w):
    xf = x.astype(jnp.float32)
    xf = xf * lax.rsqrt(jnp.mean(xf * xf, axis=-1, keepdims=True) + NORM_EPS)
    return (xf * w.astype(jnp.float32)).astype(x.dtype)


def _head_norm(y, w, center, eps=NORM_EPS):
    yf = y.astype(jnp.float32)
    if center:
        yf = yf - jnp.mean(yf, axis=-1, keepdims=True)
    yf = yf * lax.rsqrt(jnp.mean(yf * yf, axis=-1, keepdims=True) + eps)
    return yf * w.reshape(y.shape[-2], y.shape[-1])


def _heads(z, d):
    return z.reshape(z.shape[0], z.shape[1], -1, d)


def _flip(z):
    return jnp.flip(z, axis=1)


def _rotary(x, positions):
    d = x.shape[-1]
    inv = jnp.power(ROPE_BASE, -jnp.arange(0, d, 2, dtype=jnp.float32) / d)
    ang = positions.astype(jnp.float32)[..., None] * inv
    cos = jnp.cos(ang)[:, :, None, :]
    sin = jnp.sin(ang)[:, :, None, :]
    x1, x2 = x[..., : d // 2], x[..., d // 2:]
    return jnp.concatenate([x1 * cos - x2 * sin, x1 * sin + x2 * cos], axis=-1)


def _retention_chunked(q, k, v, log_gamma):
    B, T, H, d = q.shape
    C = RET_CHUNK
    n = T // C
    qc, kc, vc = (z.reshape(B, n, C, H, d) for z in (q, k, v))
    lg = log_gamma.astype(jnp.float32)
    idx = jnp.arange(C, dtype=jnp.float32)
    rel = idx[:, None] - idx[None, :]
    dmask = jnp.where(rel >= 0, jnp.exp(lg[:, None, None] * jnp.maximum(rel, 0.0)), 0.0)
    scores = jnp.einsum('bnihd,bnjhd->bnhij', qc, kc) * dmask
    y_intra = jnp.einsum('bnhij,bnjhe->bnihe', scores, vc)
    zeta = jnp.exp(lg[:, None] * (C - 1 - idx)[None, :])
    kv = jnp.einsum('bnjhd,hj,bnjhe->bnhde', kc, zeta, vc)
    chunk_decay = jnp.exp(lg * C)[:, None, None]

    def step(state, kv_i):
        return state * chunk_decay + kv_i, state

    _, prev = lax.scan(step, jnp.zeros((B, H, d, d), kv.dtype), jnp.moveaxis(kv, 1, 0))
    prev = jnp.moveaxis(prev, 0, 1)
    xi = jnp.exp(lg[:, None] * (idx + 1.0)[None, :])
    y_cross = jnp.einsum('bnihd,bnhde,hi->bnihe', qc, prev, xi)
    return (y_intra + y_cross).reshape(B, T, H, d)


def _retention_group(q, k, v, g, positions, log_decay, gn_w):
    B, T, _ = q.shape
    qh = _rotary(_heads(q, HEAD_DIM), positions)
    kh = _rotary(_heads(k, HEAD_DIM), positions) * (HEAD_DIM ** -0.5)
    vh = _heads(v, HEAD_DIM)
    y = _retention_chunked(qh, kh, vh, log_decay[0]) + _flip(
        _retention_chunked(_flip(qh), _flip(kh), _flip(vh), log_decay[1]))
    return _head_norm(y, gn_w, center=True).reshape(B, T, GROUP_W) * jax.nn.silu(g)


def _token_shift(z, mu):
    prev = jnp.pad(z, ((0, 0), (1, 0), (0, 0)))[:, :-1]
    return z + mu * (prev - z)


def _rwkv7_direction(r, k, v, xw, xa, mu_rkv, mu_w, mu_a, w0, w_up, a0, a_up, k_k, k_a, r_k):
    B, T, _ = r.shape
    r = _token_shift(r, mu_rkv[0])
    k = _token_shift(k, mu_rkv[1])
    v = _token_shift(v, mu_rkv[2])
    xw = _token_shift(xw, mu_w)
    xa = _token_shift(xa, mu_a)
    w_log = -jax.nn.softplus(-(w0 + jnp.tanh(xw) @ w_up)) - 0.5
    decay = jnp.exp(-jnp.exp(w_log))
    a = jax.nn.sigmoid(a0 + xa @ a_up)
    kk = _heads(k * k_k, HEAD_DIM)
    kk = kk / jnp.maximum(jnp.sqrt(jnp.sum(kk * kk, axis=-1, keepdims=True)), 1e-12)
    k = k * (1.0 + (a - 1.0) * k_a)
    r_h, k_h, v_h, w_h, a_h = (_heads(z, HEAD_DIM) for z in (r, k, v, decay, a))

    def step(S, inp):
        r_t, w_t, k_t, v_t, kk_t, a_t = inp
        sa = jnp.einsum('bhvk,bhk->bhv', S, -kk_t)
        S = (S * w_t[:, :, None, :] + sa[..., None] * (kk_t * a_t)[:, :, None, :]
             + v_t[..., None] * k_t[:, :, None, :])
        return S, jnp.einsum('bhvk,bhk->bhv', S, r_t)

    xs = tuple(jnp.moveaxis(z, 1, 0) for z in (r_h, w_h, k_h, v_h, kk, a_h))
    _, y = lax.scan(step, jnp.zeros((B, N_GROUP_HEADS, HEAD_DIM, HEAD_DIM), r.dtype), xs)
    y = jnp.moveaxis(y, 0, 1)
    bonus = jnp.sum(r_h * k_h * r_k, axis=-1, keepdims=True) * v_h
    return y, bonus


def _rwkv7_group(r, k, v, xw, xa, xg, mu_rkv, mu_w, mu_a, w0, w_up, a0, a_up, g_up,
                 k_k, k_a, r_k, gn_w):
    B, T, _ = r.shape
    y_f, b_f = _rwkv7_direction(r, k, v, xw, xa, mu_rkv, mu_w, mu_a,
                                w0[0], w_up[0], a0[0], a_up[0], k_k, k_a, r_k)
    y_b, b_b = _rwkv7_direction(_flip(r), _flip(k), _flip(v), _flip(xw), _flip(xa), mu_rkv, mu_w, mu_a,
                                w0[1], w_up[1], a0[1], a_up[1], k_k, k_a, r_k)
    y = y_f + _flip(y_b)
    bonus = b_f + _flip(b_b)
    o = (_head_norm(y, gn_w, center=True, eps=RWKV_GN_EPS) + bonus).reshape(B, T, GROUP_W)
    return o * (jax.nn.sigmoid(xg) @ g_up)


def _conv_centred(x, w, b):
    T = x.shape[1]
    xp = jnp.pad(x, ((0, 0), (CONV_PAD_LEFT, CONV_W - 1 - CONV_PAD_LEFT), (0, 0)))
    return sum(xp[:, j:j + T] * w[j] for j in range(CONV_W)) + b


def _lin_comb(left, right):
    a1, b1 = left
    a2, b2 = right
    return a1 * a2, a2 * b1 + b2


def _rglru_group(xr, gate, conv_w, conv_b, gate_w, gate_b, lam):
    B, T, _ = xr.shape
    xc = _conv_centred(xr, conv_w, conv_b)
    xh = _heads(xc, HEAD_DIM)

    def direction(dirn, reverse):
        gx = jnp.einsum('bthd,ghde->gbthe', xh, gate_w[dirn]).reshape(2, B, T, GROUP_W)
        gx = gx + gate_b[dirn][:, None, None, :]
        rec_gate = jax.nn.sigmoid(gx[0])
        in_gate = jax.nn.sigmoid(gx[1])
        log_a = -RGLRU_C * rec_gate * jax.nn.softplus(-lam[dirn])
        a = jnp.exp(log_a)
        bx = jnp.sqrt(-jnp.expm1(2.0 * log_a)) * (in_gate * xc)
        _, h = lax.associative_scan(_lin_comb, (a, bx), axis=1, reverse=reverse)
        return h

    h = direction(0, False) + direction(1, True)
    return h * jax.nn.gelu(gate)


def _gla_chunked(q, k, v, log_alpha):
    B, T, H, dk = q.shape
    dv = v.shape[-1]
    C = GLA_CHUNK
    n = T // C
    q, k, log_alpha = (z.reshape(B, n, C, H, dk) for z in (q, k, log_alpha))
    v = v.reshape(B, n, C, H, dv)
    bcum = jnp.cumsum(log_alpha, axis=2)
    b_last = bcum[:, :, -1:]
    q_dec = q * jnp.exp(bcum)
    k_dec = k * jnp.exp(-bcum)
    lower = jnp.tril(jnp.ones((C, C), dtype=bool))
    scores = jnp.where(lower, jnp.einsum('bnihk,bnjhk->bnhij', q_dec, k_dec), 0.0)
    y_intra = jnp.einsum('bnhij,bnjhv->bnihv', scores, v)
    kv = jnp.einsum('bnjhk,bnjhv->bnhkv', k * jnp.exp(b_last - bcum), v)
    chunk_decay = jnp.exp(b_last[:, :, 0])

    def step(S, inp):
        dec, kv_i = inp
        return S * dec[..., None] + kv_i, S

    _, s_prev = lax.scan(step, jnp.zeros((B, H, dk, dv), kv.dtype),
                         (jnp.moveaxis(chunk_decay, 1, 0), jnp.moveaxis(kv, 1, 0)))
    y_cross = jnp.einsum('bnihk,bnhkv->bnihv', q_dec, jnp.moveaxis(s_prev, 0, 1))
    return (y_intra + y_cross).reshape(B, T, H, dv)


def _gla_group(q, k, v, og, xa, alpha_up, alpha_b, gn_w):
    B, T, _ = q.shape
    qh = _heads(q, GLA_HEAD_K) * (GLA_HEAD_K ** -0.5)
    kh = _heads(k, GLA_HEAD_K)
    vh = _heads(v, HEAD_DIM)
    la_f = _heads(jax.nn.log_sigmoid(xa @ alpha_up[0] + alpha_b[0]) / GLA_TAU, GLA_HEAD_K)
    la_b = _heads(jax.nn.log_sigmoid(xa @ alpha_up[1] + alpha_b[1]) / GLA_TAU, GLA_HEAD_K)
    y = _gla_chunked(qh, kh, vh, la_f) + _flip(
        _gla_chunked(_flip(qh), _flip(kh), _flip(vh), _flip(la_b)))
    return _head_norm(y, gn_w, center=False).reshape(B, T, GROUP_W) * jax.nn.silu(og)


def _expert_choice_ffn(x, router_w, router_b, w_gate, w_up, w_down):
    B, T, D = x.shape
    cap = EC_CAPACITY_FACTOR * T // N_EXPERTS
    logits = jnp.einsum('btd,de->bte', x, router_w) + router_b
    aff = jax.nn.softmax(logits.astype(jnp.float32), axis=-1)
    gates, idx = lax.top_k(jnp.swapaxes(aff, 1, 2), cap)
    bidx = jnp.arange(B)[:, None, None]
    xs = x[bidx, idx]
    hid = jax.nn.silu(jnp.einsum('becd,edf->becf', xs, w_gate)) * jnp.einsum('becd,edf->becf', xs, w_up)
    out = jnp.einsum('becf,efd->becd', hid, w_down) * gates[..., None].astype(x.dtype)
    return jnp.zeros_like(x).at[bidx, idx].add(out.astype(x.dtype))


def setup_inputs(seed: int = 0) -> dict:
    key = jax.random.key(seed)
    ks = iter(jax.random.split(key, 40))
    f32 = jnp.float32
    L, D, W, H, hd = DEPTH, D_MODEL, GROUP_W, N_GROUP_HEADS, HEAD_DIM

    def normal(shape, scale):
        return scale * jax.random.normal(next(ks), shape, f32)

    def gain(shape):
        return 1.0 + normal(shape, 0.02)

    def uniform(shape, lo=0.0, hi=1.0):
        return jax.random.uniform(next(ks), shape, f32, lo, hi)

    x = normal((BATCH, SEQ, D), 1.0)
    positions = jnp.broadcast_to(jnp.arange(SEQ, dtype=jnp.int32), (BATCH, SEQ))
    norm_mix = gain((L, D))
    w_in = normal((L, D, IN_COLS), D ** -0.5)
    w_out = normal((L, D, D), D ** -0.5)
    base = jnp.log1p(-jnp.exp2(-5.0 - jnp.arange(H, dtype=f32)))
    ret_log_decay = base * (1.0 + normal((L, 2, H), 0.05))
    ret_gn = gain((L, W))
    rwkv_mu_rkv = uniform((L, 3, W))
    rwkv_mu_w = uniform((L, RWKV_DECAY_LORA))
    rwkv_mu_a = uniform((L, RWKV_AAA_LORA))
    ramp = jnp.linspace(0.0, 1.0, W, dtype=f32)
    rwkv_w0 = -6.0 + 5.0 * ramp ** 1.5 + normal((L, 2, W), 0.1)
    rwkv_w_up = normal((L, 2, RWKV_DECAY_LORA, W), 0.5 * RWKV_DECAY_LORA ** -0.5)
    rwkv_a0 = normal((L, 2, W), 0.1)
    rwkv_a_up = normal((L, 2, RWKV_AAA_LORA, W), 0.5 * RWKV_AAA_LORA ** -0.5)
    rwkv_g_up = normal((L, RWKV_GATE_LORA, W), RWKV_GATE_LORA ** -0.5)
    rwkv_k_k = 0.85 + normal((L, W), 0.02)
    rwkv_k_a = 1.0 + normal((L, W), 0.02)
    rwkv_r_k = normal((L, H, hd), 0.1)
    rwkv_gn = gain((L, W))
    lru_conv_w = normal((L, CONV_W, W), CONV_W ** -0.5)
    lru_conv_b = normal((L, W), 0.01)
    lru_gate_w = normal((L, 2, 2, H, hd, hd), hd ** -0.5)
    lru_gate_b = normal((L, 2, 2, W), 0.01)
    a_init = uniform((L, 2, W), 0.9, 0.999) ** (1.0 / RGLRU_C)
    lru_lambda = jnp.log(a_init) - jnp.log1p(-a_init)
    gla_alpha_up = normal((L, 2, GLA_GATE_LORA, GLA_KDIM), GLA_GATE_LORA ** -0.5)
    gla_alpha_b = 1.0 + 2.0 * uniform((L, 2, GLA_KDIM))
    gla_gn = gain((L, W))
    norm_ffn = gain((L, D))
    router_w = normal((L, D, N_EXPERTS), D ** -0.5)
    router_b = normal((L, N_EXPERTS), 0.01)
    exp_w_gate = normal((L, N_EXPERTS, D, D_EXPERT), D ** -0.5)
    exp_w_up = normal((L, N_EXPERTS, D, D_EXPERT), D ** -0.5)
    exp_w_down = normal((L, N_EXPERTS, D_EXPERT, D), D_EXPERT ** -0.5)
    norm_final = gain((D,))
    return {
        "x": x, "positions": positions, "norm_mix": norm_mix, "w_in": w_in, "w_out": w_out,
        "ret_log_decay": ret_log_decay, "ret_gn": ret_gn,
        "rwkv_mu_rkv": rwkv_mu_rkv, "rwkv_mu_w": rwkv_mu_w, "rwkv_mu_a": rwkv_mu_a,
        "rwkv_w0": rwkv_w0, "rwkv_w_up": rwkv_w_up, "rwkv_a0": rwkv_a0, "rwkv_a_up": rwkv_a_up,
        "rwkv_g_up": rwkv_g_up, "rwkv_k_k": rwkv_k_k, "rwkv_k_a": rwkv_k_a, "rwkv_r_k": rwkv_r_k,
        "rwkv_gn": rwkv_gn, "lru_conv_w": lru_conv_w, "lru_conv_b": lru_conv_b,
        "lru_gate_w": lru_gate_w, "lru_gate_b": lru_gate_b, "lru_lambda": lru_lambda,
        "gla_alpha_up": gla_alpha_up, "gla_alpha_b": gla_alpha_b, "gla_gn": gla_gn,
        "norm_ffn": norm_ffn, "router_w": router_w, "router_b": router_b,
        "exp_w_gate": exp_w_gate, "exp_w_up": exp_w_up, "exp_w_down": exp_w_down,
        "norm_final": norm_final,
    }


def reference(x, positions, norm_mix, w_in, w_out, ret_log_decay, ret_gn,
              rwkv_mu_rkv, rwkv_mu_w, rwkv_mu_a, rwkv_w0, rwkv_w_up, rwkv_a0, rwkv_a_up,
              rwkv_g_up, rwkv_k_k, rwkv_k_a, rwkv_r_k, rwkv_gn,
              lru_conv_w, lru_conv_b, lru_gate_w, lru_gate_b, lru_lambda,
              gla_alpha_up, gla_alpha_b, gla_gn,
              norm_ffn, router_w, router_b, exp_w_gate, exp_w_up, exp_w_down, norm_final):
    for l in range(DEPTH):
        h = _rmsnorm(x, norm_mix[l])
        proj = jnp.einsum('btd,dc->btc', h, w_in[l]).astype(jnp.float32)
        (rq, rk, rv, rg, wr, wk, wv, wxw, wxa, wxg, lx, lgate,
         gq, gk, gv, gog, gxa) = jnp.split(proj, SPLIT_POINTS, axis=-1)
        o_ret = _retention_group(rq, rk, rv, rg, positions, ret_log_decay[l], ret_gn[l])
        o_rwkv = _rwkv7_group(wr, wk, wv, wxw, wxa, wxg, rwkv_mu_rkv[l], rwkv_mu_w[l], rwkv_mu_a[l],
                              rwkv_w0[l], rwkv_w_up[l], rwkv_a0[l], rwkv_a_up[l], rwkv_g_up[l],
                              rwkv_k_k[l], rwkv_k_a[l], rwkv_r_k[l], rwkv_gn[l])
        o_lru = _rglru_group(lx, lgate, lru_conv_w[l], lru_conv_b[l], lru_gate_w[l],
                             lru_gate_b[l], lru_lambda[l])
        o_gla = _gla_group(gq, gk, gv, gog, gxa, gla_alpha_up[l], gla_alpha_b[l], gla_gn[l])
        mixed = jnp.concatenate([o_ret, o_rwkv, o_lru, o_gla], axis=-1).astype(x.dtype)
        x = x + jnp.einsum('btc,cd->btd', mixed, w_out[l])
        x = x + _expert_choice_ffn(_rmsnorm(x, norm_ffn[l]), router_w[l], router_b[l],
                                   exp_w_gate[l], exp_w_up[l], exp_w_down[l])
    return _rmsnorm(x, norm_final)
```

```python
import numpy as np
import concourse.bass as bass
import concourse.mybir as mybir
from concourse.bass_utils import run_bass_kernel_spmd

F32 = mybir.dt.float32
BF16 = mybir.dt.bfloat16
I32 = mybir.dt.int32
U32 = mybir.dt.uint32
AF = mybir.ActivationFunctionType
ALU = mybir.AluOpType
AX = mybir.AxisListType


class Res:
    __slots__ = ("name", "w", "r")

    def __init__(self, name):
        self.name = name
        self.w = None
        self.r = {}


class KB:
    NDMA = 32
    NHW = 20

    def __init__(self, nc, needed=None):
        self.nc = nc
        self.needed = needed
        self.used = {}
        self.rank = None
        if needed is not None:
            self.rank = {e: {v: i + 1 for i, v in enumerate(sorted(vs))} for e, vs in needed.items()}
        self.eng = {"pe": nc.tensor, "act": nc.scalar, "dve": nc.vector, "pool": nc.gpsimd, "sp": nc.sync}
        self.sem = {}
        self.cnt = {}
        self.pending = {}
        self._ctx = []
        for e in self.eng:
            s = nc.semaphore("s_" + e)
            self.sem[e] = s.__enter__()
            self._ctx.append(s)
            self.cnt[e] = 0
            self.pending[e] = False
        self.dsem = []
        self.dtarget = []
        for i in range(self.NDMA):
            s = nc.semaphore("d_%d" % i)
            self.dsem.append(s.__enter__())
            self._ctx.append(s)
            self.dtarget.append(0)
        self.drr = 0
        self.drr_sw = self.NHW
        self.waited = {e: {} for e in self.eng}
        self.ninst = 0
        self.outputs_tokens = []

    def sb(self, name, shape, dt):
        self._uid = getattr(self, "_uid", 0) + 1
        name = "%s_u%d" % (name, self._uid)
        g = self.nc.sbuf_tensor(name, list(shape), dt)
        t = g.__enter__()
        self._ctx.append(g)
        return t

    def ps(self, name, shape, dt):
        self._uid = getattr(self, "_uid", 0) + 1
        name = "%s_u%d" % (name, self._uid)
        g = self.nc.psum_tensor(name, list(shape), dt)
        t = g.__enter__()
        self._ctx.append(g)
        return t

    def _wait(self, e, tok):
        kind, key, val = tok
        if kind == "c":
            if key == e and e == "pe":
                return
            semkey = ("c", key)
            sem = self.sem[key]
        else:
            semkey = ("d", key)
            sem = self.dsem[key]
        if self.waited[e].get(semkey, 0) >= val:
            return
        self.waited[e][semkey] = val
        if kind == "c":
            self.used.setdefault(key, set()).add(val)
            if self.rank is not None:
                val = self.rank[key][val]
        self.eng[e].wait_ge(sem, val)
        self.ninst += 1

    def _deps(self, e, reads, writes):
        toks = []
        for r in reads:
            if r.w is not None:
                toks.append(r.w)
        for w in writes:
            if w.w is not None:
                if not (w.w[0] == "c" and w.w[1] == e and e != "pool"):
                    toks.append(w.w)
            for k, t in w.r.items():
                if t[0] == "c" and t[1] == e and e != "pool":
                    continue
                toks.append(t)
        return toks

    def _record(self, tok, reads, writes):
        for r in reads:
            k = (tok[0], tok[1])
            old = r.r.get(k)
            if old is None or old[2] < tok[2]:
                r.r[k] = tok
        for w in writes:
            w.w = tok
            w.r = {}

    def op(self, e, fn, reads=(), writes=(), inc=True):
        for t in self._deps(e, reads, writes):
            self._wait(e, t)
        ins = fn(self.eng[e])
        self.ninst += 1
        if inc:
            self.cnt[e] += 1
            if self.rank is None or self.cnt[e] in self.rank.get(e, {}):
                ins.then_inc(self.sem[e], 1)
                self.nincs = getattr(self, "nincs", 0) + 1
            self.pending[e] = False
            tok = ("c", e, self.cnt[e])
        else:
            self.pending[e] = True
            tok = ("c", e, self.cnt[e] + 1)
        self._record(tok, reads, writes)
        return tok

    def dma(self, q, out, in_, reads=(), writes=(), **kw):
        for t in self._deps(q, reads, writes):
            self._wait(q, t)
        if q == "pool":
            slot = self.drr_sw
            self.drr_sw = self.NHW + (self.drr_sw - self.NHW + 1) % (self.NDMA - self.NHW)
        else:
            slot = self.drr
            self.drr = (self.drr + 1) % self.NHW
        if self.dtarget[slot] > 0:
            self._wait(q, ("d", slot, self.dtarget[slot]))
        self.dtarget[slot] += 16
        ins = self.eng[q].dma_start(out=out, in_=in_, **kw)
        ins.then_inc(self.dsem[slot], 16)
        self.ninst += 1
        tok = ("d", slot, self.dtarget[slot])
        self._record(tok, reads, writes)
        return tok

    def dma_raw(self, q, fn, reads=(), writes=()):
        for t in self._deps(q, reads, writes):
            self._wait(q, t)
        if q == "pool":
            slot = self.drr_sw
            self.drr_sw = self.NHW + (self.drr_sw - self.NHW + 1) % (self.NDMA - self.NHW)
        else:
            slot = self.drr
            self.drr = (self.drr + 1) % self.NHW
        if self.dtarget[slot] > 0:
            self._wait(q, ("d", slot, self.dtarget[slot]))
        self.dtarget[slot] += 16
        ins = fn(self.eng[q])
        ins.then_inc(self.dsem[slot], 16)
        self.ninst += 1
        tok = ("d", slot, self.dtarget[slot])
        self._record(tok, reads, writes)
        return tok

    def finish(self, final_res):
        for r in final_res:
            if r.w is not None:
                self._wait("sp", r.w)
        for slot in range(self.NDMA):
            if self.dtarget[slot] > 0:
                self._wait("sp", ("d", slot, self.dtarget[slot]))

    def close(self):
        for g in reversed(self._ctx):
            g.__exit__(None, None, None)
        self._ctx = []


def _kb_barrier(self):
    for e in self.eng:
        assert not self.pending[e], e
    for e in self.eng:
        for e2 in self.eng:
            if e2 != e and self.cnt[e2] > 0:
                self._wait(e, ("c", e2, self.cnt[e2]))
        for slot in range(self.NDMA):
            if self.dtarget[slot] > 0:
                self._wait(e, ("d", slot, self.dtarget[slot]))


def _kb_scope_begin(self):
    self._marks = getattr(self, "_marks", [])
    self._marks.append(len(self._ctx))


def _kb_scope_end(self):
    self.barrier()
    m = self._marks.pop()
    while len(self._ctx) > m:
        g = self._ctx.pop()
        g.__exit__(None, None, None)


KB.barrier = _kb_barrier
KB.scope_begin = _kb_scope_begin
KB.scope_end = _kb_scope_end

D = 1024
T = 4096
NT = T // 128
DEPTH = 2
IN_COLS = 3344
NE = 16
CAP = 512
EPS = 1e-6
C_RQ, C_RK, C_RV, C_RG = 0, 256, 512, 768
C_WR, C_WK, C_WV, C_WXW, C_WXA, C_WXG = 1024, 1280, 1536, 1792, 1856, 1920
C_LX, C_LG = 2048, 2304
C_GQ, C_GK, C_GV, C_GOG, C_GXA = 2560, 2688, 2816, 3072, 3328

PARAM_SHAPES = {
    "norm_mix": (2, 1024), "w_in": (2, 1024, 3344), "w_out": (2, 1024, 1024),
    "ret_log_decay": (2, 2, 4), "ret_gn": (2, 256),
    "rwkv_mu_rkv": (2, 3, 256), "rwkv_mu_w": (2, 64), "rwkv_mu_a": (2, 64),
    "rwkv_w0": (2, 2, 256), "rwkv_w_up": (2, 2, 64, 256), "rwkv_a0": (2, 2, 256),
    "rwkv_a_up": (2, 2, 64, 256), "rwkv_g_up": (2, 128, 256), "rwkv_k_k": (2, 256),
    "rwkv_k_a": (2, 256), "rwkv_r_k": (2, 4, 64), "rwkv_gn": (2, 256),
    "lru_conv_w": (2, 4, 256), "lru_conv_b": (2, 256), "lru_gate_w": (2, 2, 2, 4, 64, 64),
    "lru_gate_b": (2, 2, 2, 256), "lru_lambda": (2, 2, 256),
    "gla_alpha_up": (2, 2, 16, 128), "gla_alpha_b": (2, 2, 128), "gla_gn": (2, 256),
    "norm_ffn": (2, 1024), "router_w": (2, 1024, 16), "router_b": (2, 16),
    "exp_w_gate": (2, 16, 1024, 1024), "exp_w_up": (2, 16, 1024, 1024),
    "exp_w_down": (2, 16, 1024, 1024), "norm_final": (1024,),
}


class Ctx:
    pass


def make_consts(k, g):
    nc = k.nc
    g.ident_f = k.sb("ident_f", [128, 128], F32); g.r_ident_f = Res("ident_f")
    k.op("pool", lambda e: e.memset(g.ident_f[:], 0.0), writes=[g.r_ident_f])
    k.op("pool", lambda e: e.affine_select(out=g.ident_f[:], in_=g.ident_f[:], pattern=[[-1, 128]],
                                           compare_op=ALU.not_equal, fill=1.0, base=0, channel_multiplier=1),
         reads=[g.r_ident_f], writes=[g.r_ident_f])
    g.ident_b = k.sb("ident_b", [128, 128], BF16); g.r_ident_b = Res("ident_b")
    k.op("dve", lambda e: e.tensor_copy(out=g.ident_b[:], in_=g.ident_f[:]), reads=[g.r_ident_f], writes=[g.r_ident_b])
    g.ones_f = k.sb("ones_f", [128, 128], F32); g.r_ones_f = Res("ones_f")
    k.op("pool", lambda e: e.memset(g.ones_f[:], 1.0), writes=[g.r_ones_f])
    g.ones_b = k.sb("ones_b", [128, 128], BF16); g.r_ones_b = Res("ones_b")
    k.op("pool", lambda e: e.memset(g.ones_b[:], 1.0), writes=[g.r_ones_b])

    def tri(name, pattern, cm, cmp):
        t = k.sb(name, [128, 128], F32); r = Res(name)
        k.op("pool", lambda e: e.memset(t[:], 1.0), writes=[r])
        k.op("pool", lambda e: e.affine_select(out=t[:], in_=t[:], pattern=pattern, compare_op=cmp, fill=0.0,
                                               base=0, channel_multiplier=cm), reads=[r], writes=[r])
        return t, r
    g.tri_le, g.r_tri_le = tri("tri_le", [[1, 128]], -1, ALU.is_ge)
    g.tri_lt, g.r_tri_lt = tri("tri_lt", [[1, 128]], -1, ALU.is_gt)
    g.tri_ge, g.r_tri_ge = tri("tri_ge", [[-1, 128]], 1, ALU.is_ge)
    g.tri_gt, g.r_tri_gt = tri("tri_gt", [[-1, 128]], 1, ALU.is_gt)
    g.dmat = k.sb("dmat", [128, 128], F32); g.r_dmat = Res("dmat")
    k.op("pool", lambda e: e.iota(g.dmat[:], pattern=[[1, 128]], base=0, channel_multiplier=-1,
                                  allow_small_or_imprecise_dtypes=True), writes=[g.r_dmat])
    g.iota_c = k.sb("iota_c", [128, 512], F32); g.r_iota_c = Res("iota_c")
    k.op("pool", lambda e: e.iota(g.iota_c[:], pattern=[[1, 512]], base=0, channel_multiplier=0,
                                  allow_small_or_imprecise_dtypes=True), writes=[g.r_iota_c])
    g.iota_c16 = k.sb("iota_c16", [128, 512], mybir.dt.int16); g.r_iota_c16 = Res("iota_c16")
    k.op("pool", lambda e: e.iota(g.iota_c16[:], pattern=[[1, 512]], base=0, channel_multiplier=0), writes=[g.r_iota_c16])
    g.pidx = k.sb("pidx", [128, 1], F32); g.r_pidx = Res("pidx")
    k.op("pool", lambda e: e.iota(g.pidx[:], pattern=[[0, 1]], base=0, channel_multiplier=1,
                                  allow_small_or_imprecise_dtypes=True), writes=[g.r_pidx])
    g.eps_c = k.sb("eps_c", [128, 1], F32); g.r_eps = Res("eps_c")
    k.op("pool", lambda e: e.memset(g.eps_c[:], EPS), writes=[g.r_eps])
    g.one_c = k.sb("one_c", [128, 1], F32); g.r_one = Res("one_c")
    k.op("pool", lambda e: e.memset(g.one_c[:], 1.0), writes=[g.r_one])


def rmsnorm_tile(k, g, xt, rx, wb, rwb, out_bf, rout, tagi):
    sq = g.nrm_sq; ss = g.nrm_ss[tagi % 2]; rss = g.r_nrm_ss[tagi % 2]
    k.op("act", lambda e: e.activation(out=sq[:], in_=xt, func=AF.Square, accum_out=ss[:, 0:1]),
         reads=[rx], writes=[g.r_nrm_sq, rss])
    k.op("act", lambda e: e.activation(out=ss[:, 1:2], in_=ss[:, 0:1], func=AF.Sqrt, scale=1.0 / D, bias=g.eps_c[:, 0:1]),
         reads=[rss, g.r_eps], writes=[rss])
    k.op("dve", lambda e: e.reciprocal(out=ss[:, 2:3], in_=ss[:, 1:2]), reads=[rss], writes=[rss])
    k.op("dve", lambda e: e.scalar_tensor_tensor(out=out_bf, in0=xt, scalar=ss[:, 2:3], in1=wb,
                                                 op0=ALU.mult, op1=ALU.mult), reads=[rx, rss, rwb], writes=[rout])


def bcast_rows(ap_row, nparts):
    if len(ap_row.shape) == 1:
        ap_row = ap_row.unsqueeze(0)
    return ap_row.to_broadcast([nparts] + list(ap_row.shape[1:]))


TM_CHUNKS = [(0, 512), (512, 512), (1024, 512), (1536, 512), (2560, 512), (3072, 272)]


def phase_inproj(k, g, l):
    k.scope_begin()
    P = g.P
    W = k.sb("w_in_bf", [128, 8, IN_COLS], BF16)
    rW = [Res("w_in%d" % i) for i in range(8)]
    src = P["w_in"][l].rearrange("(kc p) c -> p kc c", p=128)
    for kc in range(8):
        k.dma("pool", W[:, kc, :], src[:, kc, :], writes=[rW[kc]], max_dma_last_dim=4096)
    wb = k.sb("nw_b", [128, D], F32); rwb = Res("nw_b")
    k.dma("sp", wb[:], bcast_rows(P["norm_mix"][l], 128), writes=[rwb])
    g.nrm_sq = k.sb("nrm_sq", [128, D], F32); g.r_nrm_sq = Res("nrm_sq")
    g.nrm_ss = [k.sb("nrm_ss%d" % i, [128, 4], F32) for i in range(2)]
    g.r_nrm_ss = [Res("nrm_ss%d" % i) for i in range(2)]
    xt = [k.sb("xt%d" % i, [128, D], F32) for i in range(2)]; rxt = [Res("xt%d" % i) for i in range(2)]
    xn = [k.sb("xn%d" % i, [128, D], BF16) for i in range(4)]; rxn = [Res("xn%d" % i) for i in range(4)]
    hT = [k.sb("hT%d" % i, [128, 8, 512], BF16) for i in range(2)]
    rhT = [[Res("hT%d_%d" % (i, t)) for t in range(4)] for i in range(2)]
    ptr = [k.ps("ptr%d" % i, [128, 8, 128], BF16) for i in range(2)]; rptr = [Res("ptr%d" % i) for i in range(2)]
    pj = [k.ps("pj%d" % i, [128, 512], F32) for i in range(4)]; rpj = [Res("pj%d" % i) for i in range(4)]
    stage = [k.sb("stage%d" % i, [128, IN_COLS], F32) for i in range(2)]; rstage = [Res("stage%d" % i) for i in range(2)]
    stT = [k.sb("stT%d" % i, [128, 512], F32) for i in range(2)]; rstT = [Res("stT%d" % i) for i in range(2)]
    cntr = {'npj': 0, 'nst': 0}

    def prep_a(gi):
        for ti in range(4):
            j = gi * 4 + ti
            s = j % 2
            k.dma("sp", xt[s][:], g.x_src[j * 128:(j + 1) * 128, :], reads=[g.r_x], writes=[rxt[s]])
            rmsnorm_tile(k, g, xt[s][:], rxt[s], wb[:], rwb, xn[ti][:], rxn[ti], j)

    def prep_b(gi):
        hs = gi % 2
        for ti in range(4):
            j = gi * 4 + ti
            s = j % 2
            for kc in range(8):
                k.op("pe", lambda e: e.transpose(out=ptr[s][:, kc, :], in_=xn[ti][:, kc * 128:(kc + 1) * 128],
                                                 identity=g.ident_b[:]),
                     reads=[rxn[ti], g.r_ident_b], writes=[rptr[s]], inc=(kc == 7))
            k.op("act", lambda e: e.copy(out=hT[hs][:, :, ti * 128:(ti + 1) * 128], in_=ptr[s][:]),
                 reads=[rptr[s]], writes=[rhT[hs][ti]])

    def mm(gi):
        hs = gi % 2
        for ti in range(4):
            j = gi * 4 + ti
            ss = j % 2
            for ci, (c0, cw) in enumerate(TM_CHUNKS):
                pi = cntr['npj'] % 4; cntr['npj'] += 1
                for kc in range(8):
                    k.op("pe", lambda e: e.matmul(pj[pi][:, 0:cw], lhsT=hT[hs][:, kc, ti * 128:(ti + 1) * 128],
                                                  rhs=W[:, kc, c0:c0 + cw], start=(kc == 0), stop=(kc == 7)),
                         reads=[rhT[hs][ti], rW[kc]], writes=[rpj[pi]], inc=(kc == 7))
                ev = "act" if (ci % 2 == 0) else "dve"
                if ev == "act":
                    k.op("act", lambda e: e.copy(out=stage[ss][:, c0:c0 + cw], in_=pj[pi][:, 0:cw]),
                         reads=[rpj[pi]], writes=[rstage[ss]])
                else:
                    k.op("dve", lambda e: e.tensor_copy(out=stage[ss][:, c0:c0 + cw], in_=pj[pi][:, 0:cw]),
                         reads=[rpj[pi]], writes=[rstage[ss]])
            k.dma("sp", g.proj[j * 128:(j + 1) * 128, 0:2048], stage[ss][:, 0:2048], reads=[rstage[ss]], writes=[g.r_proj])
            k.dma("sp", g.proj[j * 128:(j + 1) * 128, 2560:IN_COLS], stage[ss][:, 2560:IN_COLS], reads=[rstage[ss]], writes=[g.r_proj2])
        for fc in range(4):
            pi = cntr['npj'] % 4; cntr['npj'] += 1
            for kc in range(8):
                k.op("pe", lambda e: e.matmul(pj[pi][:, :], lhsT=W[:, kc, C_LX + fc * 128:C_LX + (fc + 1) * 128],
                                              rhs=hT[hs][:, kc, :], start=(kc == 0), stop=(kc == 7)),
                     reads=rhT[hs] + [rW[kc]], writes=[rpj[pi]], inc=(kc == 7))
            s2 = cntr['nst'] % 2; cntr['nst'] += 1
            k.op("dve", lambda e: e.tensor_copy(out=stT[s2][:], in_=pj[pi][:]), reads=[rpj[pi]], writes=[rstT[s2]])
            k.dma("sp", g.lruT[fc * 128:(fc + 1) * 128, gi * 512:(gi + 1) * 512], stT[s2][:], reads=[rstT[s2]], writes=[g.r_lruT])
    prep_a(0)
    prep_b(0)
    for gi in range(NT // 4):
        if gi + 1 < NT // 4:
            prep_a(gi + 1)
        mm(gi)
        if gi + 1 < NT // 4:
            prep_b(gi + 1)
    k.scope_end()


def build_program(debug=False, stop_after=None, which=("lru", "ret", "gla", "rwkv")):
    needed = None
    for _pass in range(2):
        nc, g, k = _build_once(debug, stop_after, which, needed)
        needed = k.used
    g.nincs = getattr(k, "nincs", 0)
    return nc, g


def _build_once(debug, stop_after, which, needed):
    nc = bass.Bass("TRN2", target_bir_lowering=False)
    k = KB(nc, needed)
    g = Ctx()
    g.debug = debug
    g.which = which
    g.x_in = nc.dram_tensor("x", [T, D], F32, kind="ExternalInput").ap()
    g.pos_in = nc.dram_tensor("positions", [T], I32, kind="ExternalInput").ap()
    g.P = {}
    for name, shp in PARAM_SHAPES.items():
        g.P[name] = nc.dram_tensor(name, list(shp), F32, kind="ExternalInput").ap()
    g.out = nc.dram_tensor("out", [T, D], F32, kind="ExternalOutput").ap()
    sk = "ExternalOutput" if debug else "Internal"
    g.x_cur = nc.dram_tensor("x_cur", [T, D], F32, kind=sk).ap(); g.r_x = Res("x_cur")
    g.proj = nc.dram_tensor("proj", [T, IN_COLS], F32, kind=sk).ap(); g.r_proj = Res("proj"); g.r_proj2 = Res("proj2")
    g.lruT = nc.dram_tensor("lruT", [512, T], F32, kind=sk).ap(); g.r_lruT = Res("lruT")
    g.mixedT = nc.dram_tensor("mixedT", [D, T], BF16, kind=sk).ap()
    g.r_mixedT = [Res("mixedT%d" % i) for i in range(4)]
    g.xn2 = nc.dram_tensor("xn2", [T, D], BF16, kind=sk).ap(); g.r_xn2 = Res("xn2")

    make_consts(k, g)
    g.aff_all = k.sb("aff_all", [128, NT, NE], F32); g.r_aff = Res("aff_all")
    g.r_out = Res("out")
    g.x_src = g.x_in
    for l in range(DEPTH):
        phase_inproj(k, g, l)
        if stop_after == ("inproj", l):
            break
        if "lru" in g.which: phase_lru(k, g, l)
        if "ret" in g.which: phase_ret(k, g, l)
        if "gla" in g.which: phase_gla(k, g, l)
        if "rwkv" in g.which: phase_rwkv(k, g, l)
        if stop_after == ("mix", l):
            break
        phase_outproj_router(k, g, l)
        if stop_after == ("outproj", l):
            break
        phase_moe(k, g, l)
        if stop_after == ("moe", l):
            break
    if stop_after is None:
        phase_final(k, g)
    k.barrier()
    k.finish([])
    k.close()
    g.ninst = k.ninst
    return nc, g, k


def make_in_maps(inputs, cores):
    maps = []
    for b in cores:
        m = {"x": np.ascontiguousarray(inputs["x"][b]), "positions": np.ascontiguousarray(inputs["positions"][b]).astype(np.int32)}
        for name in PARAM_SHAPES:
            m[name] = np.ascontiguousarray(inputs[name])
        maps.append(m)
    return maps


def kernel(**inputs):
    nc, g = build_program()
    in_maps = make_in_maps(inputs, list(range(8)))
    res = run_bass_kernel_spmd(nc, in_maps, core_ids=list(range(8)))
    out = np.stack([np.asarray(r["out"]) for r in res.results], axis=0)
    return out.astype(np.float32)


import math
PI = math.pi


def make_rope(k, g):
    k.scope_begin()
    posi = k.sb("posi", [128, 32], I32); rposi = Res("posi")
    k.dma("sp", posi[:], g.pos_in.rearrange("(j p) -> p j", p=128), writes=[rposi], allow_slow_non_contiguous=True)
    posf = k.sb("posf", [128, 32], F32); rposf = Res("posf")
    k.op("dve", lambda e: e.tensor_copy(out=posf[:], in_=posi[:]), reads=[rposi], writes=[rposf])
    fi = k.sb("fi", [128, 32], F32); rfi = Res("fi")
    k.op("pool", lambda e: e.iota(fi[:], pattern=[[1, 32]], base=0, channel_multiplier=0,
                                  allow_small_or_imprecise_dtypes=True), writes=[rfi])
    inv = k.sb("inv", [128, 32], F32); rinv = Res("inv")
    k.op("act", lambda e: e.activation(out=inv[:], in_=fi[:], func=AF.Exp, scale=-math.log(10000.0) / 32.0),
         reads=[rfi], writes=[rinv])
    ang = k.sb("ang", [128, 32, 32], F32); rang = Res("ang")
    k.op("dve", lambda e: e.tensor_tensor(out=ang[:], in0=posf[:].unsqueeze(2).to_broadcast([128, 32, 32]),
                                          in1=inv[:].unsqueeze(1).to_broadcast([128, 32, 32]), op=ALU.mult),
         reads=[rposf, rinv], writes=[rang])
    ni = k.sb("rp_ni", [128, 1024], I32); rni = Res("rp_ni")
    nf = k.sb("rp_nf", [128, 1024], F32); rnf = Res("rp_nf")
    y = k.sb("rp_y", [128, 1024], F32); ry = Res("rp_y")
    m = k.sb("rp_m", [128, 1024], F32); rm = Res("rp_m")
    a2 = k.sb("rp_a2", [128, 1024], F32); ra2 = Res("rp_a2")
    C1 = 6.28125
    C2 = 2.0 * PI - C1
    angf = ang[:].rearrange("p a b -> p (a b)")

    def reduce_sin(src, rsrc, dst, rdst, scale):
        k.op("dve", lambda e: e.tensor_scalar(out=ni[:], in0=src, scalar1=1.0 / (2.0 * PI), scalar2=None, op0=ALU.mult),
             reads=[rsrc], writes=[rni])
        k.op("dve", lambda e: e.tensor_copy(out=nf[:], in_=ni[:]), reads=[rni], writes=[rnf])
        k.op("dve", lambda e: e.scalar_tensor_tensor(out=y[:], in0=nf[:], scalar=-C1, in1=src, op0=ALU.mult, op1=ALU.add),
             reads=[rnf, rsrc], writes=[ry])
        k.op("dve", lambda e: e.scalar_tensor_tensor(out=y[:], in0=nf[:], scalar=-C2, in1=y[:], op0=ALU.mult, op1=ALU.add),
             reads=[rnf, ry], writes=[ry])
        k.op("dve", lambda e: e.tensor_scalar(out=m[:], in0=y[:], scalar1=PI, scalar2=-2.0 * PI, op0=ALU.is_gt, op1=ALU.mult),
             reads=[ry], writes=[rm])
        k.op("dve", lambda e: e.tensor_tensor(out=y[:], in0=y[:], in1=m[:], op=ALU.add), reads=[ry, rm], writes=[ry])
        k.op("dve", lambda e: e.tensor_scalar(out=m[:], in0=y[:], scalar1=-PI, scalar2=2.0 * PI, op0=ALU.is_lt, op1=ALU.mult),
             reads=[ry], writes=[rm])
        k.op("dve", lambda e: e.tensor_tensor(out=y[:], in0=y[:], in1=m[:], op=ALU.add), reads=[ry, rm], writes=[ry])
        k.op("dve", lambda e: e.tensor_scalar(out=y[:], in0=y[:], scalar1=-3.1415925, scalar2=3.1415925, op0=ALU.max, op1=ALU.min),
             reads=[ry], writes=[ry])
        k.op("act", lambda e: e.activation(out=dst, in_=y[:], func=AF.Sin), reads=[ry], writes=[rdst])

    reduce_sin(angf, rang, g.sinq[:].rearrange("p a b -> p (a b)"), g.r_rope, 1.0)
    k.op("dve", lambda e: e.tensor_scalar(out=a2[:], in0=angf, scalar1=PI / 2.0, scalar2=None, op0=ALU.add),
         reads=[rang], writes=[ra2])
    reduce_sin(a2[:], ra2, g.cosq[:].rearrange("p a b -> p (a b)"), g.r_rope, 1.0)
    k.op("dve", lambda e: e.tensor_scalar(out=g.sink[:], in0=g.sinq[:], scalar1=0.125, scalar2=None, op0=ALU.mult),
         reads=[g.r_rope], writes=[g.r_rope])
    k.op("dve", lambda e: e.tensor_scalar(out=g.cosk[:], in0=g.cosq[:], scalar1=0.125, scalar2=None, op0=ALU.mult),
         reads=[g.r_rope], writes=[g.r_rope])
    k.scope_end()


def phase_lru(k, g, l):
    k.scope_begin()
    P = g.P
    NB = 7
    buf = [k.sb("lb%d" % i, [128, T], F32) for i in range(NB)]
    rb = [Res("lb%d" % i) for i in range(NB)]
    X, XC, G0, G1, TMP, H0, H1 = range(7)
    xcb = k.sb("l_xcb", [128, T], BF16); rxcb = Res("l_xcb")
    oT = k.sb("l_oT", [128, T], BF16); roT = Res("l_oT")
    cw = k.sb("l_cw", [128, 4], F32); rcw = Res("l_cw")
    cb = k.sb("l_cb", [128, 1], F32); rcb = Res("l_cb")
    gb = k.sb("l_gb", [128, 4], F32); rgb = Res("l_gb")
    lam = k.sb("l_lam", [128, 2], F32); rlam = Res("l_lam")
    c1 = k.sb("l_c1", [128, 2], F32); rc1 = Res("l_c1")
    wst = k.sb("l_wst", [128, 4, 128], F32); rwst = Res("l_wst")
    wbd = k.sb("l_wbd", [128, 4, 128], BF16); rwbd = Res("l_wbd")
    pg = [k.ps("l_pg%d" % i, [128, 512], F32) for i in range(4)]; rpg = [Res("l_pg%d" % i) for i in range(4)]
    npg = 0
    for pt in range(2):
        ch0 = pt * 128
        k.dma("sp", cw[:], P["lru_conv_w"][l][:, ch0:ch0 + 128].rearrange("j c -> c j"), writes=[rcw], allow_slow_non_contiguous=True)
        k.dma("sp", cb[:], P["lru_conv_b"][l][ch0:ch0 + 128].unsqueeze(1), writes=[rcb], allow_slow_non_contiguous=True)
        k.dma("sp", gb[:], P["lru_gate_b"][l][:, :, ch0:ch0 + 128].rearrange("a b c -> c (a b)"), writes=[rgb], allow_slow_non_contiguous=True)
        k.dma("sp", lam[:], P["lru_lambda"][l][:, ch0:ch0 + 128].rearrange("a c -> c a"), writes=[rlam], allow_slow_non_contiguous=True)
        k.op("pool", lambda e: e.memset(wst[:], 0.0), writes=[rwst])
        for dr in range(2):
            for gt in range(2):
                for hh in range(2):
                    k.dma("sp", wst[hh * 64:(hh + 1) * 64, dr * 2 + gt, hh * 64:(hh + 1) * 64],
                          P["lru_gate_w"][l, dr, gt, 2 * pt + hh], writes=[rwst])
        k.op("dve", lambda e: e.tensor_copy(out=wbd[:], in_=wst[:]), reads=[rwst], writes=[rwbd])
        k.op("act", lambda e: e.activation(out=c1[:], in_=lam[:], func=AF.Exp, scale=-1.0), reads=[rlam], writes=[rc1])
        k.op("act", lambda e: e.activation(out=c1[:], in_=c1[:], func=AF.Ln, bias=g.one_c[:, 0:1]), reads=[rc1, g.r_one], writes=[rc1])
        k.op("dve", lambda e: e.tensor_scalar(out=c1[:], in0=c1[:], scalar1=-8.0, scalar2=None, op0=ALU.mult), reads=[rc1], writes=[rc1])
        k.dma("sp", buf[X][:], g.lruT[ch0:ch0 + 128, :], reads=[g.r_lruT], writes=[rb[X]])
        k.op("dve", lambda e: e.tensor_scalar(out=buf[XC][:], in0=buf[X][:], scalar1=cw[:, 2:3], scalar2=cb[:, 0:1],
                                              op0=ALU.mult, op1=ALU.add), reads=[rb[X], rcw, rcb], writes=[rb[XC]])
        for (j, so, do, n) in [(0, 0, 2, T - 2), (1, 0, 1, T - 1), (3, 1, 0, T - 1)]:
            k.op("dve", lambda e: e.scalar_tensor_tensor(out=buf[XC][:, do:do + n], in0=buf[X][:, so:so + n],
                                                         scalar=cw[:, j:j + 1], in1=buf[XC][:, do:do + n],
                                                         op0=ALU.mult, op1=ALU.add), reads=[rb[X], rb[XC], rcw], writes=[rb[XC]])
        k.op("act", lambda e: e.copy(out=xcb[:], in_=buf[XC][:]), reads=[rb[XC]], writes=[rxcb])
        k.dma("sp", buf[X][:], g.lruT[256 + ch0:256 + ch0 + 128, :], reads=[g.r_lruT], writes=[rb[X]])
        for dr in range(2):
            for gt in range(2):
                dst = G0 if gt == 0 else G1
                for tc in range(8):
                    pi = npg % 4; npg += 1
                    k.op("pe", lambda e: e.matmul(pg[pi][:], lhsT=wbd[:, dr * 2 + gt, :], rhs=xcb[:, tc * 512:(tc + 1) * 512],
                                                  start=True, stop=True), reads=[rwbd, rxcb], writes=[rpg[pi]])
                    k.op("act", lambda e: e.activation(out=buf[dst][:, tc * 512:(tc + 1) * 512], in_=pg[pi][:], func=AF.Sigmoid,
                                                       bias=gb[:, dr * 2 + gt:dr * 2 + gt + 1]), reads=[rpg[pi], rgb], writes=[rb[dst]])
            k.op("act", lambda e: e.activation(out=buf[G0][:], in_=buf[G0][:], func=AF.Exp, scale=c1[:, dr:dr + 1]),
                 reads=[rb[G0], rc1], writes=[rb[G0]])
            k.op("act", lambda e: e.activation(out=buf[TMP][:], in_=buf[G0][:], func=AF.Square),
                 reads=[rb[G0]], writes=[rb[TMP]])
            k.op("act", lambda e: e.activation(out=buf[TMP][:], in_=buf[TMP][:], func=AF.Sqrt, scale=-1.0, bias=g.one_c[:, 0:1]),
                 reads=[rb[TMP], g.r_one], writes=[rb[TMP]])
            k.op("dve", lambda e: e.tensor_tensor(out=buf[G1][:], in0=buf[G1][:], in1=buf[TMP][:], op=ALU.mult),
                 reads=[rb[G1], rb[TMP]], writes=[rb[G1]])
            k.op("dve", lambda e: e.tensor_tensor(out=buf[G1][:], in0=buf[G1][:], in1=buf[XC][:], op=ALU.mult),
                 reads=[rb[G1], rb[XC]], writes=[rb[G1]])
            if dr == 0:
                k.op("dve", lambda e: e.tensor_tensor_scan(out=buf[H0][:], data0=buf[G0][:], data1=buf[G1][:], initial=0.0,
                                                           op0=ALU.mult, op1=ALU.add), reads=[rb[G0], rb[G1]], writes=[rb[H0]])
            else:
                k.op("dve", lambda e: e.tensor_tensor_scan(out=buf[H1][:, ::-1], data0=buf[G0][:, ::-1], data1=buf[G1][:, ::-1],
                                                           initial=0.0, op0=ALU.mult, op1=ALU.add),
                     reads=[rb[G0], rb[G1]], writes=[rb[H1]])
        k.op("pool", lambda e: e.tensor_tensor(out=buf[H0][:, 0:1024], in0=buf[H0][:, 0:1024], in1=buf[H1][:, 0:1024], op=ALU.add),
             reads=[rb[H0], rb[H1]], writes=[rb[H0]])
        k.op("dve", lambda e: e.tensor_tensor(out=buf[H0][:, 1024:T], in0=buf[H0][:, 1024:T], in1=buf[H1][:, 1024:T], op=ALU.add),
             reads=[rb[H0], rb[H1]], writes=[rb[H0]])
        k.op("act", lambda e: e.activation(out=buf[X][:], in_=buf[X][:], func=AF.Gelu), reads=[rb[X]], writes=[rb[X]])
        k.op("dve", lambda e: e.tensor_tensor(out=oT[:], in0=buf[H0][:], in1=buf[X][:], op=ALU.mult),
             reads=[rb[H0], rb[X]], writes=[roT])
        k.dma("sp", g.mixedT[512 + ch0:512 + ch0 + 128, :], oT[:], reads=[roT], writes=[g.r_mixedT[2]])
    k.scope_end()


def head_norm_finalize(k, g, y, ry, nheads, center, eps, gnw, rgnw, tmp, rtmp, st, rst):
    hd = 256 // nheads
    yv = y.rearrange("p (h e) -> p h e", h=nheads)
    tv = tmp.rearrange("p (h e) -> p h e", h=nheads)
    if center:
        k.op("dve", lambda e: e.tensor_reduce(out=st[:, 0:nheads], in_=yv, axis=AX.X, op=ALU.add), reads=[ry], writes=[rst])
        k.op("dve", lambda e: e.scalar_tensor_tensor(out=yv, in0=st[:, 0:nheads].unsqueeze(2).to_broadcast([128, nheads, hd]),
                                                     scalar=-1.0 / hd, in1=yv, op0=ALU.mult, op1=ALU.add),
             reads=[rst, ry], writes=[ry])
    k.op("dve", lambda e: e.tensor_tensor(out=tv, in0=yv, in1=yv, op=ALU.mult), reads=[ry], writes=[rtmp])
    k.op("dve", lambda e: e.tensor_reduce(out=st[:, 4:4 + nheads], in_=tv, axis=AX.X, op=ALU.add), reads=[rtmp], writes=[rst])
    k.op("act", lambda e: e.activation(out=st[:, 8:8 + nheads], in_=st[:, 4:4 + nheads], func=AF.Sqrt, scale=1.0 / hd,
                                       bias=eps[:, 0:1]), reads=[rst, g.r_eps], writes=[rst])
    k.op("dve", lambda e: e.reciprocal(out=st[:, 12:12 + nheads], in_=st[:, 8:8 + nheads]), reads=[rst], writes=[rst])
    k.op("dve", lambda e: e.tensor_tensor(out=yv, in0=yv, in1=st[:, 12:12 + nheads].unsqueeze(2).to_broadcast([128, nheads, hd]),
                                          op=ALU.mult), reads=[ry, rst], writes=[ry])
    k.op("dve", lambda e: e.tensor_tensor(out=y, in0=y, in1=gnw, op=ALU.mult), reads=[ry, rgnw], writes=[ry])


def phase_ret(k, g, l):
    k.scope_begin()
    P = g.P
    g.sinq = k.sb("sinq", [128, 32, 32], F32); g.cosq = k.sb("cosq", [128, 32, 32], F32)
    g.sink = k.sb("sink", [128, 32, 32], F32); g.cosk = k.sb("cosk", [128, 32, 32], F32)
    g.r_rope = Res("rope")
    make_rope(k, g)
    lg_b = k.sb("r_lgb", [128, 2, 4], F32); rlg = Res("r_lgb")
    k.dma("sp", lg_b[:], bcast_rows(P["ret_log_decay"][l].rearrange("a h -> (a h)"), 128).rearrange("p (a h) -> p a h", a=2),
          writes=[rlg])
    nlgb = k.sb("r_nlgb", [128, 4], F32); rnlgb = Res("r_nlgb")
    k.op("dve", lambda e: e.tensor_scalar(out=nlgb[:], in0=lg_b[:, 1, :], scalar1=-1.0, scalar2=None, op0=ALU.mult),
         reads=[rlg], writes=[rnlgb])
    M4 = k.sb("r_M", [128, 2, 2, 128], F32); rM = Res("r_M")
    tmpm = k.sb("r_tmpm", [128, 128], F32); rtm = Res("r_tmpm")
    for h in range(4):
        M = M4[:, h % 2]
        h_, h = h, h // 2
        k.op("act", lambda e: e.activation(out=M[:, h, :], in_=g.dmat[:], func=AF.Exp, scale=lg_b[:, 0, h_:h_ + 1]),
             reads=[g.r_dmat, rlg], writes=[rM])
        k.op("dve", lambda e: e.tensor_tensor(out=M[:, h, :], in0=M[:, h, :], in1=g.tri_le[:], op=ALU.mult),
             reads=[rM, g.r_tri_le], writes=[rM])
        k.op("act", lambda e: e.activation(out=tmpm[:], in_=g.dmat[:], func=AF.Exp, scale=nlgb[:, h_:h_ + 1]),
             reads=[g.r_dmat, rnlgb], writes=[rtm])
        k.op("dve", lambda e: e.tensor_tensor(out=tmpm[:], in0=tmpm[:], in1=g.tri_ge[:], op=ALU.mult),
             reads=[rtm, g.r_tri_ge], writes=[rtm])
        k.op("dve", lambda e: e.tensor_tensor(out=M[:, h, :], in0=M[:, h, :], in1=tmpm[:], op=ALU.add),
             reads=[rM, rtm], writes=[rM])
    lgP = k.sb("r_lgP", [128, 2, 2], F32); rlgP = Res("r_lgP")
    for dr in range(2):
        for hh in range(2):
            k.dma("sp", lgP[hh * 64:(hh + 1) * 64, dr, :], bcast_rows(P["ret_log_decay"][l, dr, hh::2], 64),
                  writes=[rlgP], allow_slow_non_contiguous=True)
    i1 = k.sb("r_i1", [128, 2, 128], F32); ri1 = Res("r_i1")
    k.op("pool", lambda e: e.iota(i1[:, 0, :], pattern=[[1, 128]], base=1, channel_multiplier=0,
                                  allow_small_or_imprecise_dtypes=True), writes=[ri1])
    k.op("pool", lambda e: e.iota(i1[:, 1, :], pattern=[[-1, 128]], base=128, channel_multiplier=0,
                                  allow_small_or_imprecise_dtypes=True), writes=[ri1])
    XI = k.sb("r_XI", [128, 2, 2, 128], F32); rXI = Res("r_XI")
    decP = k.sb("r_decP", [128, 2, 2], F32); rdecP = Res("r_decP")
    for dr in range(2):
        for hp in range(2):
            k.op("act", lambda e: e.activation(out=XI[:, dr, hp, :], in_=i1[:, dr, :], func=AF.Exp, scale=lgP[:, dr, hp:hp + 1]),
                 reads=[ri1, rlgP], writes=[rXI])
    k.op("act", lambda e: e.activation(out=decP[:], in_=lgP[:], func=AF.Exp, scale=128.0), reads=[rlgP], writes=[rdecP])
    jr = k.sb("r_jr", [128, 2], F32); rjr = Res("r_jr")
    k.op("dve", lambda e: e.tensor_scalar(out=jr[:, 0:1], in0=g.pidx[:], scalar1=-1.0, scalar2=127.0, op0=ALU.mult, op1=ALU.add),
         reads=[g.r_pidx], writes=[rjr])
    k.op("dve", lambda e: e.tensor_copy(out=jr[:, 1:2], in_=g.pidx[:]), reads=[g.r_pidx], writes=[rjr])
    ZT = k.sb("r_ZT", [128, 2, 4], F32); rZT = Res("r_ZT")
    for dr in range(2):
        k.op("dve", lambda e: e.tensor_scalar(out=ZT[:, dr, :], in0=lg_b[:, dr, :], scalar1=jr[:, dr:dr + 1], scalar2=None,
                                              op0=ALU.mult), reads=[rlg, rjr], writes=[rZT])
    k.op("act", lambda e: e.activation(out=ZT[:], in_=ZT[:], func=AF.Exp), reads=[rZT], writes=[rZT])
    BD = k.sb("r_BD", [128, 128], F32); rBD = Res("r_BD")
    k.op("pool", lambda e: e.memset(BD[:], 0.0), writes=[rBD])
    k.op("pool", lambda e: e.memset(BD[0:64, 0:64], 1.0), writes=[rBD])
    k.op("pool", lambda e: e.memset(BD[64:128, 64:128], 1.0), writes=[rBD])
    gnw = k.sb("r_gnw", [128, 256], F32); rgnw = Res("r_gnw")
    k.dma("sp", gnw[:], bcast_rows(P["ret_gn"][l], 128), writes=[rgnw])
    qT = k.sb("r_qT", [128, 2, T], BF16); rqT = [Res("r_qT%d" % c) for c in range(NT)]
    kT = k.sb("r_kT", [128, 2, T], BF16); rkT = [Res("r_kT%d" % c) for c in range(NT)]
    k_all = k.sb("r_k", [128, NT, 256], BF16); rk_all = [Res("r_k%d" % c) for c in range(NT)]
    v_all = k.sb("r_v", [128, NT, 256], BF16); rv_all = [Res("r_v%d" % c) for c in range(NT)]
    y_all = k.sb("r_y", [128, NT, 256], F32); ry_all = [Res("r_y%d" % c) for c in range(NT)]
    oT = k.sb("r_oT", [128, 2, T], BF16); roT = Res("r_oT")
    gs_all = k.sb("r_gs", [128, NT, 256], BF16); rgs_all = [Res("r_gs%d" % c) for c in range(NT)]
    gld = [k.sb("r_gld%d" % i, [128, 256], F32) for i in range(2)]; rgld = [Res("r_gld%d" % i) for i in range(2)]
    qkv = [k.sb("r_qkv%d" % i, [128, 768], F32) for i in range(2)]; rqkv = [Res("r_qkv%d" % i) for i in range(2)]
    qtm = [k.sb("r_qtm%d" % i, [128, 256], BF16) for i in range(2)]; rqtm = [Res("r_qtm%d" % i) for i in range(2)]
    tq = [k.sb("r_tq%d" % i, [128, 4, 4, 32], F32) for i in range(2)]; rtq = [Res("r_tq%d" % i) for i in range(2)]
    tk = [k.sb("r_tk%d" % i, [128, 4, 4, 32], F32) for i in range(2)]; rtk = [Res("r_tk%d" % i) for i in range(2)]
    ptr = [k.ps("r_ptr%d" % i, [128, 4, 128], BF16) for i in range(2)]; rptr = [Res("r_ptr%d" % i) for i in range(2)]

    def rotary(eng, src, cosT, sinT, c, tt, rtt, dst, rsrc, rdst):
        xv = src.rearrange("p (h two f) -> p h two f", h=4, two=2)
        dv = dst.rearrange("p (h two f) -> p h two f", h=4, two=2)
        cb = cosT[:, c, :].unsqueeze(1).to_broadcast([128, 4, 32])
        sbb = sinT[:, c, :].unsqueeze(1).to_broadcast([128, 4, 32])
        x1 = xv[:, :, 0, :]; x2 = xv[:, :, 1, :]
        k.op(eng, lambda e: e.tensor_tensor(out=tt[:, 0], in0=x1, in1=cb, op=ALU.mult), reads=[rsrc, g.r_rope], writes=[rtt])
        k.op(eng, lambda e: e.tensor_tensor(out=tt[:, 1], in0=x2, in1=sbb, op=ALU.mult), reads=[rsrc, g.r_rope], writes=[rtt])
        k.op(eng, lambda e: e.tensor_tensor(out=tt[:, 2], in0=x1, in1=sbb, op=ALU.mult), reads=[rsrc, g.r_rope], writes=[rtt])
        k.op(eng, lambda e: e.tensor_tensor(out=tt[:, 3], in0=x2, in1=cb, op=ALU.mult), reads=[rsrc, g.r_rope], writes=[rtt])
        k.op(eng, lambda e: e.tensor_tensor(out=dv[:, :, 0, :], in0=tt[:, 0], in1=tt[:, 1], op=ALU.subtract), reads=[rtt], writes=[rdst])
        k.op(eng, lambda e: e.tensor_tensor(out=dv[:, :, 1, :], in0=tt[:, 2], in1=tt[:, 3], op=ALU.add), reads=[rtt], writes=[rdst])

    for c in range(NT):
        s = c % 2
        k.dma("sp", qkv[s][:], g.proj[c * 128:(c + 1) * 128, 0:768], reads=[g.r_proj], writes=[rqkv[s]])
        rotary("dve", qkv[s][:, 0:256], g.cosq, g.sinq, c, tq[s], rtq[s], qtm[s][:], rqkv[s], rqtm[s])
        rotary("pool", qkv[s][:, 256:512], g.cosk, g.sink, c, tk[s], rtk[s], k_all[:, c, :], rqkv[s], rk_all[c])
        k.op("act", lambda e: e.copy(out=v_all[:, c, :], in_=qkv[s][:, 512:768]), reads=[rqkv[s]], writes=[rv_all[c]])
        k.dma("sp", gld[s][:], g.proj[c * 128:(c + 1) * 128, C_RG:C_RG + 256], reads=[g.r_proj], writes=[rgld[s]])
        k.op("act", lambda e: e.activation(out=gs_all[:, c, :], in_=gld[s][:], func=AF.Silu), reads=[rgld[s]], writes=[rgs_all[c]])
        for hp in range(2):
            k.op("pe", lambda e: e.transpose(out=ptr[s][:, hp, :], in_=qtm[s][:, hp * 128:(hp + 1) * 128], identity=g.ident_b[:]),
                 reads=[rqtm[s], g.r_ident_b], writes=[rptr[s]], inc=False)
        for hp in range(2):
            k.op("pe", lambda e: e.transpose(out=ptr[s][:, 2 + hp, :], in_=k_all[:, c, hp * 128:(hp + 1) * 128], identity=g.ident_b[:]),
                 reads=[rk_all[c], g.r_ident_b], writes=[rptr[s]], inc=(hp == 1))
        k.op("act", lambda e: e.copy(out=qT[:, :, c * 128:(c + 1) * 128], in_=ptr[s][:, 0:2, :]), reads=[rptr[s]], writes=[rqT[c]])
        k.op("act", lambda e: e.copy(out=kT[:, :, c * 128:(c + 1) * 128], in_=ptr[s][:, 2:4, :]), reads=[rptr[s]], writes=[rkT[c]])
    S32 = [k.sb("r_S32_%d" % d, [128, 2, 128], F32) for d in range(2)]; rS32 = [Res("r_S32_%d" % d) for d in range(2)]
    Sbf = [[k.sb("r_Sbf_%d_%d" % (d, q), [128, 2, 128], BF16) for q in range(2)] for d in range(2)]
    rSbf = [[Res("r_Sbf_%d_%d" % (d, q)) for q in range(2)] for d in range(2)]
    for d in range(2):
        k.op("pool", lambda e: e.memset(S32[d][:], 0.0), writes=[rS32[d]])
        for q in range(2):
            k.op("pool", lambda e: e.memset(Sbf[d][q][:], 0.0), writes=[rSbf[d][q]])
    psc1 = k.ps("r_psc", [128, 2, 512], F32); psc = [psc1, psc1]; rpsc1 = Res("r_psc"); rpsc = [rpsc1, rpsc1]
    py1 = k.ps("r_py", [128, 2, 512], F32); py = [py1, py1]; rpy1 = Res("r_py"); rpy = [rpy1, rpy1]
    pds = k.ps("r_pds", [128, 2, 128], F32); rpds = Res("r_pds")
    PT = [k.sb("r_PT%d" % i, [128, 2, 2, 128], BF16) for i in range(2)]; rPT = [Res("r_PT%d" % i) for i in range(2)]
    QX = [k.sb("r_QX%d" % i, [128, 2, 128], BF16) for i in range(2)]; rQX = [Res("r_QX%d" % i) for i in range(2)]
    KZ = [k.sb("r_KZ%d" % i, [128, 256], BF16) for i in range(2)]; rKZ = [Res("r_KZ%d" % i) for i in range(2)]
    dsm2 = [k.sb("r_dsm%d" % i, [128, 2, 128], F32) for i in range(2)]; rdsm2 = [Res("r_dsm%d" % i) for i in range(2)]

    def state_pre(d, c, s):
        dsm, rdsm = dsm2[s], rdsm2[s]
        k.op("pool", lambda e: e.tensor_tensor(out=KZ[s][:].rearrange("p (h e) -> p h e", h=4),
                                               in0=k_all[:, c, :].rearrange("p (h e) -> p h e", h=4),
                                               in1=ZT[:, d, :].unsqueeze(2).to_broadcast([128, 4, 64]), op=ALU.mult),
             reads=[rk_all[c], rZT], writes=[rKZ[s]])
        for hp in range(2):
            k.op("pe", lambda e: e.matmul(pds[:, hp, :], lhsT=KZ[s][:, hp * 128:(hp + 1) * 128], rhs=v_all[:, c, hp * 128:(hp + 1) * 128],
                                          start=True, stop=True), reads=[rKZ[s], rv_all[c]], writes=[rpds], inc=(hp == 1))
        k.op("dve", lambda e: e.tensor_tensor(out=dsm[:], in0=pds[:], in1=BD[:].unsqueeze(1).to_broadcast([128, 2, 128]), op=ALU.mult),
             reads=[rpds, rBD], writes=[rdsm])

    def state_post(d, c, s):
        dsm, rdsm = dsm2[s], rdsm2[s]
        for hp in range(2):
            k.op("dve", lambda e: e.scalar_tensor_tensor(out=S32[d][:, hp, :], in0=S32[d][:, hp, :], scalar=decP[:, d, hp:hp + 1],
                                                         in1=dsm[:, hp, :], op0=ALU.mult, op1=ALU.add),
                 reads=[rS32[d], rdecP, rdsm], writes=[rS32[d]])
        k.op("act", lambda e: e.copy(out=Sbf[d][c % 2][:], in_=S32[d][:]), reads=[rS32[d]], writes=[rSbf[d][c % 2]])

    for c in range(NT):
        s = c % 2
        cs = slice(c * 128, (c + 1) * 128)
        for h in range(4):
            hp, hh = h // 2, h % 2
            k.op("pe", lambda e: e.matmul(psc[s][:, hh, hp * 128:(hp + 1) * 128], lhsT=kT[hh * 64:(hh + 1) * 64, hp, cs], rhs=qT[hh * 64:(hh + 1) * 64, hp, cs],
                                          start=True, stop=True), reads=[rkT[c], rqT[c]], writes=[rpsc[s]], inc=(h == 3))
        k.op("dve", lambda e: e.tensor_tensor(out=PT[s][:], in0=psc[s][:, :, 0:256].rearrange("p a (b c) -> p a b c", b=2), in1=M4[:], op=ALU.mult), reads=[rpsc[s], rM], writes=[rPT[s]])
        k.op("pool", lambda e: e.tensor_tensor(out=QX[s][:], in0=qT[:, :, cs], in1=XI[:, 0], op=ALU.mult), reads=[rqT[c], rXI], writes=[rQX[s]])
        state_pre(0, c, s)
        for h in range(4):
            hp, hh = h // 2, h % 2
            k.op("pe", lambda e: e.matmul(py[s][:, hh, hp * 64:(hp + 1) * 64], lhsT=PT[s][:, hh, hp, :], rhs=v_all[:, c, h * 64:(h + 1) * 64],
                                          start=True, stop=False), reads=[rPT[s], rv_all[c]], writes=[rpy[s]], inc=False)
            k.op("pe", lambda e: e.matmul(py[s][:, hh, hp * 64:(hp + 1) * 64], lhsT=QX[s][hh * 64:(hh + 1) * 64, hp, :],
                                          rhs=Sbf[0][(c + 1) % 2][hh * 64:(hh + 1) * 64, hp, hh * 64:(hh + 1) * 64], start=False, stop=True),
                 reads=[rQX[s], rSbf[0][(c + 1) % 2]], writes=[rpy[s]], inc=(h == 3))
        k.op("act", lambda e: e.copy(out=y_all[:, c, :].rearrange("p (hp hh e) -> p hh hp e", hp=2, hh=2),
                                     in_=py[s][:, :, 0:128].rearrange("p hh (hp e) -> p hh hp e", hp=2)), reads=[rpy[s]], writes=[ry_all[c]])
        state_post(0, c, s)
    gt_ = [k.sb("r_g%d" % i, [128, 256], F32) for i in range(2)]; rgt = [Res("r_g%d" % i) for i in range(2)]
    tmp = [k.sb("r_tmp%d" % i, [128, 256], F32) for i in range(2)]; rtmp = [Res("r_tmp%d" % i) for i in range(2)]
    st = [k.sb("r_st%d" % i, [128, 16], F32) for i in range(2)]; rst = [Res("r_st%d" % i) for i in range(2)]
    ob = [k.sb("r_ob%d" % i, [128, 256], BF16) for i in range(2)]; rob = [Res("r_ob%d" % i) for i in range(2)]
    for c in range(NT - 1, -1, -1):
        s = c % 2
        cs = slice(c * 128, (c + 1) * 128)
        k.op("pool", lambda e: e.tensor_tensor(out=QX[s][:], in0=qT[:, :, cs], in1=XI[:, 1], op=ALU.mult), reads=[rqT[c], rXI], writes=[rQX[s]])
        state_pre(1, c, s)
        for h in range(4):
            hp, hh = h // 2, h % 2
            k.op("pe", lambda e: e.matmul(py[s][:, hh, hp * 64:(hp + 1) * 64], lhsT=QX[s][hh * 64:(hh + 1) * 64, hp, :],
                                          rhs=Sbf[1][(c + 1) % 2][hh * 64:(hh + 1) * 64, hp, hh * 64:(hh + 1) * 64], start=True, stop=True),
                 reads=[rQX[s], rSbf[1][(c + 1) % 2]], writes=[rpy[s]], inc=(h == 3))
        k.op("dve", lambda e: e.tensor_tensor(out=y_all[:, c, :].rearrange("p (hp hh e) -> p hh hp e", hp=2, hh=2),
                                              in0=y_all[:, c, :].rearrange("p (hp hh e) -> p hh hp e", hp=2, hh=2),
                                              in1=py[s][:, :, 0:128].rearrange("p hh (hp e) -> p hh hp e", hp=2), op=ALU.add),
             reads=[ry_all[c], rpy[s]], writes=[ry_all[c]])
        state_post(1, c, s)
        head_norm_finalize(k, g, y_all[:, c, :], ry_all[c], 4, True, g.eps_c, gnw[:], rgnw, tmp[s][:], rtmp[s], st[s], rst[s])
        k.op("pool", lambda e: e.tensor_tensor(out=ob[s][:], in0=y_all[:, c, :], in1=gs_all[:, c, :], op=ALU.mult),
             reads=[ry_all[c], rgs_all[c]], writes=[rob[s]])
        for hp in range(2):
            k.op("pe", lambda e: e.transpose(out=ptr[s][:, hp, :], in_=ob[s][:, hp * 128:(hp + 1) * 128], identity=g.ident_b[:]),
                 reads=[rob[s], g.r_ident_b], writes=[rptr[s]], inc=(hp == 1))
        k.op("act", lambda e: e.copy(out=oT[:, :, cs], in_=ptr[s][:, 0:2, :]), reads=[rptr[s]], writes=[roT])
    for hp in range(2):
        k.dma("sp", g.mixedT[hp * 128:(hp + 1) * 128, :], oT[:, hp, :], reads=[roT], writes=[g.r_mixedT[0]])
    k.scope_end()


class BankPool:
    def __init__(self, k, name):
        self.t = k.ps(name, [128, 8, 512], F32)
        self.r = [Res("%s_b%d" % (name, i)) for i in range(8)]
        self.nxt = 0

    def take(self, n=1):
        if self.nxt + n > 8:
            self.nxt = 0
        i = self.nxt
        self.nxt = (self.nxt + n) % 8
        return i


def phase_gla(k, g, l):
    k.scope_begin()
    P = g.P
    BP = BankPool(k, "g_ps")
    AU = k.sb("g_AU", [16, 256], F32); rAU = Res("g_AU")
    AB = k.sb("g_AB", [1, 256], F32); rAB = Res("g_AB")
    for d in range(2):
        k.dma("sp", AU[:, d * 128:(d + 1) * 128], P["gla_alpha_up"][l, d], writes=[rAU])
        k.dma("sp", AB[:, d * 128:(d + 1) * 128], P["gla_alpha_b"][l, d].unsqueeze(0), writes=[rAB])
    gnw = k.sb("g_gnw", [128, 256], F32); rgnw = Res("g_gnw")
    k.dma("sp", gnw[:], bcast_rows(P["gla_gn"][l], 128), writes=[rgnw])
    mask2 = k.sb("g_mask2", [128, 2, 4, 128], F32); rmask2 = Res("g_mask2")
    k.op("dve", lambda e: e.tensor_copy(out=mask2[:, 0], in_=g.tri_le[:].unsqueeze(1).to_broadcast([128, 4, 128])), reads=[g.r_tri_le], writes=[rmask2])
    k.op("dve", lambda e: e.tensor_copy(out=mask2[:, 1], in_=g.tri_ge[:].unsqueeze(1).to_broadcast([128, 4, 128])), reads=[g.r_tri_ge], writes=[rmask2])
    BD4 = k.sb("g_BD4", [128, 256], F32); rBD4 = Res("g_BD4")
    k.op("pool", lambda e: e.memset(BD4[:], 0.0), writes=[rBD4])
    for h in range(3):
        k.op("pool", lambda e: e.memset(BD4[h * 32:(h + 1) * 32, h * 64:(h + 1) * 64], 1.0), writes=[rBD4])
    k.op("pool", lambda e: e.memset(BD4[64:128, 192:256], 1.0), writes=[rBD4])
    k.op("pool", lambda e: e.memset(BD4[64:96, 192:256], 0.0), writes=[rBD4])
    v_all = k.sb("g_v", [128, NT, 256], BF16); rv_all = [Res("g_v%d" % c) for c in range(NT)]
    y_all = k.sb("g_y", [128, NT, 256], F32); ry_all = [Res("g_y%d" % c) for c in range(NT)]
    khb_all = k.sb("g_khb", [128, NT, 128], BF16); rkhb = [Res("g_khb%d" % c) for c in range(NT)]
    qdbT = k.sb("g_qdbT", [128, T], BF16); rqdbT = [Res("g_qdbT%d" % c) for c in range(NT)]
    dec_all = k.sb("g_dec", [128, NT, 2], F32); rdec = [Res("g_dec%d" % c) for c in range(NT)]
    oT = k.sb("g_oT", [128, 2, T], BF16); roT = Res("g_oT")
    S32 = [k.sb("g_S32_%d" % d, [128, 256], F32) for d in range(2)]; rS32 = [Res("g_S32_%d" % d) for d in range(2)]
    Sbf = [[k.sb("g_Sbf_%d_%d" % (d, q), [128, 256], BF16) for q in range(2)] for d in range(2)]
    rSbf = [[Res("g_Sbf_%d_%d" % (d, q)) for q in range(2)] for d in range(2)]
    for d in range(2):
        k.op("pool", lambda e: e.memset(S32[d][:], 0.0), writes=[rS32[d]])
        for q in range(2):
            k.op("pool", lambda e: e.memset(Sbf[d][q][:], 0.0), writes=[rSbf[d][q]])

    def dbl(name, shape, dt):
        return [k.sb("%s%d" % (name, i), shape, dt) for i in range(2)], [Res("%s%d" % (name, i)) for i in range(2)]
    gl, rgl = dbl("g_gl", [128, 784], F32)
    xaT, rxaT = dbl("g_xaT", [16, 128], F32)
    ez, rez = dbl("g_ez", [128, 256], F32)
    la, rla = dbl("g_la", [128, 256], F32)
    cum, rcum = dbl("g_cum", [128, 512], F32)
    E1, rE1 = dbl("g_E1", [128, 256], F32)
    E2, rE2 = dbl("g_E2", [128, 256], F32)
    E3, rE3 = dbl("g_E3", [128, 256], F32)
    qd, rqd = dbl("g_qd", [128, 2, 128], BF16)
    kd, rkd = dbl("g_kd", [128, 2, 128], BF16)
    kh, rkh = dbl("g_kh", [128, 2, 128], BF16)
    sT, rsT = dbl("g_sT", [32, 16, 128], BF16)
    qdfT, rqdfT = dbl("g_qdfT", [128, 128], BF16)
    PT, rPT = dbl("g_PT", [128, 2, 4, 128], BF16)
    dsm, rdsm = dbl("g_dsm", [128, 256], F32)

    def state_pre(d, c, khap, rkhr, s):
        bi = BP.take()
        k.op("pe", lambda e: e.matmul(BP.t[:, bi, 0:256], lhsT=khap, rhs=v_all[:, c, :], start=True, stop=True),
             reads=[rkhr, rv_all[c]], writes=[BP.r[bi]])
        k.op("dve", lambda e: e.tensor_tensor(out=dsm[s][:], in0=BP.t[:, bi, 0:256], in1=BD4[:], op=ALU.mult),
             reads=[BP.r[bi], rBD4], writes=[rdsm[s]])

    def state_post(d, c, s):
        k.op("dve", lambda e: e.scalar_tensor_tensor(out=S32[d][:], in0=S32[d][:], scalar=dec_all[:, c, d:d + 1], in1=dsm[s][:],
                                                     op0=ALU.mult, op1=ALU.add), reads=[rS32[d], rdec[c], rdsm[s]], writes=[rS32[d]])
        k.op("act", lambda e: e.copy(out=Sbf[d][c % 2][:], in_=S32[d][:]), reads=[rS32[d]], writes=[rSbf[d][c % 2]])

    post_done = [0]

    def p1(c):
        s = c % 2
        cs = slice(c * 128, (c + 1) * 128)
        k.dma("sp", gl[s][:], g.proj[cs, C_GQ:IN_COLS], reads=[g.r_proj2], writes=[rgl[s]])
        q = gl[s][:, 0:128]; kx = gl[s][:, 128:256]; v = gl[s][:, 256:512]; xa = gl[s][:, 768:784]
        k.op("act", lambda e: e.copy(out=v_all[:, c, :], in_=v), reads=[rgl[s]], writes=[rv_all[c]])
        yield
        b0 = BP.take()
        k.op("pe", lambda e: e.transpose(out=BP.t[0:16, b0, 0:128], in_=xa, identity=g.ident_f[:]),
             reads=[rgl[s], g.r_ident_f], writes=[BP.r[b0]])
        k.op("act", lambda e: e.copy(out=xaT[s][:], in_=BP.t[0:16, b0, 0:128]), reads=[BP.r[b0]], writes=[rxaT[s]])
        yield
        b1 = BP.take()
        k.op("pe", lambda e: e.matmul(BP.t[:, b1, 0:256], lhsT=xaT[s][:], rhs=AU[:], start=True, stop=False),
             reads=[rxaT[s], rAU], writes=[BP.r[b1]], inc=False)
        k.op("pe", lambda e: e.matmul(BP.t[:, b1, 0:256], lhsT=g.ones_f[0:1, :], rhs=AB[:], start=False, stop=True),
             reads=[g.r_ones_f, rAB], writes=[BP.r[b1]])
        k.op("act", lambda e: e.activation(out=ez[s][:], in_=BP.t[:, b1, 0:256], func=AF.Exp, scale=-1.0), reads=[BP.r[b1]], writes=[rez[s]])
        k.op("act", lambda e: e.activation(out=ez[s][:], in_=ez[s][:], func=AF.Ln, bias=g.one_c[:, 0:1]), reads=[rez[s], g.r_one], writes=[rez[s]])
        k.op("dve", lambda e: e.tensor_scalar(out=la[s][:], in0=ez[s][:], scalar1=-1.0 / 16.0, scalar2=None, op0=ALU.mult),
             reads=[rez[s]], writes=[rla[s]])
        yield
        b2 = BP.take()
        for qi, (lt, rlt, co) in enumerate([(g.tri_le, g.r_tri_le, 0), (g.tri_ge, g.r_tri_ge, 128), (g.ones_f, g.r_ones_f, 0), (g.ones_f, g.r_ones_f, 128)]):
            k.op("pe", lambda e: e.matmul(BP.t[:, b2, qi * 128:(qi + 1) * 128], lhsT=lt[:], rhs=la[s][:, co:co + 128], start=True, stop=True),
                 reads=[rlt, rla[s]], writes=[BP.r[b2]], inc=(qi == 3))
        k.op("act", lambda e: e.copy(out=cum[s][:], in_=BP.t[:, b2, :]), reads=[BP.r[b2]], writes=[rcum[s]])
        yield
        b3 = BP.take()
        for d in range(2):
            k.op("pe", lambda e: e.matmul(BP.t[:, b3, d:d + 1], lhsT=la[s][:, d * 128:(d + 1) * 128], rhs=g.ones_f[:, 0:1], start=True, stop=True),
                 reads=[rla[s], g.r_ones_f], writes=[BP.r[b3]], inc=(d == 1))
        k.op("act", lambda e: e.activation(out=dec_all[:, c, :], in_=BP.t[:, b3, 0:2], func=AF.Exp), reads=[BP.r[b3]], writes=[rdec[c]])
        k.op("act", lambda e: e.activation(out=E1[s][:], in_=cum[s][:, 0:256], func=AF.Exp), reads=[rcum[s]], writes=[rE1[s]])
        k.op("act", lambda e: e.activation(out=E2[s][:], in_=cum[s][:, 0:256], func=AF.Exp, scale=-1.0), reads=[rcum[s]], writes=[rE2[s]])
        k.op("dve", lambda e: e.tensor_tensor(out=E3[s][:], in0=cum[s][:, 256:512], in1=cum[s][:, 0:256], op=ALU.subtract), reads=[rcum[s]], writes=[rE3[s]])
        k.op("act", lambda e: e.activation(out=E3[s][:], in_=E3[s][:], func=AF.Exp), reads=[rE3[s]], writes=[rE3[s]])
        yield
        qb = q.unsqueeze(1).to_broadcast([128, 2, 128]); kb = kx.unsqueeze(1).to_broadcast([128, 2, 128])
        k.op("dve", lambda e: e.scalar_tensor_tensor(out=qd[s][:], in0=qb, scalar=32.0 ** -0.5, in1=E1[s][:].rearrange("p (a b) -> p a b", a=2),
                                                     op0=ALU.mult, op1=ALU.mult), reads=[rgl[s], rE1[s]], writes=[rqd[s]])
        k.op("pool", lambda e: e.tensor_tensor(out=kd[s][:], in0=kb, in1=E2[s][:].rearrange("p (a b) -> p a b", a=2), op=ALU.mult),
             reads=[rgl[s], rE2[s]], writes=[rkd[s]])
        k.op("pool", lambda e: e.tensor_tensor(out=kh[s][:], in0=kb, in1=E3[s][:].rearrange("p (a b) -> p a b", a=2), op=ALU.mult),
             reads=[rgl[s], rE3[s]], writes=[rkh[s]])
        k.op("pool", lambda e: e.tensor_copy(out=khb_all[:, c, :], in_=kh[s][:, 1, :]), reads=[rkh[s]], writes=[rkhb[c]])
        yield
        b4 = BP.take(2)
        ptv = BP.t[0:32, b4:b4 + 2, :].rearrange("p a b -> p (a b)").bitcast(BF16)
        for ai, (arr, rarr, d) in enumerate([(qd[s], rqd[s], 0), (qd[s], rqd[s], 1), (kd[s], rkd[s], 0), (kd[s], rkd[s], 1)]):
            for h in range(4):
                idx = ai * 4 + h
                k.op("pe", lambda e: e.transpose(out=ptv[:, idx * 128:(idx + 1) * 128], in_=arr[:, d, h * 32:(h + 1) * 32], identity=g.ident_b[:]),
                     reads=[rarr, g.r_ident_b], writes=[BP.r[b4], BP.r[b4 + 1]], inc=(idx == 15))
        k.op("act", lambda e: e.copy(out=sT[s][:].rearrange("p a b -> p (a b)"), in_=ptv), reads=[BP.r[b4], BP.r[b4 + 1]], writes=[rsT[s]])
        yield
        b5 = BP.take()
        pfv = BP.t[:, b5, :].bitcast(BF16)
        for d in range(2):
            k.op("pe", lambda e: e.transpose(out=pfv[:, d * 128:(d + 1) * 128], in_=qd[s][:, d, :], identity=g.ident_b[:]),
                 reads=[rqd[s], g.r_ident_b], writes=[BP.r[b5]], inc=(d == 1))
        k.op("act", lambda e: e.copy(out=qdfT[s][:], in_=pfv[:, 0:128]), reads=[BP.r[b5]], writes=[rqdfT[s]])
        k.op("act", lambda e: e.copy(out=qdbT[:, cs], in_=pfv[:, 128:256]), reads=[BP.r[b5]], writes=[rqdbT[c]])
        yield
        b6 = BP.take(2)
        for d in range(2):
            for h in range(4):
                k.op("pe", lambda e: e.matmul(BP.t[:, b6 + d, h * 128:(h + 1) * 128], lhsT=sT[s][:, (2 + d) * 4 + h, :], rhs=sT[s][:, d * 4 + h, :],
                                              start=True, stop=True), reads=[rsT[s]], writes=[BP.r[b6 + d]], inc=(h == 3))
        k.op("dve", lambda e: e.tensor_tensor(out=PT[s][:].rearrange("p a b c -> p a (b c)"), in0=BP.t[:, b6:b6 + 2, :],
                                              in1=mask2[:].rearrange("p a b c -> p a (b c)"), op=ALU.mult),
             reads=[BP.r[b6], BP.r[b6 + 1], rmask2], writes=[rPT[s]])
        yield
        while post_done[0] < c:
            yield
        state_pre(0, c, kh[s][:, 0, :], rkh[s], s)
        b7 = BP.take()
        k.op("pe", lambda e: e.matmul(BP.t[:, b7, 0:256], lhsT=qdfT[s][:], rhs=Sbf[0][(c + 1) % 2][:], start=True, stop=False),
             reads=[rqdfT[s], rSbf[0][(c + 1) % 2]], writes=[BP.r[b7]], inc=False)
        for h in range(4):
            for d in range(2):
                last = (h == 3 and d == 1)
                k.op("pe", lambda e: e.matmul(BP.t[:, b7, h * 64:(h + 1) * 64], lhsT=PT[s][:, d, h, :], rhs=v_all[:, c, h * 64:(h + 1) * 64],
                                              start=False, stop=last, skip_group_check=True), reads=[rPT[s], rv_all[c]], writes=[BP.r[b7]], inc=last)
        k.op("act", lambda e: e.copy(out=y_all[:, c, :], in_=BP.t[:, b7, 0:256]), reads=[BP.r[b7]], writes=[ry_all[c]])
        state_post(0, c, s)
        post_done[0] += 1
    import itertools
    chains = []
    for par in range(2):
        glist = [p1(c) for c in range(NT) if c % 2 == par]
        chains.append([itertools.chain.from_iterable(glist), 5 * par, True])
    sentinel = object()
    rnd = 0
    while any(cg[2] for cg in chains):
        for cg in chains:
            if cg[2] and rnd >= cg[1]:
                if next(cg[0], sentinel) is sentinel:
                    cg[2] = False
        rnd += 1
        assert rnd < 100000
    og, rog = dbl("g_og", [128, 256], F32)
    ogs_all = k.sb("g_ogs", [128, NT, 256], BF16); rogs_all = [Res("g_ogs%d" % c) for c in range(NT)]
    for c in range(NT):
        s = c % 2
        k.dma("sp", og[s][:], g.proj[c * 128:(c + 1) * 128, C_GOG:C_GOG + 256], reads=[g.r_proj2], writes=[rog[s]])
        k.op("act", lambda e: e.activation(out=ogs_all[:, c, :], in_=og[s][:], func=AF.Silu), reads=[rog[s]], writes=[rogs_all[c]])
    tmp, rtmp = dbl("g_tmp", [128, 256], F32)
    st, rst = dbl("g_st", [128, 16], F32)
    ob, rob = dbl("g_ob", [128, 256], BF16)
    post2_done = [0]

    def p2(c, n):
        s = c % 2
        cs = slice(c * 128, (c + 1) * 128)
        state_pre(1, c, khb_all[:, c, :], rkhb[c], s)
        yield
        while post2_done[0] < n:
            yield
        b0 = BP.take()
        k.op("pe", lambda e: e.matmul(BP.t[:, b0, 0:256], lhsT=qdbT[:, cs], rhs=Sbf[1][(c + 1) % 2][:], start=True, stop=True),
             reads=[rqdbT[c], rSbf[1][(c + 1) % 2]], writes=[BP.r[b0]])
        k.op("dve", lambda e: e.tensor_tensor(out=y_all[:, c, :], in0=y_all[:, c, :], in1=BP.t[:, b0, 0:256], op=ALU.add),
             reads=[ry_all[c], BP.r[b0]], writes=[ry_all[c]])
        state_post(1, c, s)
        post2_done[0] += 1
        yield
        head_norm_finalize(k, g, y_all[:, c, :], ry_all[c], 4, False, g.eps_c, gnw[:], rgnw, tmp[s][:], rtmp[s], st[s], rst[s])
        yield
        k.op("pool", lambda e: e.tensor_tensor(out=ob[s][:], in0=y_all[:, c, :], in1=ogs_all[:, c, :], op=ALU.mult),
             reads=[ry_all[c], rogs_all[c]], writes=[rob[s]])
        b1 = BP.take()
        pfv = BP.t[:, b1, :].bitcast(BF16)
        for hp in range(2):
            k.op("pe", lambda e: e.transpose(out=pfv[:, hp * 128:(hp + 1) * 128], in_=ob[s][:, hp * 128:(hp + 1) * 128], identity=g.ident_b[:]),
                 reads=[rob[s], g.r_ident_b], writes=[BP.r[b1]], inc=(hp == 1))
        k.op("act", lambda e: e.copy(out=oT[:, :, cs], in_=pfv[:, 0:256].rearrange("p (a b) -> p a b", a=2)), reads=[BP.r[b1]], writes=[roT])
    chains2 = []
    for par in range(2):
        glist = [p2(NT - 1 - n, n) for n in range(NT) if n % 2 == par]
        chains2.append([itertools.chain.from_iterable(glist), 2 * par, True])
    rnd = 0
    while any(cg[2] for cg in chains2):
        for cg in chains2:
            if cg[2] and rnd >= cg[1]:
                if next(cg[0], sentinel) is sentinel:
                    cg[2] = False
        rnd += 1
        assert rnd < 100000
    for hp in range(2):
        k.dma("sp", g.mixedT[768 + hp * 128:768 + (hp + 1) * 128, :], oT[:, hp, :], reads=[roT], writes=[g.r_mixedT[3]])
    k.scope_end()


RWKV_OFFSET = 9


def phase_rwkv(k, g, l):
    k.scope_begin()
    P = g.P
    BP = BankPool(k, "w_ps")

    def cst(name, shape, dt, src, **kw):
        t = k.sb(name, shape, dt); r = Res(name)
        k.dma("sp", t[:], src, writes=[r], **kw)
        return t, r
    mu_b = k.sb("w_mu", [128, 896], F32); rmu = Res("w_mu")
    k.dma("sp", mu_b[:, 0:768], bcast_rows(P["rwkv_mu_rkv"][l].rearrange("a c -> (a c)"), 128), writes=[rmu])
    k.dma("sp", mu_b[:, 768:832], bcast_rows(P["rwkv_mu_w"][l], 128), writes=[rmu])
    k.dma("sp", mu_b[:, 832:896], bcast_rows(P["rwkv_mu_a"][l], 128), writes=[rmu])
    WUP = []; AUP = []
    for d in range(2):
        for (lst, nm, up, b0n) in ((WUP, "w_wup", "rwkv_w_up", "rwkv_w0"), (AUP, "w_aup", "rwkv_a_up", "rwkv_a0")):
            t_ = k.sb("%s%d" % (nm, d), [65, 256], F32); r_ = Res("%s%d" % (nm, d))
            k.dma("sp", t_[0:64, :], P[up][l, d], writes=[r_])
            k.dma("sp", t_[64:65, :], P[b0n][l, d].unsqueeze(0), writes=[r_])
            lst.append((t_, r_))
    kk_b, rkk_b = cst("w_kkb", [128, 256], F32, bcast_rows(P["rwkv_k_k"][l], 128))
    ka_b, rka_b = cst("w_kab", [128, 256], F32, bcast_rows(P["rwkv_k_a"][l], 128))
    rk_b, rrk_b = cst("w_rkb", [128, 256], F32, bcast_rows(P["rwkv_r_k"][l].rearrange("h e -> (h e)"), 128))
    gnw, rgnw = cst("w_gnw", [128, 256], F32, bcast_rows(P["rwkv_gn"][l], 128))
    om_b = k.sb("w_om", [128, 256], F32); rom = Res("w_om")
    k.op("dve", lambda e: e.tensor_scalar(out=om_b[:], in0=ka_b[:], scalar1=-1.0, scalar2=1.0, op0=ALU.mult, op1=ALU.add), reads=[rka_b], writes=[rom])
    gup = k.sb("w_gup", [128, 256], BF16); rgup = Res("w_gup")
    k.dma("pool", gup[:], P["rwkv_g_up"][l], writes=[rgup])
    eps2 = k.sb("w_eps2", [128, 1], F32); reps2 = Res("w_eps2")
    k.op("pool", lambda e: e.memset(eps2[:], 64e-5), writes=[reps2])
    y_all = k.sb("w_y", [128, NT, 256], BF16); ry_all = [Res("w_y%d" % c) for c in range(NT)]
    bo_all = k.sb("w_bo", [128, NT, 256], BF16); rbo_all = [Res("w_bo%d" % c) for c in range(NT)]
    oT = k.sb("w_oT", [128, 2, T], BF16); roT = Res("w_oT")
    H32 = [k.sb("w_H32_%d" % d, [64, 4, 64], F32) for d in range(2)]; rH32 = [Res("w_H32_%d" % d) for d in range(2)]
    Hbf = [k.sb("w_Hbf_%d" % d, [64, 4, 64], BF16) for d in range(2)]; rHbf = [Res("w_Hbf_%d" % d) for d in range(2)]
    for d in range(2):
        k.op("pool", lambda e: e.memset(H32[d][:], 0.0), writes=[rH32[d]])
        k.op("pool", lambda e: e.memset(Hbf[d][:], 0.0), writes=[rHbf[d]])
    cnt = [0]

    def T_(shape, dt, nb=2):
        cnt[0] += 1
        n = "w_t%d" % cnt[0]
        return [k.sb(n + "_%d" % i, shape, dt) for i in range(nb)], [Res(n + "_%d" % i) for i in range(nb)]
    k.scope_begin()
    cur, rcur = T_([128, 1024], F32)
    prv, rprv = T_([128, 896], F32)
    sh, rsh = T_([128, 896], F32)
    txw, rtxw = T_([128, 64], F32)
    xT, rxT = T_([65, 2, 128], F32)
    for i_ in range(2):
        k.op("pool", lambda e: e.memset(xT[i_][64:65], 1.0), writes=[rxT[i_]])
    ld, rld = T_([128, 256], F32)
    aa, raa = T_([128, 256], F32)
    kk, rkk = T_([128, 256], F32)
    t1, rt1 = T_([128, 256], F32)
    t2, rt2 = T_([128, 256], F32)
    km, rkm = T_([128, 256], F32)
    bb, rbb = T_([128, 256], F32)
    st, rst = T_([128, 16], F32)
    LC, rLC = T_([128, 512], F32)
    Einc, rEinc = T_([128, 256], F32)
    Eneg, rEneg = T_([128, 256], F32)
    Eex, rEex = T_([128, 256], F32)
    Ehat, rEhat = T_([128, 256], F32)
    OPS, rOPS = T_([128, 4, 256], BF16, 4)
    Bhat, rBhat = T_([128, 256], BF16, 4)
    Khat, rKhat = T_([128, 256], BF16, 4)
    Vb, rVb = T_([128, 256], BF16, 4)
    GCe, rGCe = T_([64, 4], F32, 4)
    FT, rFT = T_([128, 2, 4, 128], BF16, 4)
    RT, rRT = T_([64, 4, 128], BF16, 4)
    MK, rMK = T_([128, 4, 2, 2, 128], BF16, 4)
    X0, rX0 = T_([128, 1, 4, 128], BF16, 4)
    X1, rX1 = T_([128, 1, 4, 128], BF16, 4)
    TA, rTA = T_([128, 4, 128], BF16, 4)
    PA, rPA = T_([128, 4, 2, 128], BF16, 4)
    PB, rPB = T_([128, 4, 2, 128], BF16, 4)
    Xv, rXv = T_([128, 256], BF16, 4)
    WT, rWT = T_([64, 4, 128], BF16, 4)
    Ub, rUb = T_([128, 256], BF16, 4)
    visited = set()
    visited_y = set()
    H_done = [0, 0]
    early_lock = [None, None]

    def chunk(d, c, n, cid):
        s = d
        L = cid
        while early_lock[d] is not None:
            yield
        early_lock[d] = cid
        cs = slice(c * 128, (c + 1) * 128)
        strict = (g.tri_lt, g.r_tri_lt) if d == 0 else (g.tri_gt, g.r_tri_gt)
        incl = (g.tri_le, g.r_tri_le) if d == 0 else (g.tri_ge, g.r_tri_ge)
        strictT = (g.tri_gt, g.r_tri_gt) if d == 0 else (g.tri_lt, g.r_tri_lt)
        k.dma("sp", cur[s][:], g.proj[cs, 1024:2048], reads=[g.r_proj], writes=[rcur[s]])
        if d == 0:
            if c == 0:
                k.op("pool", lambda e: e.memset(prv[s][:], 0.0), writes=[rprv[s]])
                k.dma("sp", prv[s][1:128, :], g.proj[0:127, 1024:1920], reads=[g.r_proj], writes=[rprv[s]])
            else:
                k.dma("sp", prv[s][:], g.proj[c * 128 - 1:c * 128 + 127, 1024:1920], reads=[g.r_proj], writes=[rprv[s]])
        else:
            if c == NT - 1:
                k.op("pool", lambda e: e.memset(prv[s][:], 0.0), writes=[rprv[s]])
                k.dma("sp", prv[s][0:127, :], g.proj[c * 128 + 1:T, 1024:1920], reads=[g.r_proj], writes=[rprv[s]])
            else:
                k.dma("sp", prv[s][:], g.proj[c * 128 + 1:c * 128 + 129, 1024:1920], reads=[g.r_proj], writes=[rprv[s]])
        k.op("pool", lambda e: e.tensor_tensor(out=prv[s][:], in0=prv[s][:], in1=cur[s][:, 0:896], op=ALU.subtract), reads=[rprv[s], rcur[s]], writes=[rprv[s]])
        k.op("pool", lambda e: e.tensor_tensor(out=prv[s][:], in0=prv[s][:], in1=mu_b[:], op=ALU.mult), reads=[rprv[s], rmu], writes=[rprv[s]])
        k.op("dve", lambda e: e.tensor_tensor(out=sh[s][:], in0=prv[s][:], in1=cur[s][:, 0:896], op=ALU.add), reads=[rprv[s], rcur[s]], writes=[rsh[s]])
        yield
        r_s = sh[s][:, 0:256]; k_s = sh[s][:, 256:512]; v_s = sh[s][:, 512:768]
        k.op("act", lambda e: e.activation(out=txw[s][:], in_=sh[s][:, 768:832], func=AF.Tanh), reads=[rsh[s]], writes=[rtxw[s]])
        b0 = BP.take()
        k.op("pe", lambda e: e.transpose(out=BP.t[0:64, b0, 0:128], in_=txw[s][:], identity=g.ident_f[:]), reads=[rtxw[s], g.r_ident_f], writes=[BP.r[b0]], inc=False)
        k.op("pe", lambda e: e.transpose(out=BP.t[0:64, b0, 128:256], in_=sh[s][:, 832:896], identity=g.ident_f[:]), reads=[rsh[s], g.r_ident_f], writes=[BP.r[b0]])
        k.op("act", lambda e: e.copy(out=xT[s][0:64].rearrange("p a b -> p (a b)"), in_=BP.t[0:64, b0, 0:256]), reads=[BP.r[b0]], writes=[rxT[s]])
        b1 = BP.take()
        for qi, UPt in enumerate([WUP[d], AUP[d]]):
            k.op("pe", lambda e: e.matmul(BP.t[:, b1, qi * 256:(qi + 1) * 256], lhsT=xT[s][:, qi, :], rhs=UPt[0][:], start=True, stop=True),
                 reads=[rxT[s], UPt[1]], writes=[BP.r[b1]], inc=(qi == 1))
        k.op("act", lambda e: e.activation(out=ld[s][:], in_=BP.t[:, b1, 0:256], func=AF.Sigmoid), reads=[BP.r[b1]], writes=[rld[s]])
        k.op("act", lambda e: e.activation(out=aa[s][:], in_=BP.t[:, b1, 256:512], func=AF.Sigmoid), reads=[BP.r[b1]], writes=[raa[s]])
        k.op("dve", lambda e: e.tensor_scalar(out=ld[s][:], in0=ld[s][:], scalar1=-math.exp(-0.5), scalar2=None, op0=ALU.mult), reads=[rld[s]], writes=[rld[s]])
        yield
        k.op("dve", lambda e: e.tensor_tensor(out=kk[s][:], in0=k_s, in1=kk_b[:], op=ALU.mult), reads=[rsh[s], rkk_b], writes=[rkk[s]])
        k.op("pool", lambda e: e.tensor_tensor(out=t1[s][:], in0=kk[s][:], in1=kk[s][:], op=ALU.mult), reads=[rkk[s]], writes=[rt1[s]])
        k.op("dve", lambda e: e.tensor_reduce(out=st[s][:, 0:4], in_=t1[s][:].rearrange("p (h e) -> p h e", h=4), axis=AX.X, op=ALU.add), reads=[rt1[s]], writes=[rst[s]])
        k.op("act", lambda e: e.activation(out=st[s][:, 4:8], in_=st[s][:, 0:4], func=AF.Sqrt), reads=[rst[s]], writes=[rst[s]])
        k.op("dve", lambda e: e.tensor_scalar(out=st[s][:, 4:8], in0=st[s][:, 4:8], scalar1=1e-12, scalar2=None, op0=ALU.max), reads=[rst[s]], writes=[rst[s]])
        k.op("dve", lambda e: e.reciprocal(out=st[s][:, 8:12], in_=st[s][:, 4:8]), reads=[rst[s]], writes=[rst[s]])
        k.op("dve", lambda e: e.tensor_tensor(out=kk[s][:].rearrange("p (h e) -> p h e", h=4), in0=kk[s][:].rearrange("p (h e) -> p h e", h=4),
                                              in1=st[s][:, 8:12].unsqueeze(2).to_broadcast([128, 4, 64]), op=ALU.mult), reads=[rkk[s], rst[s]], writes=[rkk[s]])
        k.op("pool", lambda e: e.tensor_tensor(out=t1[s][:], in0=aa[s][:], in1=ka_b[:], op=ALU.mult), reads=[raa[s], rka_b], writes=[rt1[s]])
        k.op("pool", lambda e: e.tensor_tensor(out=t1[s][:], in0=t1[s][:], in1=om_b[:], op=ALU.add), reads=[rt1[s], rom], writes=[rt1[s]])
        k.op("dve", lambda e: e.tensor_tensor(out=km[s][:], in0=k_s, in1=t1[s][:], op=ALU.mult), reads=[rsh[s], rt1[s]], writes=[rkm[s]])
        k.op("pool", lambda e: e.tensor_tensor(out=bb[s][:], in0=kk[s][:], in1=aa[s][:], op=ALU.mult), reads=[rkk[s], raa[s]], writes=[rbb[s]])
        k.op("pool", lambda e: e.tensor_tensor(out=t2[s][:], in0=r_s, in1=km[s][:], op=ALU.mult), reads=[rsh[s], rkm[s]], writes=[rt2[s]])
        k.op("pool", lambda e: e.tensor_tensor(out=t2[s][:], in0=t2[s][:], in1=rk_b[:], op=ALU.mult), reads=[rt2[s], rrk_b], writes=[rt2[s]])
        k.op("dve", lambda e: e.tensor_reduce(out=st[s][:, 12:16], in_=t2[s][:].rearrange("p (h e) -> p h e", h=4), axis=AX.X, op=ALU.add), reads=[rt2[s]], writes=[rst[s]])
        first = c not in visited
        visited.add(c)
        bdst = bo_all[:, c, :] if first else t2[s][:]
        k.op("dve", lambda e: e.tensor_tensor(out=bdst.rearrange("p (h e) -> p h e", h=4), in0=v_s.rearrange("p (h e) -> p h e", h=4),
                                              in1=st[s][:, 12:16].unsqueeze(2).to_broadcast([128, 4, 64]), op=ALU.mult),
             reads=[rsh[s], rst[s]], writes=[rbo_all[c] if first else rt2[s]])
        if not first:
            k.op("pool", lambda e: e.tensor_tensor(out=bo_all[:, c, :], in0=bo_all[:, c, :], in1=t2[s][:], op=ALU.add), reads=[rbo_all[c], rt2[s]], writes=[rbo_all[c]])
        yield
        b2 = BP.take()
        k.op("pe", lambda e: e.matmul(BP.t[:, b2, 0:256], lhsT=incl[0][:], rhs=ld[s][:], start=True, stop=True), reads=[incl[1], rld[s]], writes=[BP.r[b2]], inc=False)
        k.op("pe", lambda e: e.matmul(BP.t[:, b2, 256:512], lhsT=g.ones_f[:], rhs=ld[s][:], start=True, stop=True), reads=[g.r_ones_f, rld[s]], writes=[BP.r[b2]])
        k.op("act", lambda e: e.copy(out=LC[s][:], in_=BP.t[:, b2, :]), reads=[BP.r[b2]], writes=[rLC[s]])
        b3 = BP.take()
        for h in range(4):
            k.op("pe", lambda e: e.matmul(BP.t[0:64, b3, h:h + 1], lhsT=ld[s][:, h * 64:(h + 1) * 64], rhs=g.ones_f[:, 0:1], start=True, stop=True),
                 reads=[rld[s], g.r_ones_f], writes=[BP.r[b3]], inc=(h == 3))
        k.op("act", lambda e: e.activation(out=GCe[L][:], in_=BP.t[0:64, b3, 0:4], func=AF.Exp), reads=[BP.r[b3]], writes=[rGCe[L]])
        k.op("act", lambda e: e.activation(out=Einc[s][:], in_=LC[s][:, 0:256], func=AF.Exp), reads=[rLC[s]], writes=[rEinc[s]])
        k.op("act", lambda e: e.activation(out=Eneg[s][:], in_=LC[s][:, 0:256], func=AF.Exp, scale=-1.0), reads=[rLC[s]], writes=[rEneg[s]])
        k.op("dve", lambda e: e.tensor_tensor(out=Eex[s][:], in0=LC[s][:, 0:256], in1=ld[s][:], op=ALU.subtract), reads=[rLC[s], rld[s]], writes=[rEex[s]])
        k.op("act", lambda e: e.activation(out=Eex[s][:], in_=Eex[s][:], func=AF.Exp), reads=[rEex[s]], writes=[rEex[s]])
        k.op("dve", lambda e: e.tensor_tensor(out=Ehat[s][:], in0=LC[s][:, 256:512], in1=LC[s][:, 0:256], op=ALU.subtract), reads=[rLC[s]], writes=[rEhat[s]])
        k.op("act", lambda e: e.activation(out=Ehat[s][:], in_=Ehat[s][:], func=AF.Exp), reads=[rEhat[s]], writes=[rEhat[s]])
        k.op("dve", lambda e: e.scalar_tensor_tensor(out=OPS[L][:, 0, :], in0=kk[s][:], scalar=-1.0, in1=Eex[s][:], op0=ALU.mult, op1=ALU.mult), reads=[rkk[s], rEex[s]], writes=[rOPS[L]])
        k.op("pool", lambda e: e.tensor_tensor(out=OPS[L][:, 1, :], in0=r_s, in1=Einc[s][:], op=ALU.mult), reads=[rsh[s], rEinc[s]], writes=[rOPS[L]])
        k.op("dve", lambda e: e.tensor_tensor(out=OPS[L][:, 2, :], in0=bb[s][:], in1=Eneg[s][:], op=ALU.mult), reads=[rbb[s], rEneg[s]], writes=[rOPS[L]])
        k.op("pool", lambda e: e.tensor_tensor(out=OPS[L][:, 3, :], in0=km[s][:], in1=Eneg[s][:], op=ALU.mult), reads=[rkm[s], rEneg[s]], writes=[rOPS[L]])
        k.op("dve", lambda e: e.tensor_tensor(out=Bhat[L][:], in0=bb[s][:], in1=Ehat[s][:], op=ALU.mult), reads=[rbb[s], rEhat[s]], writes=[rBhat[L]])
        k.op("pool", lambda e: e.tensor_tensor(out=Khat[L][:], in0=km[s][:], in1=Ehat[s][:], op=ALU.mult), reads=[rkm[s], rEhat[s]], writes=[rKhat[L]])
        k.op("act", lambda e: e.copy(out=Vb[L][:], in_=v_s), reads=[rsh[s]], writes=[rVb[L]])
        early_lock[d] = None
        yield
        b4 = BP.take()
        pfv = BP.t[:, b4, :].bitcast(BF16)
        for hp in range(2):
            for xi in range(4):
                idx = hp * 4 + xi
                k.op("pe", lambda e: e.transpose(out=pfv[:, idx * 128:(idx + 1) * 128], in_=OPS[L][:, xi, hp * 128:(hp + 1) * 128], identity=g.ident_b[:]),
                     reads=[rOPS[L], g.r_ident_b], writes=[BP.r[b4]], inc=(idx == 7))
        k.op("act", lambda e: e.copy(out=FT[L][:].rearrange("p a b c -> p (a b c)"), in_=pfv), reads=[BP.r[b4]], writes=[rFT[L]])
        b5 = BP.take()
        prv_ = BP.t[0:64, b5, :].bitcast(BF16)
        for h in range(4):
            k.op("pe", lambda e: e.transpose(out=prv_[:, h * 128:(h + 1) * 128], in_=OPS[L][:, 1, h * 64:(h + 1) * 64], identity=g.ident_b[:]),
                 reads=[rOPS[L], g.r_ident_b], writes=[BP.r[b5]], inc=(h == 3))
        k.op("act", lambda e: e.copy(out=RT[L][:].rearrange("p a b -> p (a b)"), in_=prv_[:, 0:512]), reads=[BP.r[b5]], writes=[rRT[L]])
        yield
        for li, xi_l in enumerate([2, 3]):
            b6 = BP.take(2)
            for hp in range(2):
                for hh in range(2):
                    k.op("pe", lambda e: e.matmul(BP.t[:, b6 + hh, hp * 256:(hp + 1) * 256], lhsT=FT[L][hh * 64:(hh + 1) * 64, hp, xi_l, :],
                                                  rhs=FT[L][hh * 64:(hh + 1) * 64, hp, 0:2, :].rearrange("p a b -> p (a b)"), start=True, stop=True),
                         reads=[rFT[L]], writes=[BP.r[b6 + hh]], inc=(hp == 1))
            for hh in range(2):
                pv = BP.t[:, b6 + hh, :].rearrange("p (hp x t) -> p hp x t", hp=2, x=2)
                k.op("dve", lambda e: e.tensor_tensor(out=MK[L][:, 2 * li, hh], in0=pv[:, :, 0, :], in1=strict[0][:].unsqueeze(1).to_broadcast([128, 2, 128]), op=ALU.mult),
                     reads=[BP.r[b6 + hh], strict[1]], writes=[rMK[L]])
                k.op("dve", lambda e: e.tensor_tensor(out=MK[L][:, 2 * li + 1, hh], in0=pv[:, :, 1, :], in1=incl[0][:].unsqueeze(1).to_broadcast([128, 2, 128]), op=ALU.mult),
                     reads=[BP.r[b6 + hh], incl[1]], writes=[rMK[L]])
        yield
        b7 = BP.take(2)
        for hp in range(2):
            for hh in range(2):
                k.op("pe", lambda e: e.matmul(BP.t[:, b7 + hh, hp * 128:(hp + 1) * 128], lhsT=FT[L][hh * 64:(hh + 1) * 64, hp, 0, :],
                                              rhs=FT[L][hh * 64:(hh + 1) * 64, hp, 2, :], start=True, stop=True), reads=[rFT[L]], writes=[BP.r[b7 + hh]], inc=(hp == 1))
        for hh in range(2):
            k.op("dve", lambda e: e.tensor_tensor(out=X0[L][:, 0, hh * 2:hh * 2 + 2, :], in0=BP.t[:, b7 + hh, 0:256].rearrange("p (a b) -> p a b", a=2),
                                                  in1=strictT[0][:].unsqueeze(1).to_broadcast([128, 2, 128]), op=ALU.mult), reads=[BP.r[b7 + hh], strictT[1]], writes=[rX0[L]])
        k.op("pool", lambda e: e.tensor_copy(out=PA[L][:, :, 0, :], in_=MK[L][:, 0].rearrange("p a b c -> p (a b) c")), reads=[rMK[L]], writes=[rPA[L]])
        k.op("pool", lambda e: e.tensor_copy(out=PA[L][:, :, 1, :], in_=g.ident_b[:].unsqueeze(1).to_broadcast([128, 4, 128])), reads=[g.r_ident_b], writes=[rPA[L]])
        Xc, rXc, Xn, rXn = X0[L][:, 0], rX0[L], X1[L][:, 0], rX1[L]
        Pc, rPc, Pn, rPn = PA[L], rPA[L], PB[L], rPB[L]
        for it in range(6):
            b8 = BP.take()
            for hx in range(4):
                k.op("pe", lambda e: e.matmul(BP.t[:, b8, hx * 128:(hx + 1) * 128], lhsT=Pc[:, hx, 0, :], rhs=Xc[:, hx, :], start=True, stop=True),
                     reads=[rPc, rXc], writes=[BP.r[b8]], inc=(hx == 3))
            k.op("act", lambda e: e.copy(out=Xn.rearrange("p b c -> p (b c)"), in_=BP.t[:, b8, :]), reads=[BP.r[b8]], writes=[rXn])
            b9 = BP.take(2)
            if it < 5:
                for hx in range(4):
                    k.op("pe", lambda e: e.matmul(BP.t[:, b9 + hx // 2, (hx % 2) * 256:(hx % 2 + 1) * 256], lhsT=Xc[:, hx, :],
                                                  rhs=Pc[:, hx, :, :].rearrange("p a b -> p (a b)"), start=True, stop=True),
                         reads=[rXc, rPc], writes=[BP.r[b9 + hx // 2]], inc=(hx % 2 == 1))
                pv = BP.t[:, b9:b9 + 2, :].rearrange("p a (h x t) -> p (a h) x t", h=2, x=2)
                k.op("act", lambda e: e.copy(out=Pn[:, :, 0, :], in_=pv[:, :, 0, :]), reads=[BP.r[b9], BP.r[b9 + 1]], writes=[rPn])
                k.op("dve", lambda e: e.tensor_tensor(out=Pn[:, :, 1, :], in0=pv[:, :, 1, :], in1=Pc[:, :, 1, :], op=ALU.add),
                     reads=[BP.r[b9], BP.r[b9 + 1], rPc], writes=[rPn])
            else:
                for hx in range(4):
                    k.op("pe", lambda e: e.matmul(BP.t[:, b9, hx * 128:(hx + 1) * 128], lhsT=Xc[:, hx, :], rhs=Pc[:, hx, 1, :], start=True, stop=True),
                         reads=[rXc, rPc], writes=[BP.r[b9]], inc=(hx == 3))
                k.op("dve", lambda e: e.tensor_tensor(out=Pn[:, :, 1, :], in0=BP.t[:, b9, :].rearrange("p (h t) -> p h t", h=4), in1=Pc[:, :, 1, :], op=ALU.add),
                     reads=[BP.r[b9], rPc], writes=[rPn])
            Xc, rXc, Xn, rXn = Xn, rXn, Xc, rXc
            Pc, rPc, Pn, rPn = Pn, rPn, Pc, rPc
            yield
        b9 = BP.take()
        for hx in range(4):
            k.op("pe", lambda e: e.matmul(BP.t[:, b9, hx * 128:(hx + 1) * 128], lhsT=Xc[:, hx, :], rhs=Pc[:, hx, 1, :], start=True, stop=True),
                 reads=[rXc, rPc], writes=[BP.r[b9]], inc=(hx == 3))
        k.op("dve", lambda e: e.tensor_tensor(out=TA[L][:], in0=BP.t[:, b9, :].rearrange("p (h t) -> p h t", h=4), in1=Pc[:, :, 1, :], op=ALU.add),
             reads=[BP.r[b9], rPc], writes=[rTA[L]])
        Tc, rTc = TA[L], rTA[L]
        Tinv, rTinv = Tc, rTc
        yield
        b10 = BP.take()
        for h in range(4):
            hp, hh = h // 2, h % 2
            k.op("pe", lambda e: e.matmul(BP.t[:, b10, h * 64:(h + 1) * 64], lhsT=MK[L][:, 2, hh, hp, :], rhs=Vb[L][:, h * 64:(h + 1) * 64], start=True, stop=True),
                 reads=[rMK[L], rVb[L]], writes=[BP.r[b10]], inc=(h == 3))
        k.op("act", lambda e: e.copy(out=Xv[L][:], in_=BP.t[:, b10, 0:256]), reads=[BP.r[b10]], writes=[rXv[L]])
        b11 = BP.take()
        for h in range(4):
            hp, hh = h // 2, h % 2
            k.op("pe", lambda e: e.matmul(BP.t[0:64, b11, h * 128:(h + 1) * 128], lhsT=OPS[L][:, 0, h * 64:(h + 1) * 64], rhs=Tinv[:, hh * 2 + hp, :], start=True, stop=True),
                 reads=[rOPS[L], rTinv], writes=[BP.r[b11]], inc=(h == 3))
        k.op("act", lambda e: e.copy(out=WT[L][:].rearrange("p a b -> p (a b)"), in_=BP.t[0:64, b11, :]), reads=[BP.r[b11]], writes=[rWT[L]])
        yield
        while H_done[d] < n:
            yield
        b12 = BP.take()
        for h in range(4):
            hp, hh = h // 2, h % 2
            k.op("pe", lambda e: e.matmul(BP.t[:, b12, h * 64:(h + 1) * 64], lhsT=Tinv[:, hh * 2 + hp, :], rhs=Xv[L][:, h * 64:(h + 1) * 64], start=True, stop=False),
                 reads=[rTinv, rXv[L]], writes=[BP.r[b12]], inc=False)
            k.op("pe", lambda e: e.matmul(BP.t[:, b12, h * 64:(h + 1) * 64], lhsT=WT[L][:, h, :], rhs=Hbf[d][:, h, :], start=False, stop=True),
                 reads=[rWT[L], rHbf[d]], writes=[BP.r[b12]], inc=(h == 3))
        k.op("act", lambda e: e.copy(out=Ub[L][:], in_=BP.t[:, b12, 0:256]), reads=[BP.r[b12]], writes=[rUb[L]])
        yield
        b13 = BP.take()
        for h in range(4):
            hp, hh = h // 2, h % 2
            k.op("pe", lambda e: e.matmul(BP.t[:, b13, h * 64:(h + 1) * 64], lhsT=RT[L][:, h, :], rhs=Hbf[d][:, h, :], start=True, stop=False),
                 reads=[rRT[L], rHbf[d]], writes=[BP.r[b13]], inc=False)
            k.op("pe", lambda e: e.matmul(BP.t[:, b13, h * 64:(h + 1) * 64], lhsT=MK[L][:, 1, hh, hp, :], rhs=Ub[L][:, h * 64:(h + 1) * 64], start=False, stop=False),
                 reads=[rMK[L], rUb[L]], writes=[BP.r[b13]], inc=False)
            k.op("pe", lambda e: e.matmul(BP.t[:, b13, h * 64:(h + 1) * 64], lhsT=MK[L][:, 3, hh, hp, :], rhs=Vb[L][:, h * 64:(h + 1) * 64], start=False, stop=True),
                 reads=[rMK[L], rVb[L]], writes=[BP.r[b13]], inc=(h == 3))
        first_y = c not in visited_y
        visited_y.add(c)
        if first_y:
            k.op("act", lambda e: e.copy(out=y_all[:, c, :], in_=BP.t[:, b13, 0:256]), reads=[BP.r[b13]], writes=[ry_all[c]])
        else:
            k.op("dve", lambda e: e.tensor_tensor(out=y_all[:, c, :], in0=y_all[:, c, :], in1=BP.t[:, b13, 0:256], op=ALU.add), reads=[ry_all[c], BP.r[b13]], writes=[ry_all[c]])
        yield
        b14 = BP.take()
        for h in range(4):
            k.op("pe", lambda e: e.matmul(BP.t[0:64, b14, h * 64:(h + 1) * 64], lhsT=Bhat[L][:, h * 64:(h + 1) * 64], rhs=Ub[L][:, h * 64:(h + 1) * 64], start=True, stop=False),
                 reads=[rBhat[L], rUb[L]], writes=[BP.r[b14]], inc=False)
            k.op("pe", lambda e: e.matmul(BP.t[0:64, b14, h * 64:(h + 1) * 64], lhsT=Khat[L][:, h * 64:(h + 1) * 64], rhs=Vb[L][:, h * 64:(h + 1) * 64], start=False, stop=True),
                 reads=[rKhat[L], rVb[L]], writes=[BP.r[b14]], inc=(h == 3))
        k.op("dve", lambda e: e.tensor_tensor(out=H32[d][:], in0=H32[d][:], in1=GCe[L][:].unsqueeze(2).to_broadcast([64, 4, 64]), op=ALU.mult),
             reads=[rH32[d], rGCe[L]], writes=[rH32[d]])
        k.op("dve", lambda e: e.tensor_tensor(out=H32[d][:].rearrange("p a b -> p (a b)"), in0=H32[d][:].rearrange("p a b -> p (a b)"), in1=BP.t[0:64, b14, 0:256], op=ALU.add),
             reads=[rH32[d], BP.r[b14]], writes=[rH32[d]])
        k.op("act", lambda e: e.copy(out=Hbf[d][:], in_=H32[d][:]), reads=[rH32[d]], writes=[rHbf[d]])
        H_done[d] += 1

    import itertools
    S_ = 18
    gens = []
    for d_ in range(2):
        for par in range(2):
            order = [nn for nn in range(NT) if nn % 2 == par]
            cc = (lambda nn, d_=d_: nn if d_ == 0 else NT - 1 - nn)
            glist = [chunk(d_, cc(nn), nn, 2 * d_ + par) for nn in order]
            gens.append([itertools.chain.from_iterable(glist), (S_ // 2) * par + (S_ // 4) * d_, True])
    sentinel = object()
    rnd = 0
    while any(gg[2] for gg in gens):
        for gg in gens:
            if gg[2] and rnd >= gg[1]:
                if next(gg[0], sentinel) is sentinel:
                    gg[2] = False
        rnd += 1
        assert rnd < 100000
    k.scope_end()
    st, rst = T_([128, 16], F32)
    xg, rxg = T_([128, 128], F32)
    sg, rsg = T_([128, 128], BF16)
    sgT, rsgT = T_([128, 128], BF16)
    tmp, rtmp = T_([128, 256], F32)
    yf, ryf = T_([128, 256], F32)
    ob, rob = T_([128, 256], BF16)
    sg_all = k.sb("w_sgall", [128, NT, 128], BF16); rsg_all = [Res("w_sgall%d" % c) for c in range(NT)]
    for c in range(NT):
        s = c % 2
        k.dma("sp", xg[s][:], g.proj[c * 128:(c + 1) * 128, C_WXG:C_WXG + 128], reads=[g.r_proj], writes=[rxg[s]])
        k.op("act", lambda e: e.activation(out=sg_all[:, c, :], in_=xg[s][:], func=AF.Sigmoid), reads=[rxg[s]], writes=[rsg_all[c]])
    for c in range(NT):
        s = c % 2
        cs = slice(c * 128, (c + 1) * 128)
        b0 = BP.take()
        pfv = BP.t[:, b0, :].bitcast(BF16)
        k.op("pe", lambda e: e.transpose(out=pfv[:, 0:128], in_=sg_all[:, c, :], identity=g.ident_b[:]), reads=[rsg_all[c], g.r_ident_b], writes=[BP.r[b0]])
        k.op("act", lambda e: e.copy(out=sgT[s][:], in_=pfv[:, 0:128]), reads=[BP.r[b0]], writes=[rsgT[s]])
        b1 = BP.take()
        k.op("pe", lambda e: e.matmul(BP.t[:, b1, 0:256], lhsT=sgT[s][:], rhs=gup[:], start=True, stop=True), reads=[rsgT[s], rgup], writes=[BP.r[b1]])
        k.op("pool", lambda e: e.tensor_copy(out=yf[s][:], in_=y_all[:, c, :]), reads=[ry_all[c]], writes=[ryf[s]])
        head_norm_finalize(k, g, yf[s][:], ryf[s], 4, True, eps2, gnw[:], rgnw, tmp[s][:], rtmp[s], st[s], rst[s])
        k.op("pool", lambda e: e.tensor_tensor(out=yf[s][:], in0=yf[s][:], in1=bo_all[:, c, :], op=ALU.add), reads=[ryf[s], rbo_all[c]], writes=[ryf[s]])
        k.op("dve", lambda e: e.tensor_tensor(out=ob[s][:], in0=yf[s][:], in1=BP.t[:, b1, 0:256], op=ALU.mult), reads=[ryf[s], BP.r[b1]], writes=[rob[s]])
        b2 = BP.take()
        pf2 = BP.t[:, b2, :].bitcast(BF16)
        for hp in range(2):
            k.op("pe", lambda e: e.transpose(out=pf2[:, hp * 128:(hp + 1) * 128], in_=ob[s][:, hp * 128:(hp + 1) * 128], identity=g.ident_b[:]),
                 reads=[rob[s], g.r_ident_b], writes=[BP.r[b2]], inc=(hp == 1))
        k.op("act", lambda e: e.copy(out=oT[:, :, cs], in_=pf2[:, 0:256].rearrange("p (a b) -> p a b", a=2)), reads=[BP.r[b2]], writes=[roT])
    for hp in range(2):
        k.dma("sp", g.mixedT[256 + hp * 128:256 + (hp + 1) * 128, :], oT[:, hp, :], reads=[roT], writes=[g.r_mixedT[1]])
    k.scope_end()


def phase_outproj_router(k, g, l):
    k.scope_begin()
    P = g.P
    BP = BankPool(k, "o_ps")
    Wo = k.sb("o_W", [128, 8, D], BF16); rWo = Res("o_W")
    k.dma("pool", Wo[:], P["w_out"][l].rearrange("(kc p) c -> p kc c", p=128), writes=[rWo], max_dma_last_dim=4096)
    wb = k.sb("o_nwb", [128, D], F32); rwb = Res("o_nwb")
    k.dma("sp", wb[:], bcast_rows(P["norm_ffn"][l], 128), writes=[rwb])
    Wr = k.sb("o_Wr", [128, 8, NE], BF16); rWr = Res("o_Wr")
    k.dma("pool", Wr[:], P["router_w"][l].rearrange("(kc p) e -> p kc e", p=128), writes=[rWr])
    rbias = k.sb("o_rb", [1, NE], F32); rrb = Res("o_rb")
    k.dma("sp", rbias[:], P["router_b"][l].unsqueeze(0), writes=[rrb])
    g.nrm_sq = k.sb("o_sq", [128, D], F32); g.r_nrm_sq = Res("o_sq")
    g.nrm_ss = [k.sb("o_ss%d" % i, [128, 4], F32) for i in range(2)]; g.r_nrm_ss = [Res("o_ss%d" % i) for i in range(2)]

    def dbl(name, shape, dt):
        return [k.sb("%s%d" % (name, i), shape, dt) for i in range(2)], [Res("%s%d" % (name, i)) for i in range(2)]
    mT, rmT = dbl("o_mT", [128, 8, 512], BF16)
    xt, rxt = dbl("o_xt", [128, D], F32)
    xn, rxn = dbl("o_xn", [128, D], BF16)
    xnT, rxnT = dbl("o_xnT", [128, 8, 128], BF16)
    sm, rsm = dbl("o_sm", [128, 8], F32)
    ex, rex = dbl("o_ex", [128, NE], F32)
    def stage1(j):
        gi, ti = j // 4, j % 4
        hs = gi % 2
        if ti == 0:
            k.dma("sp", mT[hs][:], g.mixedT[:, gi * 512:(gi + 1) * 512].rearrange("(kc p) t -> p kc t", p=128),
                  reads=g.r_mixedT, writes=[rmT[hs]])
        s = j % 2
        rows = slice(j * 128, (j + 1) * 128)
        k.dma("sp", xt[s][:], g.x_src[rows, :], reads=[g.r_x], writes=[rxt[s]])
        b0 = BP.take(2)
        for half in range(2):
            for kc in range(8):
                k.op("pe", lambda e: e.matmul(BP.t[:, b0 + half, :], lhsT=mT[hs][:, kc, ti * 128:(ti + 1) * 128],
                                              rhs=Wo[:, kc, half * 512:(half + 1) * 512], start=(kc == 0), stop=(kc == 7)),
                     reads=[rmT[hs], rWo], writes=[BP.r[b0 + half]], inc=(kc == 7))
        k.op("dve", lambda e: e.tensor_tensor(out=xt[s][:].rearrange("p (a b) -> p a b", a=2), in0=xt[s][:].rearrange("p (a b) -> p a b", a=2),
                                              in1=BP.t[:, b0:b0 + 2, :], op=ALU.add), reads=[rxt[s], BP.r[b0], BP.r[b0 + 1]], writes=[rxt[s]])
        k.dma("sp", g.x_cur[rows, :], xt[s][:], reads=[rxt[s]], writes=[g.r_xst])
        rmsnorm_tile(k, g, xt[s][:], rxt[s], wb[:], rwb, xn[s][:], rxn[s], j)
        k.dma("sp", g.xn2[rows, :], xn[s][:], reads=[rxn[s]], writes=[g.r_xn2])

    def stage2(j):
        s = j % 2
        b1 = BP.take()
        pfv = BP.t[:, b1, :].bitcast(BF16)
        for kc in range(8):
            k.op("pe", lambda e: e.transpose(out=pfv[:, kc * 128:(kc + 1) * 128], in_=xn[s][:, kc * 128:(kc + 1) * 128], identity=g.ident_b[:]),
                 reads=[rxn[s], g.r_ident_b], writes=[BP.r[b1]], inc=(kc == 7))
        k.op("act", lambda e: e.copy(out=xnT[s][:].rearrange("p a b -> p (a b)"), in_=pfv), reads=[BP.r[b1]], writes=[rxnT[s]])
        b2 = BP.take()
        for kc in range(8):
            k.op("pe", lambda e: e.matmul(BP.t[:, b2, 0:NE], lhsT=xnT[s][:, kc, :], rhs=Wr[:, kc, :], start=(kc == 0), stop=False),
                 reads=[rxnT[s], rWr], writes=[BP.r[b2]], inc=False)
        k.op("pe", lambda e: e.matmul(BP.t[:, b2, 0:NE], lhsT=g.ones_f[0:1, :], rhs=rbias[:], start=False, stop=True),
             reads=[g.r_ones_f, rrb], writes=[BP.r[b2]])
        k.op("dve", lambda e: e.tensor_reduce(out=sm[s][:, 0:1], in_=BP.t[:, b2, 0:NE], axis=AX.X, op=ALU.max), reads=[BP.r[b2]], writes=[rsm[s]])
        k.op("dve", lambda e: e.tensor_scalar(out=sm[s][:, 1:2], in0=sm[s][:, 0:1], scalar1=-1.0, scalar2=None, op0=ALU.mult), reads=[rsm[s]], writes=[rsm[s]])
        k.op("act", lambda e: e.activation(out=ex[s][:], in_=BP.t[:, b2, 0:NE], func=AF.Exp, bias=sm[s][:, 1:2], accum_out=sm[s][:, 2:3]),
             reads=[BP.r[b2], rsm[s]], writes=[rex[s], rsm[s]])
        k.op("dve", lambda e: e.reciprocal(out=sm[s][:, 3:4], in_=sm[s][:, 2:3]), reads=[rsm[s]], writes=[rsm[s]])
        k.op("dve", lambda e: e.tensor_scalar(out=g.aff_all[:, j, :], in0=ex[s][:], scalar1=sm[s][:, 3:4], scalar2=None, op0=ALU.mult),
             reads=[rex[s], rsm[s]], writes=[g.r_aff])

    g.r_xst = Res("x_store")
    stage1(0)
    for j in range(NT):
        if j + 1 < NT:
            stage1(j + 1)
        stage2(j)
    g.r_x = Res("x_cur_next")
    g.x_src = g.x_cur
    k.scope_end()


def phase_moe(k, g, l):
    k.scope_begin()
    P = g.P
    BP = BankPool(k, "m_ps")
    aff = g.aff_all
    Wn = ["exp_w_gate", "exp_w_up", "exp_w_down"]
    Wt = [[k.sb("m_W%d_%d" % (i, b), [128, 8, D], BF16) for i in range(3)] for b in range(2)]
    rWt = [[Res("m_W%d_%d" % (i, b)) for i in range(3)] for b in range(2)]

    def load_w(ei):
        b = ei % 2
        for i in range(3):
            k.dma("pool", Wt[b][i][:], P[Wn[i]][l, ei].rearrange("(kc p) f -> p kc f", p=128), writes=[rWt[b][i]], max_dma_last_dim=4096)
    load_w(0)
    A3 = [128, NT, NE]
    th = k.sb("m_th", [128, NE], F32); rth = Res("m_th")
    lo = k.sb("m_lo", [128, NE], F32); rlo = Res("m_lo")
    ge = k.sb("m_ge", [128, NE], F32); rge = Res("m_ge")
    tq = k.sb("m_tq", [128, NE], F32); rtq = Res("m_tq")
    pc = k.sb("m_pc", [128, NE], F32); rpc = Res("m_pc")
    cmp_ = k.sb("m_cmp", A3, F32); rcmp = Res("m_cmp")
    k.op("pool", lambda e: e.memset(th[:], 0.5), writes=[rth])
    k.op("pool", lambda e: e.memset(lo[:], 0.0), writes=[rlo])
    step = 0.25
    for it in range(24):
        k.op("dve", lambda e: e.tensor_tensor(out=cmp_[:], in0=aff[:], in1=th[:].unsqueeze(1).to_broadcast(A3), op=ALU.is_gt),
             reads=[g.r_aff, rth], writes=[rcmp])
        k.op("dve", lambda e: e.tensor_reduce(out=pc[:], in_=cmp_[:].rearrange("p j e -> p e j"), axis=AX.X, op=ALU.add), reads=[rcmp], writes=[rpc])
        b0 = BP.take()
        k.op("pe", lambda e: e.matmul(BP.t[:, b0, 0:NE], lhsT=g.ones_f[:], rhs=pc[:], start=True, stop=True), reads=[g.r_ones_f, rpc], writes=[BP.r[b0]])
        k.op("dve", lambda e: e.tensor_scalar(out=ge[:], in0=BP.t[:, b0, 0:NE], scalar1=float(CAP) - 0.5, scalar2=None, op0=ALU.is_ge), reads=[BP.r[b0]], writes=[rge])
        k.op("dve", lambda e: e.tensor_tensor(out=tq[:], in0=th[:], in1=ge[:], op=ALU.mult), reads=[rth, rge], writes=[rtq])
        k.op("dve", lambda e: e.tensor_tensor(out=lo[:], in0=lo[:], in1=tq[:], op=ALU.max), reads=[rlo, rtq], writes=[rlo])
        k.op("dve", lambda e: e.tensor_scalar(out=tq[:], in0=ge[:], scalar1=2.0 * step, scalar2=-step, op0=ALU.mult, op1=ALU.add), reads=[rge], writes=[rtq])
        k.op("dve", lambda e: e.tensor_tensor(out=th[:], in0=th[:], in1=tq[:], op=ALU.add), reads=[rth, rtq], writes=[rth])
        step *= 0.5
    mask = k.sb("m_mask", A3, F32); rmask = Res("m_mask")
    k.op("dve", lambda e: e.tensor_tensor(out=mask[:], in0=aff[:], in1=lo[:].unsqueeze(1).to_broadcast(A3), op=ALU.is_gt), reads=[g.r_aff, rlo], writes=[rmask])
    ca = k.sb("m_ca", A3, F32); rca = Res("m_ca")
    cb = k.sb("m_cb", A3, F32); rcb = Res("m_cb")
    k.op("dve", lambda e: e.tensor_copy(out=ca[:], in_=mask[:]), reads=[rmask], writes=[rca])
    src, rsrc, dst, rdst = ca, rca, cb, rcb
    for sft in (1, 2, 4, 8, 16):
        k.op("dve", lambda e: e.tensor_tensor(out=dst[:, sft:, :], in0=src[:, sft:, :], in1=src[:, 0:NT - sft, :], op=ALU.add), reads=[rsrc], writes=[rdst])
        k.op("dve", lambda e: e.tensor_copy(out=dst[:, 0:sft, :], in_=src[:, 0:sft, :]), reads=[rsrc], writes=[rdst])
        src, rsrc, dst, rdst = dst, rdst, src, rsrc
    mask_b = k.sb("m_maskb", A3, BF16); rmaskb = Res("m_maskb")
    cum_b = k.sb("m_cumb", A3, BF16); rcumb = Res("m_cumb")
    k.op("dve", lambda e: e.tensor_copy(out=mask_b[:], in_=mask[:]), reads=[rmask], writes=[rmaskb])
    k.op("dve", lambda e: e.tensor_tensor(out=cum_b[:], in0=src[:], in1=mask[:], op=ALU.subtract), reads=[rsrc, rmask], writes=[rcumb])
    tlt_b = k.sb("m_tltb", [128, 128], BF16); rtltb = Res("m_tltb")
    k.op("dve", lambda e: e.tensor_copy(out=tlt_b[:], in_=g.tri_lt[:]), reads=[g.r_tri_lt], writes=[rtltb])
    b0 = BP.take()
    k.op("pe", lambda e: e.matmul(BP.t[:, b0, :], lhsT=tlt_b[:], rhs=mask_b[:].rearrange("p j e -> p (j e)"), start=True, stop=False),
         reads=[rtltb, rmaskb], writes=[BP.r[b0]], inc=False)
    k.op("pe", lambda e: e.matmul(BP.t[:, b0, :], lhsT=g.ones_b[:], rhs=cum_b[:].rearrange("p j e -> p (j e)"), start=False, stop=True),
         reads=[g.r_ones_b, rcumb], writes=[BP.r[b0]])
    rho = k.sb("m_rho", A3, F32); rrho = Res("m_rho")
    k.op("dve", lambda e: e.scalar_tensor_tensor(out=rho[:].rearrange("p j e -> p (j e)"), in0=BP.t[:, b0, :], scalar=1.0,
                                                 in1=mask[:].rearrange("p j e -> p (j e)"), op0=ALU.add, op1=ALU.mult), reads=[BP.r[b0], rmask], writes=[rrho])
    k.op("dve", lambda e: e.tensor_scalar(out=rho[:], in0=rho[:], scalar1=-1.0, scalar2=None, op0=ALU.add), reads=[rrho], writes=[rrho])
    Rbig = k.sb("m_R", [128, NE * NT * 4 + 32], BF16); rR = Res("m_R")
    k.op("pool", lambda e: e.memset(Rbig[:], 0.0), writes=[rR])
    Rall = Rbig[:, 0:NE * NT * 4].rearrange("p (e j r) -> p e j r", e=NE, j=NT)
    ahi = k.sb("m_ahi", A3, BF16); rahi = Res("m_ahi")
    alo = k.sb("m_alo", A3, F32); ralo = Res("m_alo")
    jf = k.sb("m_jf", [128, NT], F32); rjf = Res("m_jf")
    k.op("pool", lambda e: e.iota(jf[:], pattern=[[1, NT]], base=0, channel_multiplier=0, allow_small_or_imprecise_dtypes=True), writes=[rjf])
    k.op("dve", lambda e: e.tensor_copy(out=ahi[:], in_=aff[:]), reads=[g.r_aff], writes=[rahi])
    k.op("dve", lambda e: e.tensor_tensor(out=alo[:], in0=aff[:], in1=ahi[:], op=ALU.subtract), reads=[g.r_aff, rahi], writes=[ralo])
    k.op("dve", lambda e: e.tensor_copy(out=Rall[:, :, :, 0], in_=g.pidx[:, 0:1].unsqueeze(2).to_broadcast([128, NE, NT])), reads=[g.r_pidx], writes=[rR])
    k.op("dve", lambda e: e.tensor_copy(out=Rall[:, :, :, 1], in_=jf[:].unsqueeze(1).to_broadcast([128, NE, NT])), reads=[rjf], writes=[rR])
    k.op("dve", lambda e: e.tensor_copy(out=Rall[:, :, :, 2], in_=ahi[:].rearrange("p j e -> p e j")), reads=[rahi], writes=[rR])
    k.op("dve", lambda e: e.tensor_copy(out=Rall[:, :, :, 3], in_=alo[:].rearrange("p j e -> p e j")), reads=[ralo], writes=[rR])
    idxf = k.sb("m_idxf", [128, NE, 4, 4], F32); ridxf = Res("m_idxf")
    tokf = k.sb("m_tokf", [128, NE, 4], F32); rtokf = Res("m_tokf")
    idxi = k.sb("m_idxi", [128, NE, 4], I32); ridxi = Res("m_idxi")
    gat = k.sb("m_gat", [128, NE, 4], F32); rgat = Res("m_gat")
    NSEL = 6
    sel = [k.sb("m_sel%d" % i, [128, CAP], BF16) for i in range(NSEL)]; rsel = [Res("m_sel%d" % i) for i in range(NSEL)]
    res4 = [k.sb("m_res%d" % i, [4, CAP], F32) for i in range(2)]; rres4 = [Res("m_res%d" % i) for i in range(2)]
    nsel = 0
    res32 = [k.sb("m_res%d" % i, [32, CAP], F32) for i in range(2)]; rres32 = [Res("m_res%d" % i) for i in range(2)]
    for ei in range(NE):
        b1 = BP.take()
        for j in range(NT):
            si = nsel % NSEL; nsel += 1
            k.op("dve", lambda e: e.tensor_scalar(out=sel[si][:], in0=g.iota_c16[:], scalar1=rho[:, j, ei:ei + 1], scalar2=None, op0=ALU.is_equal),
                 reads=[g.r_iota_c16, rrho], writes=[rsel[si]])
            off = (ei * NT + j) * 4
            k.op("pe", lambda e: e.matmul(BP.t[0:32, b1, :], lhsT=Rbig[:, off:off + 32], rhs=sel[si][:], start=(j == 0), stop=(j == NT - 1)),
                 reads=[rR, rsel[si]], writes=[BP.r[b1]], inc=True)
        s = ei % 2
        k.op("act", lambda e: e.copy(out=res32[s][:], in_=BP.t[0:32, b1, :]), reads=[BP.r[b1]], writes=[rres32[s]])
        b2 = BP.take()
        for q in range(4):
            k.op("pe", lambda e: e.transpose(out=BP.t[:, b2, q * 32:(q + 1) * 32], in_=res32[s][:, q * 128:(q + 1) * 128], identity=g.ident_f[0:32, 0:32]),
                 reads=[rres32[s], g.r_ident_f], writes=[BP.r[b2]], inc=(q == 3))
        k.op("act", lambda e: e.copy(out=idxf[:, ei], in_=BP.t[:, b2, 0:128].rearrange("p (q c) -> p q c", q=4)[:, :, 0:4]), reads=[BP.r[b2]], writes=[ridxf])
    k.op("dve", lambda e: e.scalar_tensor_tensor(out=tokf[:], in0=idxf[:, :, :, 1], scalar=128.0, in1=idxf[:, :, :, 0], op0=ALU.mult, op1=ALU.add),
         reads=[ridxf], writes=[rtokf])
    k.op("dve", lambda e: e.tensor_scalar(out=tokf[:], in0=tokf[:], scalar1=0.0, scalar2=float(T - 1), op0=ALU.max, op1=ALU.min), reads=[rtokf], writes=[rtokf])
    k.op("dve", lambda e: e.tensor_copy(out=idxi[:], in_=tokf[:]), reads=[rtokf], writes=[ridxi])
    k.op("dve", lambda e: e.tensor_tensor(out=gat[:], in0=idxf[:, :, :, 2], in1=idxf[:, :, :, 3], op=ALU.add), reads=[ridxf], writes=[rgat])
    xs = [[k.sb("m_xs%d_%d" % (i, bb_), [128, D], BF16) for i in range(4)] for bb_ in range(2)]
    rxs = [[Res("m_xs%d_%d" % (i, bb_)) for i in range(4)] for bb_ in range(2)]
    xsT = [k.sb("m_xsT%d" % bb_, [128, 8, CAP], BF16) for bb_ in range(2)]; rxsT = [Res("m_xsT%d" % bb_) for bb_ in range(2)]
    hidT = k.sb("m_hidT", [128, 8, CAP], BF16); rhidT = Res("m_hidT")
    sg = [k.sb("m_sg%d" % i, [128, CAP], F32) for i in range(2)]; rsg = [Res("m_sg%d" % i) for i in range(2)]
    osb = [k.sb("m_osb%d" % i, [128, D], F32) for i in range(2)]; rosb = [Res("m_osb%d" % i) for i in range(2)]

    def gathers(ei):
        b_ = ei % 2
        for q in range(4):
            k.dma_raw("pool", lambda e: e.indirect_dma_start(out=xs[b_][q][:], out_offset=None, in_=g.xn2[:, :],
                                                              in_offset=bass.IndirectOffsetOnAxis(ap=idxi[:, ei, q:q + 1], axis=0)),
                      reads=[ridxi, g.r_xn2], writes=[rxs[b_][q]])

    def transposes(ei):
        b_ = ei % 2
        for q in range(4):
            b3 = BP.take()
            pfv = BP.t[:, b3, :].bitcast(BF16)
            for kc in range(8):
                k.op("pe", lambda e: e.transpose(out=pfv[:, kc * 128:(kc + 1) * 128], in_=xs[b_][q][:, kc * 128:(kc + 1) * 128], identity=g.ident_b[:]),
                     reads=[rxs[b_][q], g.r_ident_b], writes=[BP.r[b3]], inc=(kc == 7))
            k.op("act", lambda e: e.copy(out=xsT[b_][:, :, q * 128:(q + 1) * 128], in_=pfv.rearrange("p (a b) -> p a b", a=8)), reads=[BP.r[b3]], writes=[rxsT[b_]])

    gathers(0)
    transposes(0)
    nos = 0
    for ei in range(NE):
        b = ei % 2
        if ei + 1 < NE:
            load_w(ei + 1)
            gathers(ei + 1)
        for fc in range(8):
            b4 = BP.take(2)
            for wi in range(2):
                for kc in range(8):
                    k.op("pe", lambda e: e.matmul(BP.t[:, b4 + wi, :], lhsT=Wt[b][wi][:, kc, fc * 128:(fc + 1) * 128], rhs=xsT[b][:, kc, :],
                                                  start=(kc == 0), stop=(kc == 7)), reads=[rWt[b][wi], rxsT[b]], writes=[BP.r[b4 + wi]], inc=(kc == 7))
            s2 = fc % 2
            k.op("act", lambda e: e.activation(out=sg[s2][:], in_=BP.t[:, b4, :], func=AF.Silu), reads=[BP.r[b4]], writes=[rsg[s2]])
            k.op("dve", lambda e: e.tensor_tensor(out=hidT[:, fc, :], in0=sg[s2][:], in1=BP.t[:, b4 + 1, :], op=ALU.mult),
                 reads=[rsg[s2], BP.r[b4 + 1]], writes=[rhidT])
        for q in range(4):
            so = nos % 2; nos += 1
            b5 = BP.take(2)
            for half in range(2):
                for fc in range(8):
                    k.op("pe", lambda e: e.matmul(BP.t[:, b5 + half, :], lhsT=hidT[:, fc, q * 128:(q + 1) * 128], rhs=Wt[b][2][:, fc, half * 512:(half + 1) * 512],
                                                  start=(fc == 0), stop=(fc == 7)), reads=[rhidT, rWt[b][2]], writes=[BP.r[b5 + half]], inc=(fc == 7))
            k.op("dve", lambda e: e.tensor_scalar(out=osb[so][:].rearrange("p (a b) -> p a b", a=2), in0=BP.t[:, b5:b5 + 2, :],
                                                  scalar1=gat[:, ei, q:q + 1], scalar2=None, op0=ALU.mult), reads=[BP.r[b5], BP.r[b5 + 1], rgat], writes=[rosb[so]])
            k.dma_raw("pool", lambda e: e.indirect_dma_start(out=g.x_cur[:, :], out_offset=bass.IndirectOffsetOnAxis(ap=idxi[:, ei, q:q + 1], axis=0),
                                                              in_=osb[so][:], in_offset=None, compute_op=ALU.add),
                      reads=[ridxi, rosb[so]], writes=[g.r_x])
        if ei + 1 < NE:
            transposes(ei + 1)
    k.scope_end()


def phase_final(k, g):
    k.scope_begin()
    wb = k.sb("f_nwb", [128, D], F32); rwb = Res("f_nwb")
    k.dma("sp", wb[:], bcast_rows(g.P["norm_final"], 128), writes=[rwb])
    g.nrm_sq = k.sb("f_sq", [128, D], F32); g.r_nrm_sq = Res("f_sq")
    g.nrm_ss = [k.sb("f_ss%d" % i, [128, 4], F32) for i in range(2)]; g.r_nrm_ss = [Res("f_ss%d" % i) for i in range(2)]
    xt = [k.sb("f_xt%d" % i, [128, D], F32) for i in range(2)]; rxt = [Res("f_xt%d" % i) for i in range(2)]
    ot = [k.sb("f_ot%d" % i, [128, D], F32) for i in range(2)]; rot = [Res("f_ot%d" % i) for i in range(2)]
    for j in range(NT):
        s = j % 2
        k.dma("sp", xt[s][:], g.x_cur[j * 128:(j + 1) * 128, :], reads=[g.r_x], writes=[rxt[s]])
        rmsnorm_tile(k, g, xt[s][:], rxt[s], wb[:], rwb, ot[s][:], rot[s], j)
        k.dma("sp", g.out[j * 128:(j + 1) * 128, :], ot[s][:], reads=[rot[s]], writes=[g.r_out])
    k.scope_end()
```

```python
import numpy as np
import concourse.bass as bass
import concourse.mybir as mybir
from concourse.bass_utils import run_bass_kernel_spmd

F32 = mybir.dt.float32
BF16 = mybir.dt.bfloat16
I32 = mybir.dt.int32
U32 = mybir.dt.uint32
AF = mybir.ActivationFunctionType
ALU = mybir.AluOpType
AX = mybir.AxisListType


class Res:
    __slots__ = ("name", "w", "r")

    def __init__(self, name):
        self.name = name
        self.w = None
        self.r = {}


class KB:
    NDMA = 32
    NHW = 20

    def __init__(self, nc, needed=None):
        self.nc = nc
        self.needed = needed
        self.used = {}
        self.rank = None
        if needed is not None:
            self.rank = {e: {v: i + 1 for i, v in enumerate(sorted(vs))} for e, vs in needed.items()}
        self.eng = {"pe": nc.tensor, "act": nc.scalar, "dve": nc.vector, "pool": nc.gpsimd, "sp": nc.sync}
        self.sem = {}
        self.cnt = {}
        self.pending = {}
        self._ctx = []
        for e in self.eng:
            s = nc.semaphore("s_" + e)
            self.sem[e] = s.__enter__()
            self._ctx.append(s)
            self.cnt[e] = 0
            self.pending[e] = False
        self.dsem = []
        self.dtarget = []
        for i in range(self.NDMA):
            s = nc.semaphore("d_%d" % i)
            self.dsem.append(s.__enter__())
            self._ctx.append(s)
            self.dtarget.append(0)
        self.drr = 0
        self.drr_sw = self.NHW
        self.waited = {e: {} for e in self.eng}
        self.ninst = 0
        self.outputs_tokens = []

    def sb(self, name, shape, dt):
        self._uid = getattr(self, "_uid", 0) + 1
        name = "%s_u%d" % (name, self._uid)
        g = self.nc.sbuf_tensor(name, list(shape), dt)
        t = g.__enter__()
        self._ctx.append(g)
        return t

    def ps(self, name, shape, dt):
        self._uid = getattr(self, "_uid", 0) + 1
        name = "%s_u%d" % (name, self._uid)
        g = self.nc.psum_tensor(name, list(shape), dt)
        t = g.__enter__()
        self._ctx.append(g)
        return t

    def _wait(self, e, tok):
        kind, key, val = tok
        if kind == "c":
            if key == e and e == "pe":
                return
            semkey = ("c", key)
            sem = self.sem[key]
        else:
            semkey = ("d", key)
            sem = self.dsem[key]
        if self.waited[e].get(semkey, 0) >= val:
            return
        self.waited[e][semkey] = val
        if kind == "c":
            self.used.setdefault(key, set()).add(val)
            if self.rank is not None:
                val = self.rank[key][val]
        self.eng[e].wait_ge(sem, val)
        self.ninst += 1

    def _deps(self, e, reads, writes):
        toks = []
        for r in reads:
            if r.w is not None:
                toks.append(r.w)
        for w in writes:
            if w.w is not None:
                if not (w.w[0] == "c" and w.w[1] == e and e != "pool"):
                    toks.append(w.w)
            for k, t in w.r.items():
                if t[0] == "c" and t[1] == e and e != "pool":
                    continue
                toks.append(t)
        return toks

    def _record(self, tok, reads, writes):
        for r in reads:
            k = (tok[0], tok[1])
            old = r.r.get(k)
            if old is None or old[2] < tok[2]:
                r.r[k] = tok
        for w in writes:
            w.w = tok
            w.r = {}

    def op(self, e, fn, reads=(), writes=(), inc=True):
        for t in self._deps(e, reads, writes):
            self._wait(e, t)
        ins = fn(self.eng[e])
        self.ninst += 1
        if inc:
            self.cnt[e] += 1
            if self.rank is None or self.cnt[e] in self.rank.get(e, {}):
                ins.then_inc(self.sem[e], 1)
                self.nincs = getattr(self, "nincs", 0) + 1
            self.pending[e] = False
            tok = ("c", e, self.cnt[e])
        else:
            self.pending[e] = True
            tok = ("c", e, self.cnt[e] + 1)
        self._record(tok, reads, writes)
        return tok

    def dma(self, q, out, in_, reads=(), writes=(), **kw):
        for t in self._deps(q, reads, writes):
            self._wait(q, t)
        if q == "pool":
            slot = self.drr_sw
            self.drr_sw = self.NHW + (self.drr_sw - self.NHW + 1) % (self.NDMA - self.NHW)
        else:
            slot = self.drr
            self.drr = (self.drr + 1) % self.NHW
        if self.dtarget[slot] > 0:
            self._wait(q, ("d", slot, self.dtarget[slot]))
        self.dtarget[slot] += 16
        ins = self.eng[q].dma_start(out=out, in_=in_, **kw)
        ins.then_inc(self.dsem[slot], 16)
        self.ninst += 1
        tok = ("d", slot, self.dtarget[slot])
        self._record(tok, reads, writes)
        return tok

    def dma_raw(self, q, fn, reads=(), writes=()):
        for t in self._deps(q, reads, writes):
            self._wait(q, t)
        if q == "pool":
            slot = self.drr_sw
            self.drr_sw = self.NHW + (self.drr_sw - self.NHW + 1) % (self.NDMA - self.NHW)
        else:
            slot = self.drr
            self.drr = (self.drr + 1) % self.NHW
        if self.dtarget[slot] > 0:
            self._wait(q, ("d", slot, self.dtarget[slot]))
        self.dtarget[slot] += 16
        ins = fn(self.eng[q])
        ins.then_inc(self.dsem[slot], 16)
        self.ninst += 1
        tok = ("d", slot, self.dtarget[slot])
        self._record(tok, reads, writes)
        return tok

    def finish(self, final_res):
        for r in final_res:
            if r.w is not None:
                self._wait("sp", r.w)
        for slot in range(self.NDMA):
            if self.dtarget[slot] > 0:
                self._wait("sp", ("d", slot, self.dtarget[slot]))

    def close(self):
        for g in reversed(self._ctx):
            g.__exit__(None, None, None)
        self._ctx = []


def _kb_barrier(self):
    for e in self.eng:
        assert not self.pending[e], e
    for e in self.eng:
        for e2 in self.eng:
            if e2 != e and self.cnt[e2] > 0:
                self._wait(e, ("c", e2, self.cnt[e2]))
        for slot in range(self.NDMA):
            if self.dtarget[slot] > 0:
                self._wait(e, ("d", slot, self.dtarget[slot]))


def _kb_scope_begin(self):
    self._marks = getattr(self, "_marks", [])
    self._marks.append(len(self._ctx))


def _kb_scope_end(self):
    self.barrier()
    m = self._marks.pop()
    while len(self._ctx) > m:
        g = self._ctx.pop()
        g.__exit__(None, None, None)


KB.barrier = _kb_barrier
KB.scope_begin = _kb_scope_begin
KB.scope_end = _kb_scope_end

D = 1024
T = 4096
NT = T // 128
DEPTH = 2
IN_COLS = 3344
NE = 16
CAP = 512
EPS = 1e-6
C_RQ, C_RK, C_RV, C_RG = 0, 256, 512, 768
C_WR, C_WK, C_WV, C_WXW, C_WXA, C_WXG = 1024, 1280, 1536, 1792, 1856, 1920
C_LX, C_LG = 2048, 2304
C_GQ, C_GK, C_GV, C_GOG, C_GXA = 2560, 2688, 2816, 3072, 3328

PARAM_SHAPES = {
    "norm_mix": (2, 1024), "w_in": (2, 1024, 3344), "w_out": (2, 1024, 1024),
    "ret_log_decay": (2, 2, 4), "ret_gn": (2, 256),
    "rwkv_mu_rkv": (2, 3, 256), "rwkv_mu_w": (2, 64), "rwkv_mu_a": (2, 64),
    "rwkv_w0": (2, 2, 256), "rwkv_w_up": (2, 2, 64, 256), "rwkv_a0": (2, 2, 256),
    "rwkv_a_up": (2, 2, 64, 256), "rwkv_g_up": (2, 128, 256), "rwkv_k_k": (2, 256),
    "rwkv_k_a": (2, 256), "rwkv_r_k": (2, 4, 64), "rwkv_gn": (2, 256),
    "lru_conv_w": (2, 4, 256), "lru_conv_b": (2, 256), "lru_gate_w": (2, 2, 2, 4, 64, 64),
    "lru_gate_b": (2, 2, 2, 256), "lru_lambda": (2, 2, 256),
    "gla_alpha_up": (2, 2, 16, 128), "gla_alpha_b": (2, 2, 128), "gla_gn": (2, 256),
    "norm_ffn": (2, 1024), "router_w": (2, 1024, 16), "router_b": (2, 16),
    "exp_w_gate": (2, 16, 1024, 1024), "exp_w_up": (2, 16, 1024, 1024),
    "exp_w_down": (2, 16, 1024, 1024), "norm_final": (1024,),
}


class Ctx:
    pass


def make_consts(k, g):
    nc = k.nc
    g.ident_f = k.sb("ident_f", [128, 128], F32); g.r_ident_f = Res("ident_f")
    k.op("pool", lambda e: e.memset(g.ident_f[:], 0.0), writes=[g.r_ident_f])
    k.op("pool", lambda e: e.affine_select(out=g.ident_f[:], in_=g.ident_f[:], pattern=[[-1, 128]],
                                           compare_op=ALU.not_equal, fill=1.0, base=0, channel_multiplier=1),
         reads=[g.r_ident_f], writes=[g.r_ident_f])
    g.ident_b = k.sb("ident_b", [128, 128], BF16); g.r_ident_b = Res("ident_b")
    k.op("dve", lambda e: e.tensor_copy(out=g.ident_b[:], in_=g.ident_f[:]), reads=[g.r_ident_f], writes=[g.r_ident_b])
    g.ones_f = k.sb("ones_f", [128, 128], F32); g.r_ones_f = Res("ones_f")
    k.op("pool", lambda e: e.memset(g.ones_f[:], 1.0), writes=[g.r_ones_f])
    g.ones_b = k.sb("ones_b", [128, 128], BF16); g.r_ones_b = Res("ones_b")
    k.op("pool", lambda e: e.memset(g.ones_b[:], 1.0), writes=[g.r_ones_b])

    def tri(name, pattern, cm, cmp):
        t = k.sb(name, [128, 128], F32); r = Res(name)
        k.op("pool", lambda e: e.memset(t[:], 1.0), writes=[r])
        k.op("pool", lambda e: e.affine_select(out=t[:], in_=t[:], pattern=pattern, compare_op=cmp, fill=0.0,
                                               base=0, channel_multiplier=cm), reads=[r], writes=[r])
        return t, r
    g.tri_le, g.r_tri_le = tri("tri_le", [[1, 128]], -1, ALU.is_ge)
    g.tri_lt, g.r_tri_lt = tri("tri_lt", [[1, 128]], -1, ALU.is_gt)
    g.tri_ge, g.r_tri_ge = tri("tri_ge", [[-1, 128]], 1, ALU.is_ge)
    g.tri_gt, g.r_tri_gt = tri("tri_gt", [[-1, 128]], 1, ALU.is_gt)
    g.dmat = k.sb("dmat", [128, 128], F32); g.r_dmat = Res("dmat")
    k.op("pool", lambda e: e.iota(g.dmat[:], pattern=[[1, 128]], base=0, channel_multiplier=-1,
                                  allow_small_or_imprecise_dtypes=True), writes=[g.r_dmat])
    g.iota_c = k.sb("iota_c", [128, 512], F32); g.r_iota_c = Res("iota_c")
    k.op("pool", lambda e: e.iota(g.iota_c[:], pattern=[[1, 512]], base=0, channel_multiplier=0,
                                  allow_small_or_imprecise_dtypes=True), writes=[g.r_iota_c])
    g.iota_c16 = k.sb("iota_c16", [128, 512], mybir.dt.int16); g.r_iota_c16 = Res("iota_c16")
    k.op("pool", lambda e: e.iota(g.iota_c16[:], pattern=[[1, 512]], base=0, channel_multiplier=0), writes=[g.r_iota_c16])
    g.pidx = k.sb("pidx", [128, 1], F32); g.r_pidx = Res("pidx")
    k.op("pool", lambda e: e.iota(g.pidx[:], pattern=[[0, 1]], base=0, channel_multiplier=1,
                                  allow_small_or_imprecise_dtypes=True), writes=[g.r_pidx])
    g.eps_c = k.sb("eps_c", [128, 1], F32); g.r_eps = Res("eps_c")
    k.op("pool", lambda e: e.memset(g.eps_c[:], EPS), writes=[g.r_eps])
    g.one_c = k.sb("one_c", [128, 1], F32); g.r_one = Res("one_c")
    k.op("pool", lambda e: e.memset(g.one_c[:], 1.0), writes=[g.r_one])


def rmsnorm_tile(k, g, xt, rx, wb, rwb, out_bf, rout, tagi):
    sq = g.nrm_sq; ss = g.nrm_ss[tagi % 2]; rss = g.r_nrm_ss[tagi % 2]
    k.op("act", lambda e: e.activation(out=sq[:], in_=xt, func=AF.Square, accum_out=ss[:, 0:1]),
         reads=[rx], writes=[g.r_nrm_sq, rss])
    k.op("act", lambda e: e.activation(out=ss[:, 1:2], in_=ss[:, 0:1], func=AF.Sqrt, scale=1.0 / D, bias=g.eps_c[:, 0:1]),
         reads=[rss, g.r_eps], writes=[rss])
    k.op("dve", lambda e: e.reciprocal(out=ss[:, 2:3], in_=ss[:, 1:2]), reads=[rss], writes=[rss])
    k.op("dve", lambda e: e.scalar_tensor_tensor(out=out_bf, in0=xt, scalar=ss[:, 2:3], in1=wb,
                                                 op0=ALU.mult, op1=ALU.mult), reads=[rx, rss, rwb], writes=[rout])


def bcast_rows(ap_row, nparts):
    if len(ap_row.shape) == 1:
        ap_row = ap_row.unsqueeze(0)
    return ap_row.to_broadcast([nparts] + list(ap_row.shape[1:]))


TM_CHUNKS = [(0, 512), (512, 512), (1024, 512), (1536, 512), (2560, 512), (3072, 272)]


def phase_inproj(k, g, l):
    k.scope_begin()
    P = g.P
    W = k.sb("w_in_bf", [128, 8, IN_COLS], BF16)
    rW = [Res("w_in%d" % i) for i in range(8)]
    src = P["w_in"][l].rearrange("(kc p) c -> p kc c", p=128)
    for kc in range(8):
        k.dma("pool", W[:, kc, :], src[:, kc, :], writes=[rW[kc]], max_dma_last_dim=4096)
    wb = k.sb("nw_b", [128, D], F32); rwb = Res("nw_b")
    k.dma("sp", wb[:], bcast_rows(P["norm_mix"][l], 128), writes=[rwb])
    g.nrm_sq = k.sb("nrm_sq", [128, D], F32); g.r_nrm_sq = Res("nrm_sq")
    g.nrm_ss = [k.sb("nrm_ss%d" % i, [128, 4], F32) for i in range(2)]
    g.r_nrm_ss = [Res("nrm_ss%d" % i) for i in range(2)]
    xt = [k.sb("xt%d" % i, [128, D], F32) for i in range(2)]; rxt = [Res("xt%d" % i) for i in range(2)]
    xn = [k.sb("xn%d" % i, [128, D], BF16) for i in range(4)]; rxn = [Res("xn%d" % i) for i in range(4)]
    hT = [k.sb("hT%d" % i, [128, 8, 512], BF16) for i in range(2)]
    rhT = [[Res("hT%d_%d" % (i, t)) for t in range(4)] for i in range(2)]
    ptr = [k.ps("ptr%d" % i, [128, 8, 128], BF16) for i in range(2)]; rptr = [Res("ptr%d" % i) for i in range(2)]
    pj = [k.ps("pj%d" % i, [128, 512], F32) for i in range(4)]; rpj = [Res("pj%d" % i) for i in range(4)]
    stage = [k.sb("stage%d" % i, [128, IN_COLS], F32) for i in range(2)]; rstage = [Res("stage%d" % i) for i in range(2)]
    stT = [k.sb("stT%d" % i, [128, 512], F32) for i in range(2)]; rstT = [Res("stT%d" % i) for i in range(2)]
    cntr = {'npj': 0, 'nst': 0}

    def prep_a(gi):
        for ti in range(4):
            j = gi * 4 + ti
            s = j % 2
            k.dma("sp", xt[s][:], g.x_src[j * 128:(j + 1) * 128, :], reads=[g.r_x], writes=[rxt[s]])
            rmsnorm_tile(k, g, xt[s][:], rxt[s], wb[:], rwb, xn[ti][:], rxn[ti], j)

    def prep_b(gi):
        hs = gi % 2
        for ti in range(4):
            j = gi * 4 + ti
            s = j % 2
            for kc in range(8):
                k.op("pe", lambda e: e.transpose(out=ptr[s][:, kc, :], in_=xn[ti][:, kc * 128:(kc + 1) * 128],
                                                 identity=g.ident_b[:]),
                     reads=[rxn[ti], g.r_ident_b], writes=[rptr[s]], inc=(kc == 7))
            k.op("act", lambda e: e.copy(out=hT[hs][:, :, ti * 128:(ti + 1) * 128], in_=ptr[s][:]),
                 reads=[rptr[s]], writes=[rhT[hs][ti]])

    def mm(gi):
        hs = gi % 2
        for ti in range(4):
            j = gi * 4 + ti
            ss = j % 2
            for ci, (c0, cw) in enumerate(TM_CHUNKS):
                pi = cntr['npj'] % 4; cntr['npj'] += 1
                for kc in range(8):
                    k.op("pe", lambda e: e.matmul(pj[pi][:, 0:cw], lhsT=hT[hs][:, kc, ti * 128:(ti + 1) * 128],
                                                  rhs=W[:, kc, c0:c0 + cw], start=(kc == 0), stop=(kc == 7)),
                         reads=[rhT[hs][ti], rW[kc]], writes=[rpj[pi]], inc=(kc == 7))
                ev = "act" if (ci % 2 == 0) else "dve"
                if ev == "act":
                    k.op("act", lambda e: e.copy(out=stage[ss][:, c0:c0 + cw], in_=pj[pi][:, 0:cw]),
                         reads=[rpj[pi]], writes=[rstage[ss]])
                else:
                    k.op("dve", lambda e: e.tensor_copy(out=stage[ss][:, c0:c0 + cw], in_=pj[pi][:, 0:cw]),
                         reads=[rpj[pi]], writes=[rstage[ss]])
            k.dma("sp", g.proj[j * 128:(j + 1) * 128, 0:2048], stage[ss][:, 0:2048], reads=[rstage[ss]], writes=[g.r_proj])
            k.dma("sp", g.proj[j * 128:(j + 1) * 128, 2560:IN_COLS], stage[ss][:, 2560:IN_COLS], reads=[rstage[ss]], writes=[g.r_proj2])
        for fc in range(4):
            pi = cntr['npj'] % 4; cntr['npj'] += 1
            for kc in range(8):
                k.op("pe", lambda e: e.matmul(pj[pi][:, :], lhsT=W[:, kc, C_LX + fc * 128:C_LX + (fc + 1) * 128],
                                              rhs=hT[hs][:, kc, :], start=(kc == 0), stop=(kc == 7)),
                     reads=rhT[hs] + [rW[kc]], writes=[rpj[pi]], inc=(kc == 7))
            s2 = cntr['nst'] % 2; cntr['nst'] += 1
            k.op("dve", lambda e: e.tensor_copy(out=stT[s2][:], in_=pj[pi][:]), reads=[rpj[pi]], writes=[rstT[s2]])
            k.dma("sp", g.lruT[fc * 128:(fc + 1) * 128, gi * 512:(gi + 1) * 512], stT[s2][:], reads=[rstT[s2]], writes=[g.r_lruT])
    prep_a(0)
    prep_b(0)
    for gi in range(NT // 4):
        if gi + 1 < NT // 4:
            prep_a(gi + 1)
        mm(gi)
        if gi + 1 < NT // 4:
            prep_b(gi + 1)
    k.scope_end()


def build_program(debug=False, stop_after=None, which=("lru", "ret", "gla", "rwkv")):
    needed = None
    for _pass in range(2):
        nc, g, k = _build_once(debug, stop_after, which, needed)
        needed = k.used
    g.nincs = getattr(k, "nincs", 0)
    return nc, g


def _build_once(debug, stop_after, which, needed):
    nc = bass.Bass("TRN2", target_bir_lowering=False)
    k = KB(nc, needed)
    g = Ctx()
    g.debug = debug
    g.which = which
    g.x_in = nc.dram_tensor("x", [T, D], F32, kind="ExternalInput").ap()
    g.pos_in = nc.dram_tensor("positions", [T], I32, kind="ExternalInput").ap()
    g.P = {}
    for name, shp in PARAM_SHAPES.items():
        g.P[name] = nc.dram_tensor(name, list(shp), F32, kind="ExternalInput").ap()
    g.out = nc.dram_tensor("out", [T, D], F32, kind="ExternalOutput").ap()
    sk = "ExternalOutput" if debug else "Internal"
    g.x_cur = nc.dram_tensor("x_cur", [T, D], F32, kind=sk).ap(); g.r_x = Res("x_cur")
    g.proj = nc.dram_tensor("proj", [T, IN_COLS], F32, kind=sk).ap(); g.r_proj = Res("proj"); g.r_proj2 = Res("proj2")
    g.lruT = nc.dram_tensor("lruT", [512, T], F32, kind=sk).ap(); g.r_lruT = Res("lruT")
    g.mixedT = nc.dram_tensor("mixedT", [D, T], BF16, kind=sk).ap()
    g.r_mixedT = [Res("mixedT%d" % i) for i in range(4)]
    g.xn2 = nc.dram_tensor("xn2", [T, D], BF16, kind=sk).ap(); g.r_xn2 = Res("xn2")

    make_consts(k, g)
    g.aff_all = k.sb("aff_all", [128, NT, NE], F32); g.r_aff = Res("aff_all")
    g.r_out = Res("out")
    g.x_src = g.x_in
    for l in range(DEPTH):
        phase_inproj(k, g, l)
        if stop_after == ("inproj", l):
            break
        if "lru" in g.which: phase_lru(k, g, l)
        if "ret" in g.which: phase_ret(k, g, l)
        if "gla" in g.which: phase_gla(k, g, l)
        if "rwkv" in g.which: phase_rwkv(k, g, l)
        if stop_after == ("mix", l):
            break
        phase_outproj_router(k, g, l)
        if stop_after == ("outproj", l):
            break
        phase_moe(k, g, l)
        if stop_after == ("moe", l):
            break
    if stop_after is None:
        phase_final(k, g)
    k.barrier()
    k.finish([])
    k.close()
    g.ninst = k.ninst
    return nc, g, k


def make_in_maps(inputs, cores):
    maps = []
    for b in cores:
        m = {"x": np.ascontiguousarray(inputs["x"][b]), "positions": np.ascontiguousarray(inputs["positions"][b]).astype(np.int32)}
        for name in PARAM_SHAPES:
            m[name] = np.ascontiguousarray(inputs[name])
        maps.append(m)
    return maps


def kernel(**inputs):
    nc, g = build_program()
    in_maps = make_in_maps(inputs, list(range(8)))
    res = run_bass_kernel_spmd(nc, in_maps, core_ids=list(range(8)))
    out = np.stack([np.asarray(r["out"]) for r in res.results], axis=0)
    return out.astype(np.float32)


import math
PI = math.pi


def make_rope(k, g):
    k.scope_begin()
    posi = k.sb("posi", [128, 32], I32); rposi = Res("posi")
    k.dma("sp", posi[:], g.pos_in.rearrange("(j p) -> p j", p=128), writes=[rposi], allow_slow_non_contiguous=True)
    posf = k.sb("posf", [128, 32], F32); rposf = Res("posf")
    k.op("dve", lambda e: e.tensor_copy(out=posf[:], in_=posi[:]), reads=[rposi], writes=[rposf])
    fi = k.sb("fi", [128, 32], F32); rfi = Res("fi")
    k.op("pool", lambda e: e.iota(fi[:], pattern=[[1, 32]], base=0, channel_multiplier=0,
                                  allow_small_or_imprecise_dtypes=True), writes=[rfi])
    inv = k.sb("inv", [128, 32], F32); rinv = Res("inv")
    k.op("act", lambda e: e.activation(out=inv[:], in_=fi[:], func=AF.Exp, scale=-math.log(10000.0) / 32.0),
         reads=[rfi], writes=[rinv])
    ang = k.sb("ang", [128, 32, 32], F32); rang = Res("ang")
    k.op("dve", lambda e: e.tensor_tensor(out=ang[:], in0=posf[:].unsqueeze(2).to_broadcast([128, 32, 32]),
                                          in1=inv[:].unsqueeze(1).to_broadcast([128, 32, 32]), op=ALU.mult),
         reads=[rposf, rinv], writes=[rang])
    ni = k.sb("rp_ni", [128, 1024], I32); rni = Res("rp_ni")
    nf = k.sb("rp_nf", [128, 1024], F32); rnf = Res("rp_nf")
    y = k.sb("rp_y", [128, 1024], F32); ry = Res("rp_y")
    m = k.sb("rp_m", [128, 1024], F32); rm = Res("rp_m")
    a2 = k.sb("rp_a2", [128, 1024], F32); ra2 = Res("rp_a2")
    C1 = 6.28125
    C2 = 2.0 * PI - C1
    angf = ang[:].rearrange("p a b -> p (a b)")

    def reduce_sin(src, rsrc, dst, rdst, scale):
        k.op("dve", lambda e: e.tensor_scalar(out=ni[:], in0=src, scalar1=1.0 / (2.0 * PI), scalar2=None, op0=ALU.mult),
             reads=[rsrc], writes=[rni])
        k.op("dve", lambda e: e.tensor_copy(out=nf[:], in_=ni[:]), reads=[rni], writes=[rnf])
        k.op("dve", lambda e: e.scalar_tensor_tensor(out=y[:], in0=nf[:], scalar=-C1, in1=src, op0=ALU.mult, op1=ALU.add),
             reads=[rnf, rsrc], writes=[ry])
        k.op("dve", lambda e: e.scalar_tensor_tensor(out=y[:], in0=nf[:], scalar=-C2, in1=y[:], op0=ALU.mult, op1=ALU.add),
             reads=[rnf, ry], writes=[ry])
        k.op("dve", lambda e: e.tensor_scalar(out=m[:], in0=y[:], scalar1=PI, scalar2=-2.0 * PI, op0=ALU.is_gt, op1=ALU.mult),
             reads=[ry], writes=[rm])
        k.op("dve", lambda e: e.tensor_tensor(out=y[:], in0=y[:], in1=m[:], op=ALU.add), reads=[ry, rm], writes=[ry])
        k.op("dve", lambda e: e.tensor_scalar(out=m[:], in0=y[:], scalar1=-PI, scalar2=2.0 * PI, op0=ALU.is_lt, op1=ALU.mult),
             reads=[ry], writes=[rm])
        k.op("dve", lambda e: e.tensor_tensor(out=y[:], in0=y[:], in1=m[:], op=ALU.add), reads=[ry, rm], writes=[ry])
        k.op("dve", lambda e: e.tensor_scalar(out=y[:], in0=y[:], scalar1=-3.1415925, scalar2=3.1415925, op0=ALU.max, op1=ALU.min),
             reads=[ry], writes=[ry])
        k.op("act", lambda e: e.activation(out=dst, in_=y[:], func=AF.Sin), reads=[ry], writes=[rdst])

    reduce_sin(angf, rang, g.sinq[:].rearrange("p a b -> p (a b)"), g.r_rope, 1.0)
    k.op("dve", lambda e: e.tensor_scalar(out=a2[:], in0=angf, scalar1=PI / 2.0, scalar2=None, op0=ALU.add),
         reads=[rang], writes=[ra2])
    reduce_sin(a2[:], ra2, g.cosq[:].rearrange("p a b -> p (a b)"), g.r_rope, 1.0)
    k.op("dve", lambda e: e.tensor_scalar(out=g.sink[:], in0=g.sinq[:], scalar1=0.125, scalar2=None, op0=ALU.mult),
         reads=[g.r_rope], writes=[g.r_rope])
    k.op("dve", lambda e: e.tensor_scalar(out=g.cosk[:], in0=g.cosq[:], scalar1=0.125, scalar2=None, op0=ALU.mult),
         reads=[g.r_rope], writes=[g.r_rope])
    k.scope_end()


def phase_lru(k, g, l):
    k.scope_begin()
    P = g.P
    NB = 7
    buf = [k.sb("lb%d" % i, [128, T], F32) for i in range(NB)]
    rb = [Res("lb%d" % i) for i in range(NB)]
    X, XC, G0, G1, TMP, H0, H1 = range(7)
    xcb = k.sb("l_xcb", [128, T], BF16); rxcb = Res("l_xcb")
    oT = k.sb("l_oT", [128, T], BF16); roT = Res("l_oT")
    cw = k.sb("l_cw", [128, 4], F32); rcw = Res("l_cw")
    cb = k.sb("l_cb", [128, 1], F32); rcb = Res("l_cb")
    gb = k.sb("l_gb", [128, 4], F32); rgb = Res("l_gb")
    lam = k.sb("l_lam", [128, 2], F32); rlam = Res("l_lam")
    c1 = k.sb("l_c1", [128, 2], F32); rc1 = Res("l_c1")
    wst = k.sb("l_wst", [128, 4, 128], F32); rwst = Res("l_wst")
    wbd = k.sb("l_wbd", [128, 4, 128], BF16); rwbd = Res("l_wbd")
    pg = [k.ps("l_pg%d" % i, [128, 512], F32) for i in range(4)]; rpg = [Res("l_pg%d" % i) for i in range(4)]
    npg = 0
    for pt in range(2):
        ch0 = pt * 128
        k.dma("sp", cw[:], P["lru_conv_w"][l][:, ch0:ch0 + 128].rearrange("j c -> c j"), writes=[rcw], allow_slow_non_contiguous=True)
        k.dma("sp", cb[:], P["lru_conv_b"][l][ch0:ch0 + 128].unsqueeze(1), writes=[rcb], allow_slow_non_contiguous=True)
        k.dma("sp", gb[:], P["lru_gate_b"][l][:, :, ch0:ch0 + 128].rearrange("a b c -> c (a b)"), writes=[rgb], allow_slow_non_contiguous=True)
        k.dma("sp", lam[:], P["lru_lambda"][l][:, ch0:ch0 + 128].rearrange("a c -> c a"), writes=[rlam], allow_slow_non_contiguous=True)
        k.op("pool", lambda e: e.memset(wst[:], 0.0), writes=[rwst])
        for dr in range(2):
            for gt in range(2):
                for hh in range(2):
                    k.dma("sp", wst[hh * 64:(hh + 1) * 64, dr * 2 + gt, hh * 64:(hh + 1) * 64],
                          P["lru_gate_w"][l, dr, gt, 2 * pt + hh], writes=[rwst])
        k.op("dve", lambda e: e.tensor_copy(out=wbd[:], in_=wst[:]), reads=[rwst], writes=[rwbd])
        k.op("act", lambda e: e.activation(out=c1[:], in_=lam[:], func=AF.Exp, scale=-1.0), reads=[rlam], writes=[rc1])
        k.op("act", lambda e: e.activation(out=c1[:], in_=c1[:], func=AF.Ln, bias=g.one_c[:, 0:1]), reads=[rc1, g.r_one], writes=[rc1])
        k.op("dve", lambda e: e.tensor_scalar(out=c1[:], in0=c1[:], scalar1=-8.0, scalar2=None, op0=ALU.mult), reads=[rc1], writes=[rc1])
        k.dma("sp", buf[X][:], g.lruT[ch0:ch0 + 128, :], reads=[g.r_lruT], writes=[rb[X]])
        k.op("dve", lambda e: e.tensor_scalar(out=buf[XC][:], in0=buf[X][:], scalar1=cw[:, 2:3], scalar2=cb[:, 0:1],
                                              op0=ALU.mult, op1=ALU.add), reads=[rb[X], rcw, rcb], writes=[rb[XC]])
        for (j, so, do, n) in [(0, 0, 2, T - 2), (1, 0, 1, T - 1), (3, 1, 0, T - 1)]:
            k.op("dve", lambda e: e.scalar_tensor_tensor(out=buf[XC][:, do:do + n], in0=buf[X][:, so:so + n],
                                                         scalar=cw[:, j:j + 1], in1=buf[XC][:, do:do + n],
                                                         op0=ALU.mult, op1=ALU.add), reads=[rb[X], rb[XC], rcw], writes=[rb[XC]])
        k.op("act", lambda e: e.copy(out=xcb[:], in_=buf[XC][:]), reads=[rb[XC]], writes=[rxcb])
        k.dma("sp", buf[X][:], g.lruT[256 + ch0:256 + ch0 + 128, :], reads=[g.r_lruT], writes=[rb[X]])
        for dr in range(2):
            for gt in range(2):
                dst = G0 if gt == 0 else G1
                for tc in range(8):
                    pi = npg % 4; npg += 1
                    k.op("pe", lambda e: e.matmul(pg[pi][:], lhsT=wbd[:, dr * 2 + gt, :], rhs=xcb[:, tc * 512:(tc + 1) * 512],
                                                  start=True, stop=True), reads=[rwbd, rxcb], writes=[rpg[pi]])
                    k.op("act", lambda e: e.activation(out=buf[dst][:, tc * 512:(tc + 1) * 512], in_=pg[pi][:], func=AF.Sigmoid,
                                                       bias=gb[:, dr * 2 + gt:dr * 2 + gt + 1]), reads=[rpg[pi], rgb], writes=[rb[dst]])
            k.op("act", lambda e: e.activation(out=buf[G0][:], in_=buf[G0][:], func=AF.Exp, scale=c1[:, dr:dr + 1]),
                 reads=[rb[G0], rc1], writes=[rb[G0]])
            k.op("act", lambda e: e.activation(out=buf[TMP][:], in_=buf[G0][:], func=AF.Square),
                 reads=[rb[G0]], writes=[rb[TMP]])
            k.op("act", lambda e: e.activation(out=buf[TMP][:], in_=buf[TMP][:], func=AF.Sqrt, scale=-1.0, bias=g.one_c[:, 0:1]),
                 reads=[rb[TMP], g.r_one], writes=[rb[TMP]])
            k.op("dve", lambda e: e.tensor_tensor(out=buf[G1][:], in0=buf[G1][:], in1=buf[TMP][:], op=ALU.mult),
                 reads=[rb[G1], rb[TMP]], writes=[rb[G1]])
            k.op("dve", lambda e: e.tensor_tensor(out=buf[G1][:], in0=buf[G1][:], in1=buf[XC][:], op=ALU.mult),
                 reads=[rb[G1], rb[XC]], writes=[rb[G1]])
            if dr == 0:
                k.op("dve", lambda e: e.tensor_tensor_scan(out=buf[H0][:], data0=buf[G0][:], data1=buf[G1][:], initial=0.0,
                                                           op0=ALU.mult, op1=ALU.add), reads=[rb[G0], rb[G1]], writes=[rb[H0]])
            else:
                k.op("dve", lambda e: e.tensor_tensor_scan(out=buf[H1][:, ::-1], data0=buf[G0][:, ::-1], data1=buf[G1][:, ::-1],
                                                           initial=0.0, op0=ALU.mult, op1=ALU.add),
                     reads=[rb[G0], rb[G1]], writes=[rb[H1]])
        k.op("pool", lambda e: e.tensor_tensor(out=buf[H0][:, 0:1024], in0=buf[H0][:, 0:1024], in1=buf[H1][:, 0:1024], op=ALU.add),
             reads=[rb[H0], rb[H1]], writes=[rb[H0]])
        k.op("dve", lambda e: e.tensor_tensor(out=buf[H0][:, 1024:T], in0=buf[H0][:, 1024:T], in1=buf[H1][:, 1024:T], op=ALU.add),
             reads=[rb[H0], rb[H1]], writes=[rb[H0]])
        k.op("act", lambda e: e.activation(out=buf[X][:], in_=buf[X][:], func=AF.Gelu), reads=[rb[X]], writes=[rb[X]])
        k.op("dve", lambda e: e.tensor_tensor(out=oT[:], in0=buf[H0][:], in1=buf[X][:], op=ALU.mult),
             reads=[rb[H0], rb[X]], writes=[roT])
        k.dma("sp", g.mixedT[512 + ch0:512 + ch0 + 128, :], oT[:], reads=[roT], writes=[g.r_mixedT[2]])
    k.scope_end()


def head_norm_finalize(k, g, y, ry, nheads, center, eps, gnw, rgnw, tmp, rtmp, st, rst):
    hd = 256 // nheads
    yv = y.rearrange("p (h e) -> p h e", h=nheads)
    tv = tmp.rearrange("p (h e) -> p h e", h=nheads)
    if center:
        k.op("dve", lambda e: e.tensor_reduce(out=st[:, 0:nheads], in_=yv, axis=AX.X, op=ALU.add), reads=[ry], writes=[rst])
        k.op("dve", lambda e: e.scalar_tensor_tensor(out=yv, in0=st[:, 0:nheads].unsqueeze(2).to_broadcast([128, nheads, hd]),
                                                     scalar=-1.0 / hd, in1=yv, op0=ALU.mult, op1=ALU.add),
             reads=[rst, ry], writes=[ry])
    k.op("dve", lambda e: e.tensor_tensor(out=tv, in0=yv, in1=yv, op=ALU.mult), reads=[ry], writes=[rtmp])
    k.op("dve", lambda e: e.tensor_reduce(out=st[:, 4:4 + nheads], in_=tv, axis=AX.X, op=ALU.add), reads=[rtmp], writes=[rst])
    k.op("act", lambda e: e.activation(out=st[:, 8:8 + nheads], in_=st[:, 4:4 + nheads], func=AF.Sqrt, scale=1.0 / hd,
                                       bias=eps[:, 0:1]), reads=[rst, g.r_eps], writes=[rst])
    k.op("dve", lambda e: e.reciprocal(out=st[:, 12:12 + nheads], in_=st[:, 8:8 + nheads]), reads=[rst], writes=[rst])
    k.op("dve", lambda e: e.tensor_tensor(out=yv, in0=yv, in1=st[:, 12:12 + nheads].unsqueeze(2).to_broadcast([128, nheads, hd]),
                                          op=ALU.mult), reads=[ry, rst], writes=[ry])
    k.op("dve", lambda e: e.tensor_tensor(out=y, in0=y, in1=gnw, op=ALU.mult), reads=[ry, rgnw], writes=[ry])


def phase_ret(k, g, l):
    k.scope_begin()
    P = g.P
    g.sinq = k.sb("sinq", [128, 32, 32], F32); g.cosq = k.sb("cosq", [128, 32, 32], F32)
    g.sink = k.sb("sink", [128, 32, 32], F32); g.cosk = k.sb("cosk", [128, 32, 32], F32)
    g.r_rope = Res("rope")
    make_rope(k, g)
    lg_b = k.sb("r_lgb", [128, 2, 4], F32); rlg = Res("r_lgb")
    k.dma("sp", lg_b[:], bcast_rows(P["ret_log_decay"][l].rearrange("a h -> (a h)"), 128).rearrange("p (a h) -> p a h", a=2),
          writes=[rlg])
    nlgb = k.sb("r_nlgb", [128, 4], F32); rnlgb = Res("r_nlgb")
    k.op("dve", lambda e: e.tensor_scalar(out=nlgb[:], in0=lg_b[:, 1, :], scalar1=-1.0, scalar2=None, op0=ALU.mult),
         reads=[rlg], writes=[rnlgb])
    M4 = k.sb("r_M", [128, 2, 2, 128], F32); rM = Res("r_M")
    tmpm = k.sb("r_tmpm", [128, 128], F32); rtm = Res("r_tmpm")
    for h in range(4):
        M = M4[:, h % 2]
        h_, h = h, h // 2
        k.op("act", lambda e: e.activation(out=M[:, h, :], in_=g.dmat[:], func=AF.Exp, scale=lg_b[:, 0, h_:h_ + 1]),
             reads=[g.r_dmat, rlg], writes=[rM])
        k.op("dve", lambda e: e.tensor_tensor(out=M[:, h, :], in0=M[:, h, :], in1=g.tri_le[:], op=ALU.mult),
             reads=[rM, g.r_tri_le], writes=[rM])
        k.op("act", lambda e: e.activation(out=tmpm[:], in_=g.dmat[:], func=AF.Exp, scale=nlgb[:, h_:h_ + 1]),
             reads=[g.r_dmat, rnlgb], writes=[rtm])
        k.op("dve", lambda e: e.tensor_tensor(out=tmpm[:], in0=tmpm[:], in1=g.tri_ge[:], op=ALU.mult),
             reads=[rtm, g.r_tri_ge], writes=[rtm])
        k.op("dve", lambda e: e.tensor_tensor(out=M[:, h, :], in0=M[:, h, :], in1=tmpm[:], op=ALU.add),
             reads=[rM, rtm], writes=[rM])
    lgP = k.sb("r_lgP", [128, 2, 2], F32); rlgP = Res("r_lgP")
    for dr in range(2):
        for hh in range(2):
            k.dma("sp", lgP[hh * 64:(hh + 1) * 64, dr, :], bcast_rows(P["ret_log_decay"][l, dr, hh::2], 64),
                  writes=[rlgP], allow_slow_non_contiguous=True)
    i1 = k.sb("r_i1", [128, 2, 128], F32); ri1 = Res("r_i1")
    k.op("pool", lambda e: e.iota(i1[:, 0, :], pattern=[[1, 128]], base=1, channel_multiplier=0,
                                  allow_small_or_imprecise_dtypes=True), writes=[ri1])
    k.op("pool", lambda e: e.iota(i1[:, 1, :], pattern=[[-1, 128]], base=128, channel_multiplier=0,
                                  allow_small_or_imprecise_dtypes=True), writes=[ri1])
    XI = k.sb("r_XI", [128, 2, 2, 128], F32); rXI = Res("r_XI")
    decP = k.sb("r_decP", [128, 2, 2], F32); rdecP = Res("r_decP")
    for dr in range(2):
        for hp in range(2):
            k.op("act", lambda e: e.activation(out=XI[:, dr, hp, :], in_=i1[:, dr, :], func=AF.Exp, scale=lgP[:, dr, hp:hp + 1]),
                 reads=[ri1, rlgP], writes=[rXI])
    k.op("act", lambda e: e.activation(out=decP[:], in_=lgP[:], func=AF.Exp, scale=128.0), reads=[rlgP], writes=[rdecP])
    jr = k.sb("r_jr", [128, 2], F32); rjr = Res("r_jr")
    k.op("dve", lambda e: e.tensor_scalar(out=jr[:, 0:1], in0=g.pidx[:], scalar1=-1.0, scalar2=127.0, op0=ALU.mult, op1=ALU.add),
         reads=[g.r_pidx], writes=[rjr])
    k.op("dve", lambda e: e.tensor_copy(out=jr[:, 1:2], in_=g.pidx[:]), reads=[g.r_pidx], writes=[rjr])
    ZT = k.sb("r_ZT", [128, 2, 4], F32); rZT = Res("r_ZT")
    for dr in range(2):
        k.op("dve", lambda e: e.tensor_scalar(out=ZT[:, dr, :], in0=lg_b[:, dr, :], scalar1=jr[:, dr:dr + 1], scalar2=None,
                                              op0=ALU.mult), reads=[rlg, rjr], writes=[rZT])
    k.op("act", lambda e: e.activation(out=ZT[:], in_=ZT[:], func=AF.Exp), reads=[rZT], writes=[rZT])
    BD = k.sb("r_BD", [128, 128], F32); rBD = Res("r_BD")
    k.op("pool", lambda e: e.memset(BD[:], 0.0), writes=[rBD])
    k.op("pool", lambda e: e.memset(BD[0:64, 0:64], 1.0), writes=[rBD])
    k.op("pool", lambda e: e.memset(BD[64:128, 64:128], 1.0), writes=[rBD])
    gnw = k.sb("r_gnw", [128, 256], F32); rgnw = Res("r_gnw")
    k.dma("sp", gnw[:], bcast_rows(P["ret_gn"][l], 128), writes=[rgnw])
    qT = k.sb("r_qT", [128, 2, T], BF16); rqT = [Res("r_qT%d" % c) for c in range(NT)]
    kT = k.sb("r_kT", [128, 2, T], BF16); rkT = [Res("r_kT%d" % c) for c in range(NT)]
    k_all = k.sb("r_k", [128, NT, 256], BF16); rk_all = [Res("r_k%d" % c) for c in range(NT)]
    v_all = k.sb("r_v", [128, NT, 256], BF16); rv_all = [Res("r_v%d" % c) for c in range(NT)]
    y_all = k.sb("r_y", [128, NT, 256], F32); ry_all = [Res("r_y%d" % c) for c in range(NT)]
    oT = k.sb("r_oT", [128, 2, T], BF16); roT = Res("r_oT")
    gs_all = k.sb("r_gs", [128, NT, 256], BF16); rgs_all = [Res("r_gs%d" % c) for c in range(NT)]
    gld = [k.sb("r_gld%d" % i, [128, 256], F32) for i in range(2)]; rgld = [Res("r_gld%d" % i) for i in range(2)]
    qkv = [k.sb("r_qkv%d" % i, [128, 768], F32) for i in range(2)]; rqkv = [Res("r_qkv%d" % i) for i in range(2)]
    qtm = [k.sb("r_qtm%d" % i, [128, 256], BF16) for i in range(2)]; rqtm = [Res("r_qtm%d" % i) for i in range(2)]
    tq = [k.sb("r_tq%d" % i, [128, 4, 4, 32], F32) for i in range(2)]; rtq = [Res("r_tq%d" % i) for i in range(2)]
    tk = [k.sb("r_tk%d" % i, [128, 4, 4, 32], F32) for i in range(2)]; rtk = [Res("r_tk%d" % i) for i in range(2)]
    ptr = [k.ps("r_ptr%d" % i, [128, 4, 128], BF16) for i in range(2)]; rptr = [Res("r_ptr%d" % i) for i in range(2)]

    def rotary(eng, src, cosT, sinT, c, tt, rtt, dst, rsrc, rdst):
        xv = src.rearrange("p (h two f) -> p h two f", h=4, two=2)
        dv = dst.rearrange("p (h two f) -> p h two f", h=4, two=2)
        cb = cosT[:, c, :].unsqueeze(1).to_broadcast([128, 4, 32])
        sbb = sinT[:, c, :].unsqueeze(1).to_broadcast([128, 4, 32])
        x1 = xv[:, :, 0, :]; x2 = xv[:, :, 1, :]
        k.op(eng, lambda e: e.tensor_tensor(out=tt[:, 0], in0=x1, in1=cb, op=ALU.mult), reads=[rsrc, g.r_rope], writes=[rtt])
        k.op(eng, lambda e: e.tensor_tensor(out=tt[:, 1], in0=x2, in1=sbb, op=ALU.mult), reads=[rsrc, g.r_rope], writes=[rtt])
        k.op(eng, lambda e: e.tensor_tensor(out=tt[:, 2], in0=x1, in1=sbb, op=ALU.mult), reads=[rsrc, g.r_rope], writes=[rtt])
        k.op(eng, lambda e: e.tensor_tensor(out=tt[:, 3], in0=x2, in1=cb, op=ALU.mult), reads=[rsrc, g.r_rope], writes=[rtt])
        k.op(eng, lambda e: e.tensor_tensor(out=dv[:, :, 0, :], in0=tt[:, 0], in1=tt[:, 1], op=ALU.subtract), reads=[rtt], writes=[rdst])
        k.op(eng, lambda e: e.tensor_tensor(out=dv[:, :, 1, :], in0=tt[:, 2], in1=tt[:, 3], op=ALU.add), reads=[rtt], writes=[rdst])

    for c in range(NT):
        s = c % 2
        k.dma("sp", qkv[s][:], g.proj[c * 128:(c + 1) * 128, 0:768], reads=[g.r_proj], writes=[rqkv[s]])
        rotary("dve", qkv[s][:, 0:256], g.cosq, g.sinq, c, tq[s], rtq[s], qtm[s][:], rqkv[s], rqtm[s])
        rotary("pool", qkv[s][:, 256:512], g.cosk, g.sink, c, tk[s], rtk[s], k_all[:, c, :], rqkv[s], rk_all[c])
        k.op("act", lambda e: e.copy(out=v_all[:, c, :], in_=qkv[s][:, 512:768]), reads=[rqkv[s]], writes=[rv_all[c]])
        k.dma("sp", gld[s][:], g.proj[c * 128:(c + 1) * 128, C_RG:C_RG + 256], reads=[g.r_proj], writes=[rgld[s]])
        k.op("act", lambda e: e.activation(out=gs_all[:, c, :], in_=gld[s][:], func=AF.Silu), reads=[rgld[s]], writes=[rgs_all[c]])
        for hp in range(2):
            k.op("pe", lambda e: e.transpose(out=ptr[s][:, hp, :], in_=qtm[s][:, hp * 128:(hp + 1) * 128], identity=g.ident_b[:]),
                 reads=[rqtm[s], g.r_ident_b], writes=[rptr[s]], inc=False)
        for hp in range(2):
            k.op("pe", lambda e: e.transpose(out=ptr[s][:, 2 + hp, :], in_=k_all[:, c, hp * 128:(hp + 1) * 128], identity=g.ident_b[:]),
                 reads=[rk_all[c], g.r_ident_b], writes=[rptr[s]], inc=(hp == 1))
        k.op("act", lambda e: e.copy(out=qT[:, :, c * 128:(c + 1) * 128], in_=ptr[s][:, 0:2, :]), reads=[rptr[s]], writes=[rqT[c]])
        k.op("act", lambda e: e.copy(out=kT[:, :, c * 128:(c + 1) * 128], in_=ptr[s][:, 2:4, :]), reads=[rptr[s]], writes=[rkT[c]])
    S32 = [k.sb("r_S32_%d" % d, [128, 2, 128], F32) for d in range(2)]; rS32 = [Res("r_S32_%d" % d) for d in range(2)]
    Sbf = [[k.sb("r_Sbf_%d_%d" % (d, q), [128, 2, 128], BF16) for q in range(2)] for d in range(2)]
    rSbf = [[Res("r_Sbf_%d_%d" % (d, q)) for q in range(2)] for d in range(2)]
    for d in range(2):
        k.op("pool", lambda e: e.memset(S32[d][:], 0.0), writes=[rS32[d]])
        for q in range(2):
            k.op("pool", lambda e: e.memset(Sbf[d][q][:], 0.0), writes=[rSbf[d][q]])
    psc1 = k.ps("r_psc", [128, 2, 512], F32); psc = [psc1, psc1]; rpsc1 = Res("r_psc"); rpsc = [rpsc1, rpsc1]
    py1 = k.ps("r_py", [128, 2, 512], F32); py = [py1, py1]; rpy1 = Res("r_py"); rpy = [rpy1, rpy1]
    pds = k.ps("r_pds", [128, 2, 128], F32); rpds = Res("r_pds")
    PT = [k.sb("r_PT%d" % i, [128, 2, 2, 128], BF16) for i in range(2)]; rPT = [Res("r_PT%d" % i) for i in range(2)]
    QX = [k.sb("r_QX%d" % i, [128, 2, 128], BF16) for i in range(2)]; rQX = [Res("r_QX%d" % i) for i in range(2)]
    KZ = [k.sb("r_KZ%d" % i, [128, 256], BF16) for i in range(2)]; rKZ = [Res("r_KZ%d" % i) for i in range(2)]
    dsm2 = [k.sb("r_dsm%d" % i, [128, 2, 128], F32) for i in range(2)]; rdsm2 = [Res("r_dsm%d" % i) for i in range(2)]

    def state_pre(d, c, s):
        dsm, rdsm = dsm2[s], rdsm2[s]
        k.op("pool", lambda e: e.tensor_tensor(out=KZ[s][:].rearrange("p (h e) -> p h e", h=4),
                                               in0=k_all[:, c, :].rearrange("p (h e) -> p h e", h=4),
                                               in1=ZT[:, d, :].unsqueeze(2).to_broadcast([128, 4, 64]), op=ALU.mult),
             reads=[rk_all[c], rZT], writes=[rKZ[s]])
        for hp in range(2):
            k.op("pe", lambda e: e.matmul(pds[:, hp, :], lhsT=KZ[s][:, hp * 128:(hp + 1) * 128], rhs=v_all[:, c, hp * 128:(hp + 1) * 128],
                                          start=True, stop=True), reads=[rKZ[s], rv_all[c]], writes=[rpds], inc=(hp == 1))
        k.op("dve", lambda e: e.tensor_tensor(out=dsm[:], in0=pds[:], in1=BD[:].unsqueeze(1).to_broadcast([128, 2, 128]), op=ALU.mult),
             reads=[rpds, rBD], writes=[rdsm])

    def state_post(d, c, s):
        dsm, rdsm = dsm2[s], rdsm2[s]
        for hp in range(2):
            k.op("dve", lambda e: e.scalar_tensor_tensor(out=S32[d][:, hp, :], in0=S32[d][:, hp, :], scalar=decP[:, d, hp:hp + 1],
                                                         in1=dsm[:, hp, :], op0=ALU.mult, op1=ALU.add),
                 reads=[rS32[d], rdecP, rdsm], writes=[rS32[d]])
        k.op("act", lambda e: e.copy(out=Sbf[d][c % 2][:], in_=S32[d][:]), reads=[rS32[d]], writes=[rSbf[d][c % 2]])

    for c in range(NT):
        s = c % 2
        cs = slice(c * 128, (c + 1) * 128)
        for h in range(4):
            hp, hh = h // 2, h % 2
            k.op("pe", lambda e: e.matmul(psc[s][:, hh, hp * 128:(hp + 1) * 128], lhsT=kT[hh * 64:(hh + 1) * 64, hp, cs], rhs=qT[hh * 64:(hh + 1) * 64, hp, cs],
                                          start=True, stop=True), reads=[rkT[c], rqT[c]], writes=[rpsc[s]], inc=(h == 3))
        k.op("dve", lambda e: e.tensor_tensor(out=PT[s][:], in0=psc[s][:, :, 0:256].rearrange("p a (b c) -> p a b c", b=2), in1=M4[:], op=ALU.mult), reads=[rpsc[s], rM], writes=[rPT[s]])
        k.op("pool", lambda e: e.tensor_tensor(out=QX[s][:], in0=qT[:, :, cs], in1=XI[:, 0], op=ALU.mult), reads=[rqT[c], rXI], writes=[rQX[s]])
        state_pre(0, c, s)
        for h in range(4):
            hp, hh = h // 2, h % 2
            k.op("pe", lambda e: e.matmul(py[s][:, hh, hp * 64:(hp + 1) * 64], lhsT=PT[s][:, hh, hp, :], rhs=v_all[:, c, h * 64:(h + 1) * 64],
                                          start=True, stop=False), reads=[rPT[s], rv_all[c]], writes=[rpy[s]], inc=False)
            k.op("pe", lambda e: e.matmul(py[s][:, hh, hp * 64:(hp + 1) * 64], lhsT=QX[s][hh * 64:(hh + 1) * 64, hp, :],
                                          rhs=Sbf[0][(c + 1) % 2][hh * 64:(hh + 1) * 64, hp, hh * 64:(hh + 1) * 64], start=False, stop=True),
                 reads=[rQX[s], rSbf[0][(c + 1) % 2]], writes=[rpy[s]], inc=(h == 3))
        k.op("act", lambda e: e.copy(out=y_all[:, c, :].rearrange("p (hp hh e) -> p hh hp e", hp=2, hh=2),
                                     in_=py[s][:, :, 0:128].rearrange("p hh (hp e) -> p hh hp e", hp=2)), reads=[rpy[s]], writes=[ry_all[c]])
        state_post(0, c, s)
    gt_ = [k.sb("r_g%d" % i, [128, 256], F32) for i in range(2)]; rgt = [Res("r_g%d" % i) for i in range(2)]
    tmp = [k.sb("r_tmp%d" % i, [128, 256], F32) for i in range(2)]; rtmp = [Res("r_tmp%d" % i) for i in range(2)]
    st = [k.sb("r_st%d" % i, [128, 16], F32) for i in range(2)]; rst = [Res("r_st%d" % i) for i in range(2)]
    ob = [k.sb("r_ob%d" % i, [128, 256], BF16) for i in range(2)]; rob = [Res("r_ob%d" % i) for i in range(2)]
    for c in range(NT - 1, -1, -1):
        s = c % 2
        cs = slice(c * 128, (c + 1) * 128)
        k.op("pool", lambda e: e.tensor_tensor(out=QX[s][:], in0=qT[:, :, cs], in1=XI[:, 1], op=ALU.mult), reads=[rqT[c], rXI], writes=[rQX[s]])
        state_pre(1, c, s)
        for h in range(4):
            hp, hh = h // 2, h % 2
            k.op("pe", lambda e: e.matmul(py[s][:, hh, hp * 64:(hp + 1) * 64], lhsT=QX[s][hh * 64:(hh + 1) * 64, hp, :],
                                          rhs=Sbf[1][(c + 1) % 2][hh * 64:(hh + 1) * 64, hp, hh * 64:(hh + 1) * 64], start=True, stop=True),
                 reads=[rQX[s], rSbf[1][(c + 1) % 2]], writes=[rpy[s]], inc=(h == 3))
        k.op("dve", lambda e: e.tensor_tensor(out=y_all[:, c, :].rearrange("p (hp hh e) -> p hh hp e", hp=2, hh=2),
                                              in0=y_all[:, c, :].rearrange("p (hp hh e) -> p hh hp e", hp=2, hh=2),
                                              in1=py[s][:, :, 0:128].rearrange("p hh (hp e) -> p hh hp e", hp=2), op=ALU.add),
             reads=[ry_all[c], rpy[s]], writes=[ry_all[c]])
        state_post(1, c, s)
        head_norm_finalize(k, g, y_all[:, c, :], ry_all[c], 4, True, g.eps_c, gnw[:], rgnw, tmp[s][:], rtmp[s], st[s], rst[s])
        k.op("pool", lambda e: e.tensor_tensor(out=ob[s][:], in0=y_all[:, c, :], in1=gs_all[:, c, :], op=ALU.mult),
             reads=[ry_all[c], rgs_all[c]], writes=[rob[s]])
        for hp in range(2):
            k.op("pe", lambda e: e.transpose(out=ptr[s][:, hp, :], in_=ob[s][:, hp * 128:(hp + 1) * 128], identity=g.ident_b[:]),
                 reads=[rob[s], g.r_ident_b], writes=[rptr[s]], inc=(hp == 1))
        k.op("act", lambda e: e.copy(out=oT[:, :, cs], in_=ptr[s][:, 0:2, :]), reads=[rptr[s]], writes=[roT])
    for hp in range(2):
        k.dma("sp", g.mixedT[hp * 128:(hp + 1) * 128, :], oT[:, hp, :], reads=[roT], writes=[g.r_mixedT[0]])
    k.scope_end()


class BankPool:
    def __init__(self, k, name):
        self.t = k.ps(name, [128, 8, 512], F32)
        self.r = [Res("%s_b%d" % (name, i)) for i in range(8)]
        self.nxt = 0

    def take(self, n=1):
        if self.nxt + n > 8:
            self.nxt = 0
        i = self.nxt
        self.nxt = (self.nxt + n) % 8
        return i


def phase_gla(k, g, l):
    k.scope_begin()
    P = g.P
    BP = BankPool(k, "g_ps")
    AU = k.sb("g_AU", [16, 256], F32); rAU = Res("g_AU")
    AB = k.sb("g_AB", [1, 256], F32); rAB = Res("g_AB")
    for d in range(2):
        k.dma("sp", AU[:, d * 128:(d + 1) * 128], P["gla_alpha_up"][l, d], writes=[rAU])
        k.dma("sp", AB[:, d * 128:(d + 1) * 128], P["gla_alpha_b"][l, d].unsqueeze(0), writes=[rAB])
    gnw = k.sb("g_gnw", [128, 256], F32); rgnw = Res("g_gnw")
    k.dma("sp", gnw[:], bcast_rows(P["gla_gn"][l], 128), writes=[rgnw])
    mask2 = k.sb("g_mask2", [128, 2, 4, 128], F32); rmask2 = Res("g_mask2")
    k.op("dve", lambda e: e.tensor_copy(out=mask2[:, 0], in_=g.tri_le[:].unsqueeze(1).to_broadcast([128, 4, 128])), reads=[g.r_tri_le], writes=[rmask2])
    k.op("dve", lambda e: e.tensor_copy(out=mask2[:, 1], in_=g.tri_ge[:].unsqueeze(1).to_broadcast([128, 4, 128])), reads=[g.r_tri_ge], writes=[rmask2])
    BD4 = k.sb("g_BD4", [128, 256], F32); rBD4 = Res("g_BD4")
    k.op("pool", lambda e: e.memset(BD4[:], 0.0), writes=[rBD4])
    for h in range(3):
        k.op("pool", lambda e: e.memset(BD4[h * 32:(h + 1) * 32, h * 64:(h + 1) * 64], 1.0), writes=[rBD4])
    k.op("pool", lambda e: e.memset(BD4[64:128, 192:256], 1.0), writes=[rBD4])
    k.op("pool", lambda e: e.memset(BD4[64:96, 192:256], 0.0), writes=[rBD4])
    v_all = k.sb("g_v", [128, NT, 256], BF16); rv_all = [Res("g_v%d" % c) for c in range(NT)]
    y_all = k.sb("g_y", [128, NT, 256], F32); ry_all = [Res("g_y%d" % c) for c in range(NT)]
    khb_all = k.sb("g_khb", [128, NT, 128], BF16); rkhb = [Res("g_khb%d" % c) for c in range(NT)]
    qdbT = k.sb("g_qdbT", [128, T], BF16); rqdbT = [Res("g_qdbT%d" % c) for c in range(NT)]
    dec_all = k.sb("g_dec", [128, NT, 2], F32); rdec = [Res("g_dec%d" % c) for c in range(NT)]
    oT = k.sb("g_oT", [128, 2, T], BF16); roT = Res("g_oT")
    S32 = [k.sb("g_S32_%d" % d, [128, 256], F32) for d in range(2)]; rS32 = [Res("g_S32_%d" % d) for d in range(2)]
    Sbf = [[k.sb("g_Sbf_%d_%d" % (d, q), [128, 256], BF16) for q in range(2)] for d in range(2)]
    rSbf = [[Res("g_Sbf_%d_%d" % (d, q)) for q in range(2)] for d in range(2)]
    for d in range(2):
        k.op("pool", lambda e: e.memset(S32[d][:], 0.0), writes=[rS32[d]])
        for q in range(2):
            k.op("pool", lambda e: e.memset(Sbf[d][q][:], 0.0), writes=[rSbf[d][q]])

    def dbl(name, shape, dt):
        return [k.sb("%s%d" % (name, i), shape, dt) for i in range(2)], [Res("%s%d" % (name, i)) for i in range(2)]
    gl, rgl = dbl("g_gl", [128, 784], F32)
    xaT, rxaT = dbl("g_xaT", [16, 128], F32)
    ez, rez = dbl("g_ez", [128, 256], F32)
    la, rla = dbl("g_la", [128, 256], F32)
    cum, rcum = dbl("g_cum", [128, 512], F32)
    E1, rE1 = dbl("g_E1", [128, 256], F32)
    E2, rE2 = dbl("g_E2", [128, 256], F32)
    E3, rE3 = dbl("g_E3", [128, 256], F32)
    qd, rqd = dbl("g_qd", [128, 2, 128], BF16)
    kd, rkd = dbl("g_kd", [128, 2, 128], BF16)
    kh, rkh = dbl("g_kh", [128, 2, 128], BF16)
    sT, rsT = dbl("g_sT", [32, 16, 128], BF16)
    qdfT, rqdfT = dbl("g_qdfT", [128, 128], BF16)
    PT, rPT = dbl("g_PT", [128, 2, 4, 128], BF16)
    dsm, rdsm = dbl("g_dsm", [128, 256], F32)

    def state_pre(d, c, khap, rkhr, s):
        bi = BP.take()
        k.op("pe", lambda e: e.matmul(BP.t[:, bi, 0:256], lhsT=khap, rhs=v_all[:, c, :], start=True, stop=True),
             reads=[rkhr, rv_all[c]], writes=[BP.r[bi]])
        k.op("dve", lambda e: e.tensor_tensor(out=dsm[s][:], in0=BP.t[:, bi, 0:256], in1=BD4[:], op=ALU.mult),
             reads=[BP.r[bi], rBD4], writes=[rdsm[s]])

    def state_post(d, c, s):
        k.op("dve", lambda e: e.scalar_tensor_tensor(out=S32[d][:], in0=S32[d][:], scalar=dec_all[:, c, d:d + 1], in1=dsm[s][:],
                                                     op0=ALU.mult, op1=ALU.add), reads=[rS32[d], rdec[c], rdsm[s]], writes=[rS32[d]])
        k.op("act", lambda e: e.copy(out=Sbf[d][c % 2][:], in_=S32[d][:]), reads=[rS32[d]], writes=[rSbf[d][c % 2]])

    post_done = [0]

    def p1(c):
        s = c % 2
        cs = slice(c * 128, (c + 1) * 128)
        k.dma("sp", gl[s][:], g.proj[cs, C_GQ:IN_COLS], reads=[g.r_proj2], writes=[rgl[s]])
        q = gl[s][:, 0:128]; kx = gl[s][:, 128:256]; v = gl[s][:, 256:512]; xa = gl[s][:, 768:784]
        k.op("act", lambda e: e.copy(out=v_all[:, c, :], in_=v), reads=[rgl[s]], writes=[rv_all[c]])
        yield
        b0 = BP.take()
        k.op("pe", lambda e: e.transpose(out=BP.t[0:16, b0, 0:128], in_=xa, identity=g.ident_f[:]),
             reads=[rgl[s], g.r_ident_f], writes=[BP.r[b0]])
        k.op("act", lambda e: e.copy(out=xaT[s][:], in_=BP.t[0:16, b0, 0:128]), reads=[BP.r[b0]], writes=[rxaT[s]])
        yield
        b1 = BP.take()
        k.op("pe", lambda e: e.matmul(BP.t[:, b1, 0:256], lhsT=xaT[s][:], rhs=AU[:], start=True, stop=False),
             reads=[rxaT[s], rAU], writes=[BP.r[b1]], inc=False)
        k.op("pe", lambda e: e.matmul(BP.t[:, b1, 0:256], lhsT=g.ones_f[0:1, :], rhs=AB[:], start=False, stop=True),
             reads=[g.r_ones_f, rAB], writes=[BP.r[b1]])
        k.op("act", lambda e: e.activation(out=ez[s][:], in_=BP.t[:, b1, 0:256], func=AF.Exp, scale=-1.0), reads=[BP.r[b1]], writes=[rez[s]])
        k.op("act", lambda e: e.activation(out=ez[s][:], in_=ez[s][:], func=AF.Ln, bias=g.one_c[:, 0:1]), reads=[rez[s], g.r_one], writes=[rez[s]])
        k.op("dve", lambda e: e.tensor_scalar(out=la[s][:], in0=ez[s][:], scalar1=-1.0 / 16.0, scalar2=None, op0=ALU.mult),
             reads=[rez[s]], writes=[rla[s]])
        yield
        b2 = BP.take()
        for qi, (lt, rlt, co) in enumerate([(g.tri_le, g.r_tri_le, 0), (g.tri_ge, g.r_tri_ge, 128), (g.ones_f, g.r_ones_f, 0), (g.ones_f, g.r_ones_f, 128)]):
            k.op("pe", lambda e: e.matmul(BP.t[:, b2, qi * 128:(qi + 1) * 128], lhsT=lt[:], rhs=la[s][:, co:co + 128], start=True, stop=True),
                 reads=[rlt, rla[s]], writes=[BP.r[b2]], inc=(qi == 3))
        k.op("act", lambda e: e.copy(out=cum[s][:], in_=BP.t[:, b2, :]), reads=[BP.r[b2]], writes=[rcum[s]])
        yield
        b3 = BP.take()
        for d in range(2):
            k.op("pe", lambda e: e.matmul(BP.t[:, b3, d:d + 1], lhsT=la[s][:, d * 128:(d + 1) * 128], rhs=g.ones_f[:, 0:1], start=True, stop=True),
                 reads=[rla[s], g.r_ones_f], writes=[BP.r[b3]], inc=(d == 1))
        k.op("act", lambda e: e.activation(out=dec_all[:, c, :], in_=BP.t[:, b3, 0:2], func=AF.Exp), reads=[BP.r[b3]], writes=[rdec[c]])
        k.op("act", lambda e: e.activation(out=E1[s][:], in_=cum[s][:, 0:256], func=AF.Exp), reads=[rcum[s]], writes=[rE1[s]])
        k.op("act", lambda e: e.activation(out=E2[s][:], in_=cum[s][:, 0:256], func=AF.Exp, scale=-1.0), reads=[rcum[s]], writes=[rE2[s]])
        k.op("dve", lambda e: e.tensor_tensor(out=E3[s][:], in0=cum[s][:, 256:512], in1=cum[s][:, 0:256], op=ALU.subtract), reads=[rcum[s]], writes=[rE3[s]])
        k.op("act", lambda e: e.activation(out=E3[s][:], in_=E3[s][:], func=AF.Exp), reads=[rE3[s]], writes=[rE3[s]])
        yield
        qb = q.unsqueeze(1).to_broadcast([128, 2, 128]); kb = kx.unsqueeze(1).to_broadcast([128, 2, 128])
        k.op("dve", lambda e: e.scalar_tensor_tensor(out=qd[s][:], in0=qb, scalar=32.0 ** -0.5, in1=E1[s][:].rearrange("p (a b) -> p a b", a=2),
                                                     op0=ALU.mult, op1=ALU.mult), reads=[rgl[s], rE1[s]], writes=[rqd[s]])
        k.op("pool", lambda e: e.tensor_tensor(out=kd[s][:], in0=kb, in1=E2[s][:].rearrange("p (a b) -> p a b", a=2), op=ALU.mult),
             reads=[rgl[s], rE2[s]], writes=[rkd[s]])
        k.op("pool", lambda e: e.tensor_tensor(out=kh[s][:], in0=kb, in1=E3[s][:].rearrange("p (a b) -> p a b", a=2), op=ALU.mult),
             reads=[rgl[s], rE3[s]], writes=[rkh[s]])
        k.op("pool", lambda e: e.tensor_copy(out=khb_all[:, c, :], in_=kh[s][:, 1, :]), reads=[rkh[s]], writes=[rkhb[c]])
        yield
        b4 = BP.take(2)
        ptv = BP.t[0:32, b4:b4 + 2, :].rearrange("p a b -> p (a b)").bitcast(BF16)
        for ai, (arr, rarr, d) in enumerate([(qd[s], rqd[s], 0), (qd[s], rqd[s], 1), (kd[s], rkd[s], 0), (kd[s], rkd[s], 1)]):
            for h in range(4):
                idx = ai * 4 + h
                k.op("pe", lambda e: e.transpose(out=ptv[:, idx * 128:(idx + 1) * 128], in_=arr[:, d, h * 32:(h + 1) * 32], identity=g.ident_b[:]),
                     reads=[rarr, g.r_ident_b], writes=[BP.r[b4], BP.r[b4 + 1]], inc=(idx == 15))
        k.op("act", lambda e: e.copy(out=sT[s][:].rearrange("p a b -> p (a b)"), in_=ptv), reads=[BP.r[b4], BP.r[b4 + 1]], writes=[rsT[s]])
        yield
        b5 = BP.take()
        pfv = BP.t[:, b5, :].bitcast(BF16)
        for d in range(2):
            k.op("pe", lambda e: e.transpose(out=pfv[:, d * 128:(d + 1) * 128], in_=qd[s][:, d, :], identity=g.ident_b[:]),
                 reads=[rqd[s], g.r_ident_b], writes=[BP.r[b5]], inc=(d == 1))
        k.op("act", lambda e: e.copy(out=qdfT[s][:], in_=pfv[:, 0:128]), reads=[BP.r[b5]], writes=[rqdfT[s]])
        k.op("act", lambda e: e.copy(out=qdbT[:, cs], in_=pfv[:, 128:256]), reads=[BP.r[b5]], writes=[rqdbT[c]])
        yield
        b6 = BP.take(2)
        for d in range(2):
            for h in range(4):
                k.op("pe", lambda e: e.matmul(BP.t[:, b6 + d, h * 128:(h + 1) * 128], lhsT=sT[s][:, (2 + d) * 4 + h, :], rhs=sT[s][:, d * 4 + h, :],
                                              start=True, stop=True), reads=[rsT[s]], writes=[BP.r[b6 + d]], inc=(h == 3))
        k.op("dve", lambda e: e.tensor_tensor(out=PT[s][:].rearrange("p a b c -> p a (b c)"), in0=BP.t[:, b6:b6 + 2, :],
                                              in1=mask2[:].rearrange("p a b c -> p a (b c)"), op=ALU.mult),
             reads=[BP.r[b6], BP.r[b6 + 1], rmask2], writes=[rPT[s]])
        yield
        while post_done[0] < c:
            yield
        state_pre(0, c, kh[s][:, 0, :], rkh[s], s)
        b7 = BP.take()
        k.op("pe", lambda e: e.matmul(BP.t[:, b7, 0:256], lhsT=qdfT[s][:], rhs=Sbf[0][(c + 1) % 2][:], start=True, stop=False),
             reads=[rqdfT[s], rSbf[0][(c + 1) % 2]], writes=[BP.r[b7]], inc=False)
        for h in range(4):
            for d in range(2):
                last = (h == 3 and d == 1)
                k.op("pe", lambda e: e.matmul(BP.t[:, b7, h * 64:(h + 1) * 64], lhsT=PT[s][:, d, h, :], rhs=v_all[:, c, h * 64:(h + 1) * 64],
                                              start=False, stop=last, skip_group_check=True), reads=[rPT[s], rv_all[c]], writes=[BP.r[b7]], inc=last)
        k.op("act", lambda e: e.copy(out=y_all[:, c, :], in_=BP.t[:, b7, 0:256]), reads=[BP.r[b7]], writes=[ry_all[c]])
        state_post(0, c, s)
        post_done[0] += 1
    import itertools
    chains = []
    for par in range(2):
        glist = [p1(c) for c in range(NT) if c % 2 == par]
        chains.append([itertools.chain.from_iterable(glist), 5 * par, True])
    sentinel = object()
    rnd = 0
    while any(cg[2] for cg in chains):
        for cg in chains:
            if cg[2] and rnd >= cg[1]:
                if next(cg[0], sentinel) is sentinel:
                    cg[2] = False
        rnd += 1
        assert rnd < 100000
    og, rog = dbl("g_og", [128, 256], F32)
    ogs_all = k.sb("g_ogs", [128, NT, 256], BF16); rogs_all = [Res("g_ogs%d" % c) for c in range(NT)]
    for c in range(NT):
        s = c % 2
        k.dma("sp", og[s][:], g.proj[c * 128:(c + 1) * 128, C_GOG:C_GOG + 256], reads=[g.r_proj2], writes=[rog[s]])
        k.op("act", lambda e: e.activation(out=ogs_all[:, c, :], in_=og[s][:], func=AF.Silu), reads=[rog[s]], writes=[rogs_all[c]])
    tmp, rtmp = dbl("g_tmp", [128, 256], F32)
    st, rst = dbl("g_st", [128, 16], F32)
    ob, rob = dbl("g_ob", [128, 256], BF16)
    post2_done = [0]

    def p2(c, n):
        s = c % 2
        cs = slice(c * 128, (c + 1) * 128)
        state_pre(1, c, khb_all[:, c, :], rkhb[c], s)
        yield
        while post2_done[0] < n:
            yield
        b0 = BP.take()
        k.op("pe", lambda e: e.matmul(BP.t[:, b0, 0:256], lhsT=qdbT[:, cs], rhs=Sbf[1][(c + 1) % 2][:], start=True, stop=True),
             reads=[rqdbT[c], rSbf[1][(c + 1) % 2]], writes=[BP.r[b0]])
        k.op("dve", lambda e: e.tensor_tensor(out=y_all[:, c, :], in0=y_all[:, c, :], in1=BP.t[:, b0, 0:256], op=ALU.add),
             reads=[ry_all[c], BP.r[b0]], writes=[ry_all[c]])
        state_post(1, c, s)
        post2_done[0] += 1
        yield
        head_norm_finalize(k, g, y_all[:, c, :], ry_all[c], 4, False, g.eps_c, gnw[:], rgnw, tmp[s][:], rtmp[s], st[s], rst[s])
        yield
        k.op("pool", lambda e: e.tensor_tensor(out=ob[s][:], in0=y_all[:, c, :], in1=ogs_all[:, c, :], op=ALU.mult),
             reads=[ry_all[c], rogs_all[c]], writes=[rob[s]])
        b1 = BP.take()
        pfv = BP.t[:, b1, :].bitcast(BF16)
        for hp in range(2):
            k.op("pe", lambda e: e.transpose(out=pfv[:, hp * 128:(hp + 1) * 128], in_=ob[s][:, hp * 128:(hp + 1) * 128], identity=g.ident_b[:]),
                 reads=[rob[s], g.r_ident_b], writes=[BP.r[b1]], inc=(hp == 1))
        k.op("act", lambda e: e.copy(out=oT[:, :, cs], in_=pfv[:, 0:256].rearrange("p (a b) -> p a b", a=2)), reads=[BP.r[b1]], writes=[roT])
    chains2 = []
    for par in range(2):
        glist = [p2(NT - 1 - n, n) for n in range(NT) if n % 2 == par]
        chains2.append([itertools.chain.from_iterable(glist), 2 * par, True])
    rnd = 0
    while any(cg[2] for cg in chains2):
        for cg in chains2:
            if cg[2] and rnd >= cg[1]:
                if next(cg[0], sentinel) is sentinel:
                    cg[2] = False
        rnd += 1
        assert rnd < 100000
    for hp in range(2):
        k.dma("sp", g.mixedT[768 + hp * 128:768 + (hp + 1) * 128, :], oT[:, hp, :], reads=[roT], writes=[g.r_mixedT[3]])
    k.scope_end()


RWKV_OFFSET = 9


def phase_rwkv(k, g, l):
    k.scope_begin()
    P = g.P
    BP = BankPool(k, "w_ps")

    def cst(name, shape, dt, src, **kw):
        t = k.sb(name, shape, dt); r = Res(name)
        k.dma("sp", t[:], src, writes=[r], **kw)
        return t, r
    mu_b = k.sb("w_mu", [128, 896], F32); rmu = Res("w_mu")
    k.dma("sp", mu_b[:, 0:768], bcast_rows(P["rwkv_mu_rkv"][l].rearrange("a c -> (a c)"), 128), writes=[rmu])
    k.dma("sp", mu_b[:, 768:832], bcast_rows(P["rwkv_mu_w"][l], 128), writes=[rmu])
    k.dma("sp", mu_b[:, 832:896], bcast_rows(P["rwkv_mu_a"][l], 128), writes=[rmu])
    WUP = []; AUP = []
    for d in range(2):
        for (lst, nm, up, b0n) in ((WUP, "w_wup", "rwkv_w_up", "rwkv_w0"), (AUP, "w_aup", "rwkv_a_up", "rwkv_a0")):
            t_ = k.sb("%s%d" % (nm, d), [65, 256], F32); r_ = Res("%s%d" % (nm, d))
            k.dma("sp", t_[0:64, :], P[up][l, d], writes=[r_])
            k.dma("sp", t_[64:65, :], P[b0n][l, d].unsqueeze(0), writes=[r_])
            lst.append((t_, r_))
    kk_b, rkk_b = cst("w_kkb", [128, 256], F32, bcast_rows(P["rwkv_k_k"][l], 128))
    ka_b, rka_b = cst("w_kab", [128, 256], F32, bcast_rows(P["rwkv_k_a"][l], 128))
    rk_b, rrk_b = cst("w_rkb", [128, 256], F32, bcast_rows(P["rwkv_r_k"][l].rearrange("h e -> (h e)"), 128))
    gnw, rgnw = cst("w_gnw", [128, 256], F32, bcast_rows(P["rwkv_gn"][l], 128))
    om_b = k.sb("w_om", [128, 256], F32); rom = Res("w_om")
    k.op("dve", lambda e: e.tensor_scalar(out=om_b[:], in0=ka_b[:], scalar1=-1.0, scalar2=1.0, op0=ALU.mult, op1=ALU.add), reads=[rka_b], writes=[rom])
    gup = k.sb("w_gup", [128, 256], BF16); rgup = Res("w_gup")
    k.dma("pool", gup[:], P["rwkv_g_up"][l], writes=[rgup])
    eps2 = k.sb("w_eps2", [128, 1], F32); reps2 = Res("w_eps2")
    k.op("pool", lambda e: e.memset(eps2[:], 64e-5), writes=[reps2])
    y_all = k.sb("w_y", [128, NT, 256], BF16); ry_all = [Res("w_y%d" % c) for c in range(NT)]
    bo_all = k.sb("w_bo", [128, NT, 256], BF16); rbo_all = [Res("w_bo%d" % c) for c in range(NT)]
    oT = k.sb("w_oT", [128, 2, T], BF16); roT = Res("w_oT")
    H32 = [k.sb("w_H32_%d" % d, [64, 4, 64], F32) for d in range(2)]; rH32 = [Res("w_H32_%d" % d) for d in range(2)]
    Hbf = [k.sb("w_Hbf_%d" % d, [64, 4, 64], BF16) for d in range(2)]; rHbf = [Res("w_Hbf_%d" % d) for d in range(2)]
    for d in range(2):
        k.op("pool", lambda e: e.memset(H32[d][:], 0.0), writes=[rH32[d]])
        k.op("pool", lambda e: e.memset(Hbf[d][:], 0.0), writes=[rHbf[d]])
    cnt = [0]

    def T_(shape, dt, nb=2):
        cnt[0] += 1
        n = "w_t%d" % cnt[0]
        return [k.sb(n + "_%d" % i, shape, dt) for i in range(nb)], [Res(n + "_%d" % i) for i in range(nb)]
    k.scope_begin()
    cur, rcur = T_([128, 1024], F32)
    prv, rprv = T_([128, 896], F32)
    sh, rsh = T_([128, 896], F32)
    txw, rtxw = T_([128, 64], F32)
    xT, rxT = T_([65, 2, 128], F32)
    for i_ in range(2):
        k.op("pool", lambda e: e.memset(xT[i_][64:65], 1.0), writes=[rxT[i_]])
    ld, rld = T_([128, 256], F32)
    aa, raa = T_([128, 256], F32)
    kk, rkk = T_([128, 256], F32)
    t1, rt1 = T_([128, 256], F32)
    t2, rt2 = T_([128, 256], F32)
    km, rkm = T_([128, 256], F32)
    bb, rbb = T_([128, 256], F32)
    st, rst = T_([128, 16], F32)
    LC, rLC = T_([128, 512], F32)
    Einc, rEinc = T_([128, 256], F32)
    Eneg, rEneg = T_([128, 256], F32)
    Eex, rEex = T_([128, 256], F32)
    Ehat, rEhat = T_([128, 256], F32)
    OPS, rOPS = T_([128, 4, 256], BF16, 4)
    Bhat, rBhat = T_([128, 256], BF16, 4)
    Khat, rKhat = T_([128, 256], BF16, 4)
    Vb, rVb = T_([128, 256], BF16, 4)
    GCe, rGCe = T_([64, 4], F32, 4)
    FT, rFT = T_([128, 2, 4, 128], BF16, 4)
    RT, rRT = T_([64, 4, 128], BF16, 4)
    MK, rMK = T_([128, 4, 2, 2, 128], BF16, 4)
    X0, rX0 = T_([128, 1, 4, 128], BF16, 4)
    X1, rX1 = T_([128, 1, 4, 128], BF16, 4)
    TA, rTA = T_([128, 4, 128], BF16, 4)
    PA, rPA = T_([128, 4, 2, 128], BF16, 4)
    PB, rPB = T_([128, 4, 2, 128], BF16, 4)
    Xv, rXv = T_([128, 256], BF16, 4)
    WT, rWT = T_([64, 4, 128], BF16, 4)
    Ub, rUb = T_([128, 256], BF16, 4)
    visited = set()
    visited_y = set()
    H_done = [0, 0]
    early_lock = [None, None]

    def chunk(d, c, n, cid):
        s = d
        L = cid
        while early_lock[d] is not None:
            yield
        early_lock[d] = cid
        cs = slice(c * 128, (c + 1) * 128)
        strict = (g.tri_lt, g.r_tri_lt) if d == 0 else (g.tri_gt, g.r_tri_gt)
        incl = (g.tri_le, g.r_tri_le) if d == 0 else (g.tri_ge, g.r_tri_ge)
        strictT = (g.tri_gt, g.r_tri_gt) if d == 0 else (g.tri_lt, g.r_tri_lt)
        k.dma("sp", cur[s][:], g.proj[cs, 1024:2048], reads=[g.r_proj], writes=[rcur[s]])
        if d == 0:
            if c == 0:
                k.op("pool", lambda e: e.memset(prv[s][:], 0.0), writes=[rprv[s]])
                k.dma("sp", prv[s][1:128, :], g.proj[0:127, 1024:1920], reads=[g.r_proj], writes=[rprv[s]])
            else:
                k.dma("sp", prv[s][:], g.proj[c * 128 - 1:c * 128 + 127, 1024:1920], reads=[g.r_proj], writes=[rprv[s]])
        else:
            if c == NT - 1:
                k.op("pool", lambda e: e.memset(prv[s][:], 0.0), writes=[rprv[s]])
                k.dma("sp", prv[s][0:127, :], g.proj[c * 128 + 1:T, 1024:1920], reads=[g.r_proj], writes=[rprv[s]])
            else:
                k.dma("sp", prv[s][:], g.proj[c * 128 + 1:c * 128 + 129, 1024:1920], reads=[g.r_proj], writes=[rprv[s]])
        k.op("pool", lambda e: e.tensor_tensor(out=prv[s][:], in0=prv[s][:], in1=cur[s][:, 0:896], op=ALU.subtract), reads=[rprv[s], rcur[s]], writes=[rprv[s]])
        k.op("pool", lambda e: e.tensor_tensor(out=prv[s][:], in0=prv[s][:], in1=mu_b[:], op=ALU.mult), reads=[rprv[s], rmu], writes=[rprv[s]])
        k.op("dve", lambda e: e.tensor_tensor(out=sh[s][:], in0=prv[s][:], in1=cur[s][:, 0:896], op=ALU.add), reads=[rprv[s], rcur[s]], writes=[rsh[s]])
        yield
        r_s = sh[s][:, 0:256]; k_s = sh[s][:, 256:512]; v_s = sh[s][:, 512:768]
        k.op("act", lambda e: e.activation(out=txw[s][:], in_=sh[s][:, 768:832], func=AF.Tanh), reads=[rsh[s]], writes=[rtxw[s]])
        b0 = BP.take()
        k.op("pe", lambda e: e.transpose(out=BP.t[0:64, b0, 0:128], in_=txw[s][:], identity=g.ident_f[:]), reads=[rtxw[s], g.r_ident_f], writes=[BP.r[b0]], inc=False)
        k.op("pe", lambda e: e.transpose(out=BP.t[0:64, b0, 128:256], in_=sh[s][:, 832:896], identity=g.ident_f[:]), reads=[rsh[s], g.r_ident_f], writes=[BP.r[b0]])
        k.op("act", lambda e: e.copy(out=xT[s][0:64].rearrange("p a b -> p (a b)"), in_=BP.t[0:64, b0, 0:256]), reads=[BP.r[b0]], writes=[rxT[s]])
        b1 = BP.take()
        for qi, UPt in enumerate([WUP[d], AUP[d]]):
            k.op("pe", lambda e: e.matmul(BP.t[:, b1, qi * 256:(qi + 1) * 256], lhsT=xT[s][:, qi, :], rhs=UPt[0][:], start=True, stop=True),
                 reads=[rxT[s], UPt[1]], writes=[BP.r[b1]], inc=(qi == 1))
        k.op("act", lambda e: e.activation(out=ld[s][:], in_=BP.t[:, b1, 0:256], func=AF.Sigmoid), reads=[BP.r[b1]], writes=[rld[s]])
        k.op("act", lambda e: e.activation(out=aa[s][:], in_=BP.t[:, b1, 256:512], func=AF.Sigmoid), reads=[BP.r[b1]], writes=[raa[s]])
        k.op("dve", lambda e: e.tensor_scalar(out=ld[s][:], in0=ld[s][:], scalar1=-math.exp(-0.5), scalar2=None, op0=ALU.mult), reads=[rld[s]], writes=[rld[s]])
        yield
        k.op("dve", lambda e: e.tensor_tensor(out=kk[s][:], in0=k_s, in1=kk_b[:], op=ALU.mult), reads=[rsh[s], rkk_b], writes=[rkk[s]])
        k.op("pool", lambda e: e.tensor_tensor(out=t1[s][:], in0=kk[s][:], in1=kk[s][:], op=ALU.mult), reads=[rkk[s]], writes=[rt1[s]])
        k.op("dve", lambda e: e.tensor_reduce(out=st[s][:, 0:4], in_=t1[s][:].rearrange("p (h e) -> p h e", h=4), axis=AX.X, op=ALU.add), reads=[rt1[s]], writes=[rst[s]])
        k.op("act", lambda e: e.activation(out=st[s][:, 4:8], in_=st[s][:, 0:4], func=AF.Sqrt), reads=[rst[s]], writes=[rst[s]])
        k.op("dve", lambda e: e.tensor_scalar(out=st[s][:, 4:8], in0=st[s][:, 4:8], scalar1=1e-12, scalar2=None, op0=ALU.max), reads=[rst[s]], writes=[rst[s]])
        k.op("dve", lambda e: e.reciprocal(out=st[s][:, 8:12], in_=st[s][:, 4:8]), reads=[rst[s]], writes=[rst[s]])
        k.op("dve", lambda e: e.tensor_tensor(out=kk[s][:].rearrange("p (h e) -> p h e", h=4), in0=kk[s][:].rearrange("p (h e) -> p h e", h=4),
                                              in1=st[s][:, 8:12].unsqueeze(2).to_broadcast([128, 4, 64]), op=ALU.mult), reads=[rkk[s], rst[s]], writes=[rkk[s]])
        k.op("pool", lambda e: e.tensor_tensor(out=t1[s][:], in0=aa[s][:], in1=ka_b[:], op=ALU.mult), reads=[raa[s], rka_b], writes=[rt1[s]])
        k.op("pool", lambda e: e.tensor_tensor(out=t1[s][:], in0=t1[s][:], in1=om_b[:], op=ALU.add), reads=[rt1[s], rom], writes=[rt1[s]])
        k.op("dve", lambda e: e.tensor_tensor(out=km[s][:], in0=k_s, in1=t1[s][:], op=ALU.mult), reads=[rsh[s], rt1[s]], writes=[rkm[s]])
        k.op("pool", lambda e: e.tensor_tensor(out=bb[s][:], in0=kk[s][:], in1=aa[s][:], op=ALU.mult), reads=[rkk[s], raa[s]], writes=[rbb[s]])
        k.op("pool", lambda e: e.tensor_tensor(out=t2[s][:], in0=r_s, in1=km[s][:], op=ALU.mult), reads=[rsh[s], rkm[s]], writes=[rt2[s]])
        k.op("pool", lambda e: e.tensor_tensor(out=t2[s][:], in0=t2[s][:], in1=rk_b[:], op=ALU.mult), reads=[rt2[s], rrk_b], writes=[rt2[s]])
        k.op("dve", lambda e: e.tensor_reduce(out=st[s][:, 12:16], in_=t2[s][:].rearrange("p (h e) -> p h e", h=4), axis=AX.X, op=ALU.add), reads=[rt2[s]], writes=[rst[s]])
        first = c not in visited
        visited.add(c)
        bdst = bo_all[:, c, :] if first else t2[s][:]
        k.op("dve", lambda e: e.tensor_tensor(out=bdst.rearrange("p (h e) -> p h e", h=4), in0=v_s.rearrange("p (h e) -> p h e", h=4),
                                              in1=st[s][:, 12:16].unsqueeze(2).to_broadcast([128, 4, 64]), op=ALU.mult),
             reads=[rsh[s], rst[s]], writes=[rbo_all[c] if first else rt2[s]])
        if not first:
            k.op("pool", lambda e: e.tensor_tensor(out=bo_all[:, c, :], in0=bo_all[:, c, :], in1=t2[s][:], op=ALU.add), reads=[rbo_all[c], rt2[s]], writes=[rbo_all[c]])
        yield
        b2 = BP.take()
        k.op("pe", lambda e: e.matmul(BP.t[:, b2, 0:256], lhsT=incl[0][:], rhs=ld[s][:], start=True, stop=True), reads=[incl[1], rld[s]], writes=[BP.r[b2]], inc=False)
        k.op("pe", lambda e: e.matmul(BP.t[:, b2, 256:512], lhsT=g.ones_f[:], rhs=ld[s][:], start=True, stop=True), reads=[g.r_ones_f, rld[s]], writes=[BP.r[b2]])
        k.op("act", lambda e: e.copy(out=LC[s][:], in_=BP.t[:, b2, :]), reads=[BP.r[b2]], writes=[rLC[s]])
        b3 = BP.take()
        for h in range(4):
            k.op("pe", lambda e: e.matmul(BP.t[0:64, b3, h:h + 1], lhsT=ld[s][:, h * 64:(h + 1) * 64], rhs=g.ones_f[:, 0:1], start=True, stop=True),
                 reads=[rld[s], g.r_ones_f], writes=[BP.r[b3]], inc=(h == 3))
        k.op("act", lambda e: e.activation(out=GCe[L][:], in_=BP.t[0:64, b3, 0:4], func=AF.Exp), reads=[BP.r[b3]], writes=[rGCe[L]])
        k.op("act", lambda e: e.activation(out=Einc[s][:], in_=LC[s][:, 0:256], func=AF.Exp), reads=[rLC[s]], writes=[rEinc[s]])
        k.op("act", lambda e: e.activation(out=Eneg[s][:], in_=LC[s][:, 0:256], func=AF.Exp, scale=-1.0), reads=[rLC[s]], writes=[rEneg[s]])
        k.op("dve", lambda e: e.tensor_tensor(out=Eex[s][:], in0=LC[s][:, 0:256], in1=ld[s][:], op=ALU.subtract), reads=[rLC[s], rld[s]], writes=[rEex[s]])
        k.op("act", lambda e: e.activation(out=Eex[s][:], in_=Eex[s][:], func=AF.Exp), reads=[rEex[s]], writes=[rEex[s]])
        k.op("dve", lambda e: e.tensor_tensor(out=Ehat[s][:], in0=LC[s][:, 256:512], in1=LC[s][:, 0:256], op=ALU.subtract), reads=[rLC[s]], writes=[rEhat[s]])
        k.op("act", lambda e: e.activation(out=Ehat[s][:], in_=Ehat[s][:], func=AF.Exp), reads=[rEhat[s]], writes=[rEhat[s]])
        k.op("dve", lambda e: e.scalar_tensor_tensor(out=OPS[L][:, 0, :], in0=kk[s][:], scalar=-1.0, in1=Eex[s][:], op0=ALU.mult, op1=ALU.mult), reads=[rkk[s], rEex[s]], writes=[rOPS[L]])
        k.op("pool", lambda e: e.tensor_tensor(out=OPS[L][:, 1, :], in0=r_s, in1=Einc[s][:], op=ALU.mult), reads=[rsh[s], rEinc[s]], writes=[rOPS[L]])
        k.op("dve", lambda e: e.tensor_tensor(out=OPS[L][:, 2, :], in0=bb[s][:], in1=Eneg[s][:], op=ALU.mult), reads=[rbb[s], rEneg[s]], writes=[rOPS[L]])
        k.op("pool", lambda e: e.tensor_tensor(out=OPS[L][:, 3, :], in0=km[s][:], in1=Eneg[s][:], op=ALU.mult), reads=[rkm[s], rEneg[s]], writes=[rOPS[L]])
        k.op("dve", lambda e: e.tensor_tensor(out=Bhat[L][:], in0=bb[s][:], in1=Ehat[s][:], op=ALU.mult), reads=[rbb[s], rEhat[s]], writes=[rBhat[L]])
        k.op("pool", lambda e: e.tensor_tensor(out=Khat[L][:], in0=km[s][:], in1=Ehat[s][:], op=ALU.mult), reads=[rkm[s], rEhat[s]], writes=[rKhat[L]])
        k.op("act", lambda e: e.copy(out=Vb[L][:], in_=v_s), reads=[rsh[s]], writes=[rVb[L]])
        early_lock[d] = None
        yield
        b4 = BP.take()
        pfv = BP.t[:, b4, :].bitcast(BF16)
        for hp in range(2):
            for xi in range(4):
                idx = hp * 4 + xi
                k.op("pe", lambda e: e.transpose(out=pfv[:, idx * 128:(idx + 1) * 128], in_=OPS[L][:, xi, hp * 128:(hp + 1) * 128], identity=g.ident_b[:]),
                     reads=[rOPS[L], g.r_ident_b], writes=[BP.r[b4]], inc=(idx == 7))
        k.op("act", lambda e: e.copy(out=FT[L][:].rearrange("p a b c -> p (a b c)"), in_=pfv), reads=[BP.r[b4]], writes=[rFT[L]])
        b5 = BP.take()
        prv_ = BP.t[0:64, b5, :].bitcast(BF16)
        for h in range(4):
            k.op("pe", lambda e: e.transpose(out=prv_[:, h * 128:(h + 1) * 128], in_=OPS[L][:, 1, h * 64:(h + 1) * 64], identity=g.ident_b[:]),
                 reads=[rOPS[L], g.r_ident_b], writes=[BP.r[b5]], inc=(h == 3))
        k.op("act", lambda e: e.copy(out=RT[L][:].rearrange("p a b -> p (a b)"), in_=prv_[:, 0:512]), reads=[BP.r[b5]], writes=[rRT[L]])
        yield
        for li, xi_l in enumerate([2, 3]):
            b6 = BP.take(2)
            for hp in range(2):
                for hh in range(2):
                    k.op("pe", lambda e: e.matmul(BP.t[:, b6 + hh, hp * 256:(hp + 1) * 256], lhsT=FT[L][hh * 64:(hh + 1) * 64, hp, xi_l, :],
                                                  rhs=FT[L][hh * 64:(hh + 1) * 64, hp, 0:2, :].rearrange("p a b -> p (a b)"), start=True, stop=True),
                         reads=[rFT[L]], writes=[BP.r[b6 + hh]], inc=(hp == 1))
            for hh in range(2):
                pv = BP.t[:, b6 + hh, :].rearrange("p (hp x t) -> p hp x t", hp=2, x=2)
                k.op("dve", lambda e: e.tensor_tensor(out=MK[L][:, 2 * li, hh], in0=pv[:, :, 0, :], in1=strict[0][:].unsqueeze(1).to_broadcast([128, 2, 128]), op=ALU.mult),
                     reads=[BP.r[b6 + hh], strict[1]], writes=[rMK[L]])
                k.op("dve", lambda e: e.tensor_tensor(out=MK[L][:, 2 * li + 1, hh], in0=pv[:, :, 1, :], in1=incl[0][:].unsqueeze(1).to_broadcast([128, 2, 128]), op=ALU.mult),
                     reads=[BP.r[b6 + hh], incl[1]], writes=[rMK[L]])
        yield
        b7 = BP.take(2)
        for hp in range(2):
            for hh in range(2):
                k.op("pe", lambda e: e.matmul(BP.t[:, b7 + hh, hp * 128:(hp + 1) * 128], lhsT=FT[L][hh * 64:(hh + 1) * 64, hp, 0, :],
                                              rhs=FT[L][hh * 64:(hh + 1) * 64, hp, 2, :], start=True, stop=True), reads=[rFT[L]], writes=[BP.r[b7 + hh]], inc=(hp == 1))
        for hh in range(2):
            k.op("dve", lambda e: e.tensor_tensor(out=X0[L][:, 0, hh * 2:hh * 2 + 2, :], in0=BP.t[:, b7 + hh, 0:256].rearrange("p (a b) -> p a b", a=2),
                                                  in1=strictT[0][:].unsqueeze(1).to_broadcast([128, 2, 128]), op=ALU.mult), reads=[BP.r[b7 + hh], strictT[1]], writes=[rX0[L]])
        k.op("pool", lambda e: e.tensor_copy(out=PA[L][:, :, 0, :], in_=MK[L][:, 0].rearrange("p a b c -> p (a b) c")), reads=[rMK[L]], writes=[rPA[L]])
        k.op("pool", lambda e: e.tensor_copy(out=PA[L][:, :, 1, :], in_=g.ident_b[:].unsqueeze(1).to_broadcast([128, 4, 128])), reads=[g.r_ident_b], writes=[rPA[L]])
        Xc, rXc, Xn, rXn = X0[L][:, 0], rX0[L], X1[L][:, 0], rX1[L]
        Pc, rPc, Pn, rPn = PA[L], rPA[L], PB[L], rPB[L]
        for it in range(6):
            b8 = BP.take()
            for hx in range(4):
                k.op("pe", lambda e: e.matmul(BP.t[:, b8, hx * 128:(hx + 1) * 128], lhsT=Pc[:, hx, 0, :], rhs=Xc[:, hx, :], start=True, stop=True),
                     reads=[rPc, rXc], writes=[BP.r[b8]], inc=(hx == 3))
            k.op("act", lambda e: e.copy(out=Xn.rearrange("p b c -> p (b c)"), in_=BP.t[:, b8, :]), reads=[BP.r[b8]], writes=[rXn])
            b9 = BP.take(2)
            if it < 5:
                for hx in range(4):
                    k.op("pe", lambda e: e.matmul(BP.t[:, b9 + hx // 2, (hx % 2) * 256:(hx % 2 + 1) * 256], lhsT=Xc[:, hx, :],
                                                  rhs=Pc[:, hx, :, :].rearrange("p a b -> p (a b)"), start=True, stop=True),
                         reads=[rXc, rPc], writes=[BP.r[b9 + hx // 2]], inc=(hx % 2 == 1))
                pv = BP.t[:, b9:b9 + 2, :].rearrange("p a (h x t) -> p (a h) x t", h=2, x=2)
                k.op("act", lambda e: e.copy(out=Pn[:, :, 0, :], in_=pv[:, :, 0, :]), reads=[BP.r[b9], BP.r[b9 + 1]], writes=[rPn])
                k.op("dve", lambda e: e.tensor_tensor(out=Pn[:, :, 1, :], in0=pv[:, :, 1, :], in1=Pc[:, :, 1, :], op=ALU.add),
                     reads=[BP.r[b9], BP.r[b9 + 1], rPc], writes=[rPn])
            else:
                for hx in range(4):
                    k.op("pe", lambda e: e.matmul(BP.t[:, b9, hx * 128:(hx + 1) * 128], lhsT=Xc[:, hx, :], rhs=Pc[:, hx, 1, :], start=True, stop=True),
                         reads=[rXc, rPc], writes=[BP.r[b9]], inc=(hx == 3))
                k.op("dve", lambda e: e.tensor_tensor(out=Pn[:, :, 1, :], in0=BP.t[:, b9, :].rearrange("p (h t) -> p h t", h=4), in1=Pc[:, :, 1, :], op=ALU.add),
                     reads=[BP.r[b9], rPc], writes=[rPn])
            Xc, rXc, Xn, rXn = Xn, rXn, Xc, rXc
            Pc, rPc, Pn, rPn = Pn, rPn, Pc, rPc
            yield
        b9 = BP.take()
        for hx in range(4):
            k.op("pe", lambda e: e.matmul(BP.t[:, b9, hx * 128:(hx + 1) * 128], lhsT=Xc[:, hx, :], rhs=Pc[:, hx, 1, :], start=True, stop=True),
                 reads=[rXc, rPc], writes=[BP.r[b9]], inc=(hx == 3))
        k.op("dve", lambda e: e.tensor_tensor(out=TA[L][:], in0=BP.t[:, b9, :].rearrange("p (h t) -> p h t", h=4), in1=Pc[:, :, 1, :], op=ALU.add),
             reads=[BP.r[b9], rPc], writes=[rTA[L]])
        Tc, rTc = TA[L], rTA[L]
        Tinv, rTinv = Tc, rTc
        yield
        b10 = BP.take()
        for h in range(4):
            hp, hh = h // 2, h % 2
            k.op("pe", lambda e: e.matmul(BP.t[:, b10, h * 64:(h + 1) * 64], lhsT=MK[L][:, 2, hh, hp, :], rhs=Vb[L][:, h * 64:(h + 1) * 64], start=True, stop=True),
                 reads=[rMK[L], rVb[L]], writes=[BP.r[b10]], inc=(h == 3))
        k.op("act", lambda e: e.copy(out=Xv[L][:], in_=BP.t[:, b10, 0:256]), reads=[BP.r[b10]], writes=[rXv[L]])
        b11 = BP.take()
        for h in range(4):
            hp, hh = h // 2, h % 2
            k.op("pe", lambda e: e.matmul(BP.t[0:64, b11, h * 128:(h + 1) * 128], lhsT=OPS[L][:, 0, h * 64:(h + 1) * 64], rhs=Tinv[:, hh * 2 + hp, :], start=True, stop=True),
                 reads=[rOPS[L], rTinv], writes=[BP.r[b11]], inc=(h == 3))
        k.op("act", lambda e: e.copy(out=WT[L][:].rearrange("p a b -> p (a b)"), in_=BP.t[0:64, b11, :]), reads=[BP.r[b11]], writes=[rWT[L]])
        yield
        while H_done[d] < n:
            yield
        b12 = BP.take()
        for h in range(4):
            hp, hh = h // 2, h % 2
            k.op("pe", lambda e: e.matmul(BP.t[:, b12, h * 64:(h + 1) * 64], lhsT=Tinv[:, hh * 2 + hp, :], rhs=Xv[L][:, h * 64:(h + 1) * 64], start=True, stop=False),
                 reads=[rTinv, rXv[L]], writes=[BP.r[b12]], inc=False)
            k.op("pe", lambda e: e.matmul(BP.t[:, b12, h * 64:(h + 1) * 64], lhsT=WT[L][:, h, :], rhs=Hbf[d][:, h, :], start=False, stop=True),
                 reads=[rWT[L], rHbf[d]], writes=[BP.r[b12]], inc=(h == 3))
        k.op("act", lambda e: e.copy(out=Ub[L][:], in_=BP.t[:, b12, 0:256]), reads=[BP.r[b12]], writes=[rUb[L]])
        yield
        b13 = BP.take()
        for h in range(4):
            hp, hh = h // 2, h % 2
            k.op("pe", lambda e: e.matmul(BP.t[:, b13, h * 64:(h + 1) * 64], lhsT=RT[L][:, h, :], rhs=Hbf[d][:, h, :], start=True, stop=False),
                 reads=[rRT[L], rHbf[d]], writes=[BP.r[b13]], inc=False)
            k.op("pe", lambda e: e.matmul(BP.t[:, b13, h * 64:(h + 1) * 64], lhsT=MK[L][:, 1, hh, hp, :], rhs=Ub[L][:, h * 64:(h + 1) * 64], start=False, stop=False),
                 reads=[rMK[L], rUb[L]], writes=[BP.r[b13]], inc=False)
            k.op("pe", lambda e: e.matmul(BP.t[:, b13, h * 64:(h + 1) * 64], lhsT=MK[L][:, 3, hh, hp, :], rhs=Vb[L][:, h * 64:(h + 1) * 64], start=False, stop=True),
                 reads=[rMK[L], rVb[L]], writes=[BP.r[b13]], inc=(h == 3))
        first_y = c not in visited_y
        visited_y.add(c)
        if first_y:
            k.op("act", lambda e: e.copy(out=y_all[:, c, :], in_=BP.t[:, b13, 0:256]), reads=[BP.r[b13]], writes=[ry_all[c]])
        else:
            k.op("dve", lambda e: e.tensor_tensor(out=y_all[:, c, :], in0=y_all[:, c, :], in1=BP.t[:, b13, 0:256], op=ALU.add), reads=[ry_all[c], BP.r[b13]], writes=[ry_all[c]])
        yield
        b14 = BP.take()
        for h in range(4):
            k.op("pe", lambda e: e.matmul(BP.t[0:64, b14, h * 64:(h + 1) * 64], lhsT=Bhat[L][:, h * 64:(h + 1) * 64], rhs=Ub[L][:, h * 64:(h + 1) * 64], start=True, stop=False),
                 reads=[rBhat[L], rUb[L]], writes=[BP.r[b14]], inc=False)
            k.op("pe", lambda e: e.matmul(BP.t[0:64, b14, h * 64:(h + 1) * 64], lhsT=Khat[L][:, h * 64:(h + 1) * 64], rhs=Vb[L][:, h * 64:(h + 1) * 64], start=False, stop=True),
                 reads=[rKhat[L], rVb[L]], writes=[BP.r[b14]], inc=(h == 3))
        k.op("dve", lambda e: e.tensor_tensor(out=H32[d][:], in0=H32[d][:], in1=GCe[L][:].unsqueeze(2).to_broadcast([64, 4, 64]), op=ALU.mult),
             reads=[rH32[d], rGCe[L]], writes=[rH32[d]])
        k.op("dve", lambda e: e.tensor_tensor(out=H32[d][:].rearrange("p a b -> p (a b)"), in0=H32[d][:].rearrange("p a b -> p (a b)"), in1=BP.t[0:64, b14, 0:256], op=ALU.add),
             reads=[rH32[d], BP.r[b14]], writes=[rH32[d]])
        k.op("act", lambda e: e.copy(out=Hbf[d][:], in_=H32[d][:]), reads=[rH32[d]], writes=[rHbf[d]])
        H_done[d] += 1

    import itertools
    S_ = 18
    gens = []
    for d_ in range(2):
        for par in range(2):
            order = [nn for nn in range(NT) if nn % 2 == par]
            cc = (lambda nn, d_=d_: nn if d_ == 0 else NT - 1 - nn)
            glist = [chunk(d_, cc(nn), nn, 2 * d_ + par) for nn in order]
            gens.append([itertools.chain.from_iterable(glist), (S_ // 2) * par + (S_ // 4) * d_, True])
    sentinel = object()
    rnd = 0
    while any(gg[2] for gg in gens):
        for gg in gens:
            if gg[2] and rnd >= gg[1]:
                if next(gg[0], sentinel) is sentinel:
                    gg[2] = False
        rnd += 1
        assert rnd < 100000
    k.scope_end()
    st, rst = T_([128, 16], F32)
    xg, rxg = T_([128, 128], F32)
    sg, rsg = T_([128, 128], BF16)
    sgT, rsgT = T_([128, 128], BF16)
    tmp, rtmp = T_([128, 256], F32)
    yf, ryf = T_([128, 256], F32)
    ob, rob = T_([128, 256], BF16)
    sg_all = k.sb("w_sgall", [128, NT, 128], BF16); rsg_all = [Res("w_sgall%d" % c) for c in range(NT)]
    for c in range(NT):
        s = c % 2
        k.dma("sp", xg[s][:], g.proj[c * 128:(c + 1) * 128, C_WXG:C_WXG + 128], reads=[g.r_proj], writes=[rxg[s]])
        k.op("act", lambda e: e.activation(out=sg_all[:, c, :], in_=xg[s][:], func=AF.Sigmoid), reads=[rxg[s]], writes=[rsg_all[c]])
    def fin(c):
        s = c % 2
        cs = slice(c * 128, (c + 1) * 128)
        b0 = BP.take()
        pfv = BP.t[:, b0, :].bitcast(BF16)
        k.op("pe", lambda e: e.transpose(out=pfv[:, 0:128], in_=sg_all[:, c, :], identity=g.ident_b[:]), reads=[rsg_all[c], g.r_ident_b], writes=[BP.r[b0]])
        k.op("act", lambda e: e.copy(out=sgT[s][:], in_=pfv[:, 0:128]), reads=[BP.r[b0]], writes=[rsgT[s]])
        b1 = BP.take()
        k.op("pe", lambda e: e.matmul(BP.t[:, b1, 0:256], lhsT=sgT[s][:], rhs=gup[:], start=True, stop=True), reads=[rsgT[s], rgup], writes=[BP.r[b1]])
        yield
        k.op("pool", lambda e: e.tensor_copy(out=yf[s][:], in_=y_all[:, c, :]), reads=[ry_all[c]], writes=[ryf[s]])
        head_norm_finalize(k, g, yf[s][:], ryf[s], 4, True, eps2, gnw[:], rgnw, tmp[s][:], rtmp[s], st[s], rst[s])
        k.op("pool", lambda e: e.tensor_tensor(out=yf[s][:], in0=yf[s][:], in1=bo_all[:, c, :], op=ALU.add), reads=[ryf[s], rbo_all[c]], writes=[ryf[s]])
        k.op("dve", lambda e: e.tensor_tensor(out=ob[s][:], in0=yf[s][:], in1=BP.t[:, b1, 0:256], op=ALU.mult), reads=[ryf[s], BP.r[b1]], writes=[rob[s]])
        yield
        b2 = BP.take()
        pf2 = BP.t[:, b2, :].bitcast(BF16)
        for hp in range(2):
            k.op("pe", lambda e: e.transpose(out=pf2[:, hp * 128:(hp + 1) * 128], in_=ob[s][:, hp * 128:(hp + 1) * 128], identity=g.ident_b[:]),
                 reads=[rob[s], g.r_ident_b], writes=[BP.r[b2]], inc=(hp == 1))
        k.op("act", lambda e: e.copy(out=oT[:, :, cs], in_=pf2[:, 0:256].rearrange("p (a b) -> p a b", a=2)), reads=[BP.r[b2]], writes=[roT])
    chains3 = []
    for par in range(2):
        glist = [fin(c) for c in range(NT) if c % 2 == par]
        chains3.append([itertools.chain.from_iterable(glist), 1 * par, True])
    rnd = 0
    while any(cg[2] for cg in chains3):
        for cg in chains3:
            if cg[2] and rnd >= cg[1]:
                if next(cg[0], sentinel) is sentinel:
                    cg[2] = False
        rnd += 1
        assert rnd < 100000
    for hp in range(2):
        k.dma("sp", g.mixedT[256 + hp * 128:256 + (hp + 1) * 128, :], oT[:, hp, :], reads=[roT], writes=[g.r_mixedT[1]])
    k.scope_end()


def phase_outproj_router(k, g, l):
    k.scope_begin()
    P = g.P
    BP = BankPool(k, "o_ps")
    Wo = k.sb("o_W", [128, 8, D], BF16); rWo = Res("o_W")
    k.dma("pool", Wo[:], P["w_out"][l].rearrange("(kc p) c -> p kc c", p=128), writes=[rWo], max_dma_last_dim=4096)
    wb = k.sb("o_nwb", [128, D], F32); rwb = Res("o_nwb")
    k.dma("sp", wb[:], bcast_rows(P["norm_ffn"][l], 128), writes=[rwb])
    Wr = k.sb("o_Wr", [128, 8, NE], BF16); rWr = Res("o_Wr")
    k.dma("pool", Wr[:], P["router_w"][l].rearrange("(kc p) e -> p kc e", p=128), writes=[rWr])
    rbias = k.sb("o_rb", [1, NE], F32); rrb = Res("o_rb")
    k.dma("sp", rbias[:], P["router_b"][l].unsqueeze(0), writes=[rrb])
    g.nrm_sq = k.sb("o_sq", [128, D], F32); g.r_nrm_sq = Res("o_sq")
    g.nrm_ss = [k.sb("o_ss%d" % i, [128, 4], F32) for i in range(2)]; g.r_nrm_ss = [Res("o_ss%d" % i) for i in range(2)]

    def dbl(name, shape, dt):
        return [k.sb("%s%d" % (name, i), shape, dt) for i in range(2)], [Res("%s%d" % (name, i)) for i in range(2)]
    mT, rmT = dbl("o_mT", [128, 8, 512], BF16)
    xt, rxt = dbl("o_xt", [128, D], F32)
    xn, rxn = dbl("o_xn", [128, D], BF16)
    xnT, rxnT = dbl("o_xnT", [128, 8, 128], BF16)
    sm, rsm = dbl("o_sm", [128, 8], F32)
    ex, rex = dbl("o_ex", [128, NE], F32)
    def stage1(j):
        gi, ti = j // 4, j % 4
        hs = gi % 2
        if ti == 0:
            k.dma("sp", mT[hs][:], g.mixedT[:, gi * 512:(gi + 1) * 512].rearrange("(kc p) t -> p kc t", p=128),
                  reads=g.r_mixedT, writes=[rmT[hs]])
        s = j % 2
        rows = slice(j * 128, (j + 1) * 128)
        k.dma("sp", xt[s][:], g.x_src[rows, :], reads=[g.r_x], writes=[rxt[s]])
        b0 = BP.take(2)
        for half in range(2):
            for kc in range(8):
                k.op("pe", lambda e: e.matmul(BP.t[:, b0 + half, :], lhsT=mT[hs][:, kc, ti * 128:(ti + 1) * 128],
                                              rhs=Wo[:, kc, half * 512:(half + 1) * 512], start=(kc == 0), stop=(kc == 7)),
                     reads=[rmT[hs], rWo], writes=[BP.r[b0 + half]], inc=(kc == 7))
        k.op("dve", lambda e: e.tensor_tensor(out=xt[s][:].rearrange("p (a b) -> p a b", a=2), in0=xt[s][:].rearrange("p (a b) -> p a b", a=2),
                                              in1=BP.t[:, b0:b0 + 2, :], op=ALU.add), reads=[rxt[s], BP.r[b0], BP.r[b0 + 1]], writes=[rxt[s]])
        k.dma("sp", g.x_cur[rows, :], xt[s][:], reads=[rxt[s]], writes=[g.r_xst])
        rmsnorm_tile(k, g, xt[s][:], rxt[s], wb[:], rwb, xn[s][:], rxn[s], j)
        k.dma("sp", g.xn2[rows, :], xn[s][:], reads=[rxn[s]], writes=[g.r_xn2])

    def stage2(j):
        s = j % 2
        b1 = BP.take()
        pfv = BP.t[:, b1, :].bitcast(BF16)
        for kc in range(8):
            k.op("pe", lambda e: e.transpose(out=pfv[:, kc * 128:(kc + 1) * 128], in_=xn[s][:, kc * 128:(kc + 1) * 128], identity=g.ident_b[:]),
                 reads=[rxn[s], g.r_ident_b], writes=[BP.r[b1]], inc=(kc == 7))
        k.op("act", lambda e: e.copy(out=xnT[s][:].rearrange("p a b -> p (a b)"), in_=pfv), reads=[BP.r[b1]], writes=[rxnT[s]])
        b2 = BP.take()
        for kc in range(8):
            k.op("pe", lambda e: e.matmul(BP.t[:, b2, 0:NE], lhsT=xnT[s][:, kc, :], rhs=Wr[:, kc, :], start=(kc == 0), stop=False),
                 reads=[rxnT[s], rWr], writes=[BP.r[b2]], inc=False)
        k.op("pe", lambda e: e.matmul(BP.t[:, b2, 0:NE], lhsT=g.ones_f[0:1, :], rhs=rbias[:], start=False, stop=True),
             reads=[g.r_ones_f, rrb], writes=[BP.r[b2]])
        k.op("dve", lambda e: e.tensor_reduce(out=sm[s][:, 0:1], in_=BP.t[:, b2, 0:NE], axis=AX.X, op=ALU.max), reads=[BP.r[b2]], writes=[rsm[s]])
        k.op("dve", lambda e: e.tensor_scalar(out=sm[s][:, 1:2], in0=sm[s][:, 0:1], scalar1=-1.0, scalar2=None, op0=ALU.mult), reads=[rsm[s]], writes=[rsm[s]])
        k.op("act", lambda e: e.activation(out=ex[s][:], in_=BP.t[:, b2, 0:NE], func=AF.Exp, bias=sm[s][:, 1:2], accum_out=sm[s][:, 2:3]),
             reads=[BP.r[b2], rsm[s]], writes=[rex[s], rsm[s]])
        k.op("dve", lambda e: e.reciprocal(out=sm[s][:, 3:4], in_=sm[s][:, 2:3]), reads=[rsm[s]], writes=[rsm[s]])
        k.op("dve", lambda e: e.tensor_scalar(out=g.aff_all[:, j, :], in0=ex[s][:], scalar1=sm[s][:, 3:4], scalar2=None, op0=ALU.mult),
             reads=[rex[s], rsm[s]], writes=[g.r_aff])

    g.r_xst = Res("x_store")
    stage1(0)
    for j in range(NT):
        if j + 1 < NT:
            stage1(j + 1)
        stage2(j)
    g.r_x = Res("x_cur_next")
    g.x_src = g.x_cur
    k.scope_end()


def phase_moe(k, g, l):
    k.scope_begin()
    P = g.P
    BP = BankPool(k, "m_ps")
    aff = g.aff_all
    Wn = ["exp_w_gate", "exp_w_up", "exp_w_down"]
    Wt = [[k.sb("m_W%d_%d" % (i, b), [128, 8, D], BF16) for i in range(3)] for b in range(2)]
    rWt = [[Res("m_W%d_%d" % (i, b)) for i in range(3)] for b in range(2)]

    def load_w(ei):
        b = ei % 2
        for i in range(3):
            k.dma("pool", Wt[b][i][:], P[Wn[i]][l, ei].rearrange("(kc p) f -> p kc f", p=128), writes=[rWt[b][i]], max_dma_last_dim=4096)
    load_w(0)
    A3 = [128, NT, NE]
    th = k.sb("m_th", [128, NE], F32); rth = Res("m_th")
    lo = k.sb("m_lo", [128, NE], F32); rlo = Res("m_lo")
    ge = k.sb("m_ge", [128, NE], F32); rge = Res("m_ge")
    tq = k.sb("m_tq", [128, NE], F32); rtq = Res("m_tq")
    pc = k.sb("m_pc", [128, NE], F32); rpc = Res("m_pc")
    cmp_ = k.sb("m_cmp", A3, F32); rcmp = Res("m_cmp")
    k.op("pool", lambda e: e.memset(th[:], 0.5), writes=[rth])
    k.op("pool", lambda e: e.memset(lo[:], 0.0), writes=[rlo])
    step = 0.25
    for it in range(24):
        k.op("dve", lambda e: e.tensor_tensor(out=cmp_[:], in0=aff[:], in1=th[:].unsqueeze(1).to_broadcast(A3), op=ALU.is_gt),
             reads=[g.r_aff, rth], writes=[rcmp])
        k.op("dve", lambda e: e.tensor_reduce(out=pc[:], in_=cmp_[:].rearrange("p j e -> p e j"), axis=AX.X, op=ALU.add), reads=[rcmp], writes=[rpc])
        b0 = BP.take()
        k.op("pe", lambda e: e.matmul(BP.t[:, b0, 0:NE], lhsT=g.ones_f[:], rhs=pc[:], start=True, stop=True), reads=[g.r_ones_f, rpc], writes=[BP.r[b0]])
        k.op("dve", lambda e: e.tensor_scalar(out=ge[:], in0=BP.t[:, b0, 0:NE], scalar1=float(CAP) - 0.5, scalar2=None, op0=ALU.is_ge), reads=[BP.r[b0]], writes=[rge])
        k.op("dve", lambda e: e.tensor_tensor(out=tq[:], in0=th[:], in1=ge[:], op=ALU.mult), reads=[rth, rge], writes=[rtq])
        k.op("dve", lambda e: e.tensor_tensor(out=lo[:], in0=lo[:], in1=tq[:], op=ALU.max), reads=[rlo, rtq], writes=[rlo])
        k.op("dve", lambda e: e.tensor_scalar(out=tq[:], in0=ge[:], scalar1=2.0 * step, scalar2=-step, op0=ALU.mult, op1=ALU.add), reads=[rge], writes=[rtq])
        k.op("dve", lambda e: e.tensor_tensor(out=th[:], in0=th[:], in1=tq[:], op=ALU.add), reads=[rth, rtq], writes=[rth])
        step *= 0.5
    mask = k.sb("m_mask", A3, F32); rmask = Res("m_mask")
    k.op("dve", lambda e: e.tensor_tensor(out=mask[:], in0=aff[:], in1=lo[:].unsqueeze(1).to_broadcast(A3), op=ALU.is_gt), reads=[g.r_aff, rlo], writes=[rmask])
    ca = k.sb("m_ca", A3, F32); rca = Res("m_ca")
    cb = k.sb("m_cb", A3, F32); rcb = Res("m_cb")
    k.op("dve", lambda e: e.tensor_copy(out=ca[:], in_=mask[:]), reads=[rmask], writes=[rca])
    src, rsrc, dst, rdst = ca, rca, cb, rcb
    for sft in (1, 2, 4, 8, 16):
        k.op("dve", lambda e: e.tensor_tensor(out=dst[:, sft:, :], in0=src[:, sft:, :], in1=src[:, 0:NT - sft, :], op=ALU.add), reads=[rsrc], writes=[rdst])
        k.op("dve", lambda e: e.tensor_copy(out=dst[:, 0:sft, :], in_=src[:, 0:sft, :]), reads=[rsrc], writes=[rdst])
        src, rsrc, dst, rdst = dst, rdst, src, rsrc
    mask_b = k.sb("m_maskb", A3, BF16); rmaskb = Res("m_maskb")
    cum_b = k.sb("m_cumb", A3, BF16); rcumb = Res("m_cumb")
    k.op("dve", lambda e: e.tensor_copy(out=mask_b[:], in_=mask[:]), reads=[rmask], writes=[rmaskb])
    k.op("dve", lambda e: e.tensor_tensor(out=cum_b[:], in0=src[:], in1=mask[:], op=ALU.subtract), reads=[rsrc, rmask], writes=[rcumb])
    tlt_b = k.sb("m_tltb", [128, 128], BF16); rtltb = Res("m_tltb")
    k.op("dve", lambda e: e.tensor_copy(out=tlt_b[:], in_=g.tri_lt[:]), reads=[g.r_tri_lt], writes=[rtltb])
    b0 = BP.take()
    k.op("pe", lambda e: e.matmul(BP.t[:, b0, :], lhsT=tlt_b[:], rhs=mask_b[:].rearrange("p j e -> p (j e)"), start=True, stop=False),
         reads=[rtltb, rmaskb], writes=[BP.r[b0]], inc=False)
    k.op("pe", lambda e: e.matmul(BP.t[:, b0, :], lhsT=g.ones_b[:], rhs=cum_b[:].rearrange("p j e -> p (j e)"), start=False, stop=True),
         reads=[g.r_ones_b, rcumb], writes=[BP.r[b0]])
    rho = k.sb("m_rho", A3, F32); rrho = Res("m_rho")
    k.op("dve", lambda e: e.scalar_tensor_tensor(out=rho[:].rearrange("p j e -> p (j e)"), in0=BP.t[:, b0, :], scalar=1.0,
                                                 in1=mask[:].rearrange("p j e -> p (j e)"), op0=ALU.add, op1=ALU.mult), reads=[BP.r[b0], rmask], writes=[rrho])
    k.op("dve", lambda e: e.tensor_scalar(out=rho[:], in0=rho[:], scalar1=-1.0, scalar2=None, op0=ALU.add), reads=[rrho], writes=[rrho])
    Rbig = k.sb("m_R", [128, NE * NT * 4 + 32], BF16); rR = Res("m_R")
    k.op("pool", lambda e: e.memset(Rbig[:], 0.0), writes=[rR])
    Rall = Rbig[:, 0:NE * NT * 4].rearrange("p (e j r) -> p e j r", e=NE, j=NT)
    ahi = k.sb("m_ahi", A3, BF16); rahi = Res("m_ahi")
    alo = k.sb("m_alo", A3, F32); ralo = Res("m_alo")
    jf = k.sb("m_jf", [128, NT], F32); rjf = Res("m_jf")
    k.op("pool", lambda e: e.iota(jf[:], pattern=[[1, NT]], base=0, channel_multiplier=0, allow_small_or_imprecise_dtypes=True), writes=[rjf])
    k.op("dve", lambda e: e.tensor_copy(out=ahi[:], in_=aff[:]), reads=[g.r_aff], writes=[rahi])
    k.op("dve", lambda e: e.tensor_tensor(out=alo[:], in0=aff[:], in1=ahi[:], op=ALU.subtract), reads=[g.r_aff, rahi], writes=[ralo])
    k.op("dve", lambda e: e.tensor_copy(out=Rall[:, :, :, 0], in_=g.pidx[:, 0:1].unsqueeze(2).to_broadcast([128, NE, NT])), reads=[g.r_pidx], writes=[rR])
    k.op("dve", lambda e: e.tensor_copy(out=Rall[:, :, :, 1], in_=jf[:].unsqueeze(1).to_broadcast([128, NE, NT])), reads=[rjf], writes=[rR])
    k.op("dve", lambda e: e.tensor_copy(out=Rall[:, :, :, 2], in_=ahi[:].rearrange("p j e -> p e j")), reads=[rahi], writes=[rR])
    k.op("dve", lambda e: e.tensor_copy(out=Rall[:, :, :, 3], in_=alo[:].rearrange("p j e -> p e j")), reads=[ralo], writes=[rR])
    idxf = k.sb("m_idxf", [128, NE, 4, 4], F32); ridxf = Res("m_idxf")
    tokf = k.sb("m_tokf", [128, NE, 4], F32); rtokf = Res("m_tokf")
    idxi = k.sb("m_idxi", [128, NE, 4], I32); ridxi = Res("m_idxi")
    gat = k.sb("m_gat", [128, NE, 4], F32); rgat = Res("m_gat")
    NSEL = 6
    sel = [k.sb("m_sel%d" % i, [128, CAP], BF16) for i in range(NSEL)]; rsel = [Res("m_sel%d" % i) for i in range(NSEL)]
    res4 = [k.sb("m_res%d" % i, [4, CAP], F32) for i in range(2)]; rres4 = [Res("m_res%d" % i) for i in range(2)]
    nsel = 0
    res32 = [k.sb("m_res%d" % i, [32, CAP], F32) for i in range(2)]; rres32 = [Res("m_res%d" % i) for i in range(2)]
    for ei in range(NE):
        b1 = BP.take()
        for j in range(NT):
            si = nsel % NSEL; nsel += 1
            k.op("dve", lambda e: e.tensor_scalar(out=sel[si][:], in0=g.iota_c16[:], scalar1=rho[:, j, ei:ei + 1], scalar2=None, op0=ALU.is_equal),
                 reads=[g.r_iota_c16, rrho], writes=[rsel[si]])
            off = (ei * NT + j) * 4
            k.op("pe", lambda e: e.matmul(BP.t[0:32, b1, :], lhsT=Rbig[:, off:off + 32], rhs=sel[si][:], start=(j == 0), stop=(j == NT - 1)),
                 reads=[rR, rsel[si]], writes=[BP.r[b1]], inc=True)
        s = ei % 2
        k.op("act", lambda e: e.copy(out=res32[s][:], in_=BP.t[0:32, b1, :]), reads=[BP.r[b1]], writes=[rres32[s]])
        b2 = BP.take()
        for q in range(4):
            k.op("pe", lambda e: e.transpose(out=BP.t[:, b2, q * 32:(q + 1) * 32], in_=res32[s][:, q * 128:(q + 1) * 128], identity=g.ident_f[0:32, 0:32]),
                 reads=[rres32[s], g.r_ident_f], writes=[BP.r[b2]], inc=(q == 3))
        k.op("act", lambda e: e.copy(out=idxf[:, ei], in_=BP.t[:, b2, 0:128].rearrange("p (q c) -> p q c", q=4)[:, :, 0:4]), reads=[BP.r[b2]], writes=[ridxf])
    k.op("dve", lambda e: e.scalar_tensor_tensor(out=tokf[:], in0=idxf[:, :, :, 1], scalar=128.0, in1=idxf[:, :, :, 0], op0=ALU.mult, op1=ALU.add),
         reads=[ridxf], writes=[rtokf])
    k.op("dve", lambda e: e.tensor_scalar(out=tokf[:], in0=tokf[:], scalar1=0.0, scalar2=float(T - 1), op0=ALU.max, op1=ALU.min), reads=[rtokf], writes=[rtokf])
    k.op("dve", lambda e: e.tensor_copy(out=idxi[:], in_=tokf[:]), reads=[rtokf], writes=[ridxi])
    k.op("dve", lambda e: e.tensor_tensor(out=gat[:], in0=idxf[:, :, :, 2], in1=idxf[:, :, :, 3], op=ALU.add), reads=[ridxf], writes=[rgat])
    xs = [[k.sb("m_xs%d_%d" % (i, bb_), [128, D], BF16) for i in range(4)] for bb_ in range(2)]
    rxs = [[Res("m_xs%d_%d" % (i, bb_)) for i in range(4)] for bb_ in range(2)]
    xsT = [k.sb("m_xsT%d" % bb_, [128, 8, CAP], BF16) for bb_ in range(2)]; rxsT = [Res("m_xsT%d" % bb_) for bb_ in range(2)]
    hidT = k.sb("m_hidT", [128, 8, CAP], BF16); rhidT = Res("m_hidT")
    sg = [k.sb("m_sg%d" % i, [128, CAP], F32) for i in range(2)]; rsg = [Res("m_sg%d" % i) for i in range(2)]
    osb = [k.sb("m_osb%d" % i, [128, D], F32) for i in range(2)]; rosb = [Res("m_osb%d" % i) for i in range(2)]

    def gathers(ei):
        b_ = ei % 2
        for q in range(4):
            k.dma_raw("pool", lambda e: e.indirect_dma_start(out=xs[b_][q][:], out_offset=None, in_=g.xn2[:, :],
                                                              in_offset=bass.IndirectOffsetOnAxis(ap=idxi[:, ei, q:q + 1], axis=0)),
                      reads=[ridxi, g.r_xn2], writes=[rxs[b_][q]])

    def transposes(ei):
        b_ = ei % 2
        for q in range(4):
            b3 = BP.take()
            pfv = BP.t[:, b3, :].bitcast(BF16)
            for kc in range(8):
                k.op("pe", lambda e: e.transpose(out=pfv[:, kc * 128:(kc + 1) * 128], in_=xs[b_][q][:, kc * 128:(kc + 1) * 128], identity=g.ident_b[:]),
                     reads=[rxs[b_][q], g.r_ident_b], writes=[BP.r[b3]], inc=(kc == 7))
            k.op("act", lambda e: e.copy(out=xsT[b_][:, :, q * 128:(q + 1) * 128], in_=pfv.rearrange("p (a b) -> p a b", a=8)), reads=[BP.r[b3]], writes=[rxsT[b_]])

    gathers(0)
    transposes(0)
    nos = 0
    for ei in range(NE):
        b = ei % 2
        if ei + 1 < NE:
            load_w(ei + 1)
            gathers(ei + 1)
        for fc in range(8):
            b4 = BP.take(2)
            for wi in range(2):
                for kc in range(8):
                    k.op("pe", lambda e: e.matmul(BP.t[:, b4 + wi, :], lhsT=Wt[b][wi][:, kc, fc * 128:(fc + 1) * 128], rhs=xsT[b][:, kc, :],
                                                  start=(kc == 0), stop=(kc == 7)), reads=[rWt[b][wi], rxsT[b]], writes=[BP.r[b4 + wi]], inc=(kc == 7))
            s2 = fc % 2
            k.op("act", lambda e: e.activation(out=sg[s2][:], in_=BP.t[:, b4, :], func=AF.Silu), reads=[BP.r[b4]], writes=[rsg[s2]])
            k.op("dve", lambda e: e.tensor_tensor(out=hidT[:, fc, :], in0=sg[s2][:], in1=BP.t[:, b4 + 1, :], op=ALU.mult),
                 reads=[rsg[s2], BP.r[b4 + 1]], writes=[rhidT])
        for q in range(4):
            so = nos % 2; nos += 1
            b5 = BP.take(2)
            for half in range(2):
                for fc in range(8):
                    k.op("pe", lambda e: e.matmul(BP.t[:, b5 + half, :], lhsT=hidT[:, fc, q * 128:(q + 1) * 128], rhs=Wt[b][2][:, fc, half * 512:(half + 1) * 512],
                                                  start=(fc == 0), stop=(fc == 7)), reads=[rhidT, rWt[b][2]], writes=[BP.r[b5 + half]], inc=(fc == 7))
            k.op("dve", lambda e: e.tensor_scalar(out=osb[so][:].rearrange("p (a b) -> p a b", a=2), in0=BP.t[:, b5:b5 + 2, :],
                                                  scalar1=gat[:, ei, q:q + 1], scalar2=None, op0=ALU.mult), reads=[BP.r[b5], BP.r[b5 + 1], rgat], writes=[rosb[so]])
            k.dma_raw("pool", lambda e: e.indirect_dma_start(out=g.x_cur[:, :], out_offset=bass.IndirectOffsetOnAxis(ap=idxi[:, ei, q:q + 1], axis=0),
                                                              in_=osb[so][:], in_offset=None, compute_op=ALU.add),
                      reads=[ridxi, rosb[so]], writes=[g.r_x])
        if ei + 1 < NE:
            transposes(ei + 1)
    k.scope_end()


def phase_final(k, g):
    k.scope_begin()
    wb = k.sb("f_nwb", [128, D], F32); rwb = Res("f_nwb")
    k.dma("sp", wb[:], bcast_rows(g.P["norm_final"], 128), writes=[rwb])
    g.nrm_sq = k.sb("f_sq", [128, D], F32); g.r_nrm_sq = Res("f_sq")
    g.nrm_ss = [k.sb("f_ss%d" % i, [128, 4], F32) for i in range(2)]; g.r_nrm_ss = [Res("f_ss%d" % i) for i in range(2)]
    xt = [k.sb("f_xt%d" % i, [128, D], F32) for i in range(2)]; rxt = [Res("f_xt%d" % i) for i in range(2)]
    ot = [k.sb("f_ot%d" % i, [128, D], F32) for i in range(2)]; rot = [Res("f_ot%d" % i) for i in range(2)]
    for j in range(NT):
        s = j % 2
        k.dma("sp", xt[s][:], g.x_cur[j * 128:(j + 1) * 128, :], reads=[g.r_x], writes=[rxt[s]])
        rmsnorm_tile(k, g, xt[s][:], rxt[s], wb[:], rwb, ot[s][:], rot[s], j)
        k.dma("sp", g.out[j * 128:(j + 1) * 128, :], ot[s][:], reads=[rot[s]], writes=[g.r_out])
    k.scope_end()
```
